# Optimizing a Trainium2 kernel written in Bass

```python
import jax, jax.numpy as jnp
from jax import lax
import numpy as np

D_MODEL = 2048
BATCH = 4
SEQ = 8192
DEPTH = 1

CHUNK = 64
Q_BLOCK = 128
EPS = 1e-6
D_RNN = 2048
RNN_BLOCKS = 16
RNN_BLOCK_DIM = D_RNN // RNN_BLOCKS
CONV_WIDTH = 4
LRU_C = 8.0
N_HEADS = 16
Q_LORA = 512
KV_LORA = 256
QK_NOPE = 128
QK_ROPE = 64
QK_HEAD = QK_NOPE + QK_ROPE
V_HEAD = 128
ROPE_THETA = 10000.0
N_BRANCH = 2
IN_WIDTHS = (D_RNN, D_RNN, Q_LORA, KV_LORA, QK_ROPE, N_BRANCH * D_MODEL)
D_IN = 2 * D_RNN + Q_LORA + KV_LORA + QK_ROPE + N_BRANCH * D_MODEL
N_EXPERTS = 64
TOP_K = 6
D_EXPERT = 1408
D_SHARED = 1408
ROUTED_SCALE = 2.5
MOE_BLOCK = 256
MOD_SCALE = 0.25

kernel_name = 'hybrid_rglru_mla_moe_adaln_block'


def rmsnorm(x, g):
    xf = x.astype(jnp.float32)
    y = xf * lax.rsqrt(jnp.mean(xf * xf, axis=-1, keepdims=True) + EPS)
    return (y * g.astype(jnp.float32)).astype(x.dtype)


def modulate(x, g, shift, scale):
    return rmsnorm(x, g) * (1 + scale[:, None, :]) + shift[:, None, :]


def apply_rope(x, cos, sin):
    x1, x2 = jnp.split(x, 2, axis=-1)
    return jnp.concatenate([x1 * cos - x2 * sin, x2 * cos + x1 * sin], axis=-1)


def swiglu(x, wg, wu, wd):
    return (jax.nn.silu(x @ wg) * (x @ wu)) @ wd


def rglru_branch(xr, yg, conv_w, conv_b, w_a, b_a, w_i, b_i, lam):
    B_, S, _ = xr.shape
    xp = jnp.pad(xr, ((0, 0), (CONV_WIDTH - 1, 0), (0, 0)))
    xc = conv_b + xp[:, 0:S, :] * conv_w[0]
    for j in range(1, CONV_WIDTH):
        xc = xc + xp[:, j:j + S, :] * conv_w[j]
    xb = xc.reshape(B_, S, RNN_BLOCKS, RNN_BLOCK_DIM)
    r = jax.nn.sigmoid(jnp.einsum('bshi,hij->bshj', xb, w_a).reshape(B_, S, D_RNN) + b_a)
    i = jax.nn.sigmoid(jnp.einsum('bshi,hij->bshj', xb, w_i).reshape(B_, S, D_RNN) + b_i)
    log_a = -LRU_C * r.astype(jnp.float32) * jax.nn.softplus(-lam.astype(jnp.float32))
    a = jnp.exp(log_a)
    b = jnp.sqrt(-jnp.expm1(2.0 * log_a)) * (i * xc).astype(jnp.float32)

    def step(h, ab):
        a_t, b_t = ab
        h = a_t * h + b_t
        return h, h

    _, hs = lax.scan(step, jnp.zeros((B_, D_RNN), jnp.float32),
                     (a.transpose(1, 0, 2), b.transpose(1, 0, 2)))
    h = hs.transpose(1, 0, 2).astype(xr.dtype)
    return h * jax.nn.gelu(yg)


def mla_branch(c_q, c_kv, k_rope, positions, q_a_norm, kv_a_norm, w_uq, w_ukv, q_norm, k_norm):
    B_, S, _ = c_q.shape
    q = jnp.einsum('bsr,rhd->bshd', rmsnorm(c_q, q_a_norm), w_uq)
    kv = jnp.einsum('bsr,rhd->bshd', rmsnorm(c_kv, kv_a_norm), w_ukv)
    k_nope, v = kv[..., :QK_NOPE], kv[..., QK_NOPE:]
    k = jnp.concatenate([k_nope, jnp.broadcast_to(k_rope[:, :, None, :], (B_, S, N_HEADS, QK_ROPE))], axis=-1)
    q = rmsnorm(q, q_norm)
    k = rmsnorm(k, k_norm)
    inv_freq = ROPE_THETA ** (-jnp.arange(0, QK_ROPE, 2, dtype=jnp.float32) / QK_ROPE)
    ang = positions.astype(jnp.float32)[..., None] * inv_freq
    cos = jnp.cos(ang)[:, :, None, :].astype(q.dtype)
    sin = jnp.sin(ang)[:, :, None, :].astype(q.dtype)
    q = jnp.concatenate([q[..., :QK_NOPE], apply_rope(q[..., QK_NOPE:], cos, sin)], axis=-1)
    k = jnp.concatenate([k[..., :QK_NOPE], apply_rope(k[..., QK_NOPE:], cos, sin)], axis=-1)
    nqb = S // Q_BLOCK
    qb = q.reshape(B_, nqb, Q_BLOCK, N_HEADS, QK_HEAD).transpose(1, 0, 2, 3, 4)
    key_chunk = jnp.arange(S) // CHUNK
    scale = QK_HEAD ** -0.5

    def attend(args):
        q_blk, blk = args
        q_chunk = (blk * Q_BLOCK + jnp.arange(Q_BLOCK)) // CHUNK
        s = jnp.einsum('bqhd,bkhd->bhqk', q_blk, k).astype(jnp.float32) * scale
        mask = key_chunk[None, :] <= q_chunk[:, None]
        s = jnp.where(mask, s, -jnp.inf)
        p = jax.nn.softmax(s, axis=-1).astype(v.dtype)
        return jnp.einsum('bhqk,bkhd->bqhd', p, v)

    o = lax.map(attend, (qb, jnp.arange(nqb)))
    return o.transpose(1, 0, 2, 3, 4).reshape(B_, S, N_HEADS * V_HEAD)


def moe(u, w_router, router_bias, w_gate, w_up, w_down, ws_gate, ws_up, ws_down):
    B_, S, D = u.shape
    T = B_ * S
    xf = u.reshape(T, D)
    scores = jax.nn.sigmoid(xf.astype(jnp.float32) @ w_router.astype(jnp.float32))
    _, top_idx = lax.top_k(scores + router_bias.astype(jnp.float32), TOP_K)
    top_s = jnp.take_along_axis(scores, top_idx, axis=-1)
    top_w = top_s / jnp.sum(top_s, axis=-1, keepdims=True) * ROUTED_SCALE
    TK = T * TOP_K
    e_flat = top_idx.reshape(TK)
    tok_flat = jnp.arange(TK, dtype=jnp.int32) // TOP_K
    w_flat = top_w.reshape(TK)
    order = jnp.argsort(e_flat)
    sorted_e = e_flat[order]
    sizes = jnp.bincount(e_flat, length=N_EXPERTS)
    padded = (sizes + MOE_BLOCK - 1) // MOE_BLOCK * MOE_BLOCK
    pad_end = jnp.cumsum(padded)
    pad_start = pad_end - padded
    grp_start = jnp.cumsum(sizes) - sizes
    dest = pad_start[sorted_e] + jnp.arange(TK) - grp_start[sorted_e]
    nb = -(-TK // MOE_BLOCK) + N_EXPERTS
    row_tok = jnp.full((nb * MOE_BLOCK,), T, jnp.int32).at[dest].set(tok_flat[order])
    row_w = jnp.zeros((nb * MOE_BLOCK,), jnp.float32).at[dest].set(w_flat[order])
    blk_e = jnp.minimum(jnp.searchsorted(pad_end, jnp.arange(nb) * MOE_BLOCK, side='right'), N_EXPERTS - 1)
    x_pad = jnp.concatenate([xf, jnp.zeros((1, D), xf.dtype)], axis=0)

    def step(acc, blk):
        rows, wts, e = blk
        xb = x_pad[rows]
        y = swiglu(xb, w_gate[e], w_up[e], w_down[e]) * wts[:, None].astype(xb.dtype)
        return acc.at[rows].add(y), None

    acc, _ = lax.scan(step, jnp.zeros_like(x_pad),
                      (row_tok.reshape(nb, MOE_BLOCK), row_w.reshape(nb, MOE_BLOCK), blk_e))
    out = acc[:T] + swiglu(xf, ws_gate, ws_up, ws_down)
    return out.reshape(B_, S, D)


def setup_inputs(seed: int = 0) -> dict:
    key = jax.random.key(seed)
    ks = iter(jax.random.split(key, 40))
    f32 = jnp.float32

    def nrm(shape, scale):
        return jax.random.normal(next(ks), shape, f32) * scale

    def gain(shape):
        return 1.0 + 0.02 * jax.random.normal(next(ks), shape, f32)

    L = DEPTH
    x = jax.random.normal(next(ks), (BATCH, SEQ, D_MODEL), f32)
    c = jax.random.normal(next(ks), (BATCH, D_MODEL), f32)
    positions = (jnp.arange(SEQ, dtype=jnp.int32)[None, :]
                 + jax.random.randint(next(ks), (BATCH, 1), 0, 4096, dtype=jnp.int32))
    a0 = jax.random.uniform(next(ks), (L, D_RNN), f32, 0.9, 0.999) ** (1.0 / LRU_C)
    lru_lambda = jnp.log(a0) - jnp.log1p(-a0)
    return {
        'x': x,
        'c': c,
        'positions': positions,
        'w_mod': nrm((L, D_MODEL, 6 * D_MODEL), MOD_SCALE * D_MODEL ** -0.5),
        'b_mod': nrm((L, 6 * D_MODEL), 0.02),
        'norm1': gain((L, D_MODEL)),
        'w_in': nrm((L, D_MODEL, D_IN), D_MODEL ** -0.5),
        'conv_w': nrm((L, CONV_WIDTH, D_RNN), CONV_WIDTH ** -0.5),
        'conv_b': nrm((L, D_RNN), 0.02),
        'w_a': nrm((L, RNN_BLOCKS, RNN_BLOCK_DIM, RNN_BLOCK_DIM), RNN_BLOCK_DIM ** -0.5),
        'b_a': nrm((L, D_RNN), 0.02),
        'w_i': nrm((L, RNN_BLOCKS, RNN_BLOCK_DIM, RNN_BLOCK_DIM), RNN_BLOCK_DIM ** -0.5),
        'b_i': nrm((L, D_RNN), 0.02),
        'lru_lambda': lru_lambda,
        'q_a_norm': gain((L, Q_LORA)),
        'kv_a_norm': gain((L, KV_LORA)),
        'w_uq': nrm((L, Q_LORA, N_HEADS, QK_HEAD), Q_LORA ** -0.5),
        'w_ukv': nrm((L, KV_LORA, N_HEADS, QK_NOPE + V_HEAD), KV_LORA ** -0.5),
        'q_norm': gain((L, QK_HEAD)),
        'k_norm': gain((L, QK_HEAD)),
        'w_rnn_out': nrm((L, D_RNN, D_MODEL), D_RNN ** -0.5),
        'w_mla_out': nrm((L, N_HEADS * V_HEAD, D_MODEL), (N_HEADS * V_HEAD) ** -0.5),
        'w_out': nrm((L, D_MODEL, D_MODEL), D_MODEL ** -0.5),
        'norm2': gain((L, D_MODEL)),
        'w_router': nrm((L, D_MODEL, N_EXPERTS), D_MODEL ** -0.5),
        'router_bias': nrm((L, N_EXPERTS), 0.01),
        'w_gate': nrm((L, N_EXPERTS, D_MODEL, D_EXPERT), D_MODEL ** -0.5),
        'w_up': nrm((L, N_EXPERTS, D_MODEL, D_EXPERT), D_MODEL ** -0.5),
        'w_down': nrm((L, N_EXPERTS, D_EXPERT, D_MODEL), D_EXPERT ** -0.5),
        'ws_gate': nrm((L, D_MODEL, D_SHARED), D_MODEL ** -0.5),
        'ws_up': nrm((L, D_MODEL, D_SHARED), D_MODEL ** -0.5),
        'ws_down': nrm((L, D_SHARED, D_MODEL), D_SHARED ** -0.5),
    }


def reference(x, c, positions, w_mod, b_mod, norm1, w_in, conv_w, conv_b, w_a, b_a, w_i, b_i,
              lru_lambda, q_a_norm, kv_a_norm, w_uq, w_ukv, q_norm, k_norm, w_rnn_out, w_mla_out,
              w_out, norm2, w_router, router_bias, w_gate, w_up, w_down, ws_gate, ws_up, ws_down):
    split_at = [int(s) for s in np.cumsum(IN_WIDTHS)[:-1]]
    for l in range(DEPTH):
        mod = jax.nn.silu(c) @ w_mod[l] + b_mod[l]
        sh1, sc1, g1, sh2, sc2, g2 = jnp.split(mod, 6, axis=-1)
        u = modulate(x, norm1[l], sh1, sc1)
        proj = u @ w_in[l]
        xr, yg, cq, ckv, kr, gates = jnp.split(proj, split_at, axis=-1)
        y_rnn = rglru_branch(xr, yg, conv_w[l], conv_b[l], w_a[l], b_a[l], w_i[l], b_i[l], lru_lambda[l])
        y_mla = mla_branch(cq, ckv, kr, positions, q_a_norm[l], kv_a_norm[l], w_uq[l], w_ukv[l],
                           q_norm[l], k_norm[l])
        g_rnn, g_mla = jnp.split(jax.nn.sigmoid(gates), N_BRANCH, axis=-1)
        merged = g_rnn * (y_rnn @ w_rnn_out[l]) + g_mla * (y_mla @ w_mla_out[l])
        x = x + g1[:, None, :] * (merged @ w_out[l])
        v = modulate(x, norm2[l], sh2, sc2)
        ffn = moe(v, w_router[l], router_bias[l], w_gate[l], w_up[l], w_down[l],
                  ws_gate[l], ws_up[l], ws_down[l])
        x = x + g2[:, None, :] * ffn
    return x
```

```python
import math
import types
import numpy as np
import ml_dtypes
import concourse.bass as bass
import concourse.mybir as mybir
from concourse.bass_utils import run_bass_kernel_spmd
from contextlib import ExitStack

F32 = mybir.dt.float32
BF16 = mybir.dt.bfloat16
I32 = mybir.dt.int32
U32 = mybir.dt.uint32
AF = mybir.ActivationFunctionType
ALU = mybir.AluOpType
AX = mybir.AxisListType

ENGS = ("pe", "act", "dve", "pool", "sp")
D = 2048
NKC = 16
EPS = 1e-6
NE = 64
DEXP = 1408
NFC = 11
TOPK = 6
PI = math.pi


def _freeze(fn):
    if fn is None or fn.__closure__ is None:
        return fn
    cells = []
    for c in fn.__closure__:
        try:
            cells.append(types.CellType(c.cell_contents))
        except ValueError:
            cells.append(c)
    return types.FunctionType(fn.__code__, fn.__globals__, fn.__name__, fn.__defaults__, tuple(cells))


class Prog:
    def __init__(self, nc, stack):
        self.nc = nc
        self.stack = stack
        self.q = {e: [] for e in ENGS}
        self.cnt = {e: 0 for e in ENGS}
        self.waited = {e: {} for e in ENGS}
        self.res = {}
        self.dsems = {}
        self.semobj = {}
        self.gen = {e: 0 for e in ENGS}
        self.dfree = []
        self.nds = 0
        self.epoch = 0
        for e in ENGS:
            self.semobj[("e", e, 0)] = stack.enter_context(nc.semaphore("s_" + e))

    def _need(self, eng, tok, waits):
        if tok is None:
            return
        sk, val, src = tok
        if src == "pe" and eng == "pe":
            return
        if self.waited[eng].get(sk, 0) >= val:
            return
        self.waited[eng][sk] = val
        waits.append((sk, val))

    def _deps(self, eng, reads, writes):
        waits = []
        for r in reads:
            st = self.res.get(r)
            if st:
                self._need(eng, st[0], waits)
        for w in writes:
            st = self.res.get(w)
            if st:
                self._need(eng, st[0], waits)
                for t in st[1]:
                    self._need(eng, t, waits)
        return waits

    def _commit(self, tok, reads, writes):
        for r in reads:
            st = self.res.setdefault(r, [None, []])
            st[1].append(tok)
        for w in writes:
            self.res[w] = [tok, []]

    def op(self, eng, fn, reads=(), writes=()):
        waits = self._deps(eng, reads, writes)
        self.cnt[eng] += 1
        sk = ("e", eng, self.gen[eng])
        tok = (sk, self.cnt[eng], eng)
        self.q[eng].append((waits, _freeze(fn), sk, 1))
        self._commit(tok, reads, writes)
        return tok

    def _dsem(self, semkey):
        if semkey not in self.dsems:
            if self.dfree:
                ent = self.dfree.pop()
            else:
                self.nds += 1
                ent = [self.stack.enter_context(self.nc.semaphore("d%d" % self.nds)), 0]
            self.dsems[semkey] = ent
            self.semobj[semkey] = ent[0]
        self.dsems[semkey][1] += 16
        return self.dsems[semkey][1]

    def raw(self, queue, fn, reads=(), writes=(), semkey=None):
        waits = self._deps(queue, reads, writes)
        semkey = ("d", semkey, self.epoch)
        val = self._dsem(semkey)
        tok = (semkey, val, None)
        self.q[queue].append((waits, _freeze(fn), semkey, 16))
        self._commit(tok, reads, writes)
        return tok

    def dma(self, queue, out, in_, reads=(), writes=(), semkey=None, **kw):
        if semkey is None:
            semkey = writes[0] if writes else reads[0]

        def fn(e, out=out, in_=in_, kw=kw):
            return e.dma_start(out=out, in_=in_, **kw)
        return self.raw(queue, fn, reads, writes, semkey)

    def barrier(self):
        toks = [(("e", e, self.gen[e]), self.cnt[e], e) for e in ENGS if self.cnt[e] > 0]
        toks += [(k, v[1], None) for k, v in self.dsems.items() if v[1] > 0]
        for eng in ENGS:
            waits = []
            for sk, val, src in toks:
                if src == eng:
                    continue
                if self.waited[eng].get(sk, 0) >= val:
                    continue
                self.waited[eng][sk] = val
                waits.append((sk, val))
            if waits:
                self.q[eng].append((waits, None, None, 0))
        self.res = {}
        self.dfree.extend(self.dsems.values())
        self.dsems = {}
        self.epoch += 1
        for e in ENGS:
            if self.cnt[e] > 30000:
                self.gen[e] += 1
                self.cnt[e] = 0
                self.semobj[("e", e, self.gen[e])] = self.stack.enter_context(
                    self.nc.semaphore("s_%s_%d" % (e, self.gen[e])))

    def emit(self):
        nc = self.nc
        with nc.Block() as block:
            def run(engname, e):
                for waits, fn, sk, inc in self.q[engname]:
                    for wk, wv in waits:
                        e.wait_ge(self.semobj[wk], wv)
                    if fn is not None:
                        fn(e).then_inc(self.semobj[sk], inc)

            @block.tensor
            def _(e):
                run("pe", e)

            @block.scalar
            def _(e):
                run("act", e)

            @block.vector
            def _(e):
                run("dve", e)

            @block.gpsimd
            def _(e):
                run("pool", e)

            @block.sync
            def _(e):
                run("sp", e)


class Arena:
    def __init__(self, ap, nelem):
        self.ap = ap
        self.n = nelem
        self.off = 0
        self.base = 0

    def alloc(self, n, dt=BF16, parts=128):
        mul = 2 if dt in (F32, I32, U32) else 1
        sz = n * mul
        sz = (sz + 15) // 16 * 16
        assert self.off + sz <= self.n, "arena overflow %d + %d > %d" % (self.off, sz, self.n)
        a = self.ap[:, self.off:self.off + n * mul]
        self.off += sz
        if dt != BF16:
            a = a.bitcast(dt)
        if parts != 128:
            a = a[0:parts, :]
        return a

    def mark(self):
        self.base = self.off

    def reset(self):
        self.off = self.base


def build(S, C, dbg=()):
    T = S // 2
    NTA = S // 512
    NTO = T // 512
    assert T % C == 0 and C % 512 == 0
    NPS = T // C
    NX = NE + NPS
    NROW = NE * C
    nc = bass.Bass("TRN2", target_bir_lowering=False)

    declared = []

    def din(name, shape, dt=F32):
        declared.append(name)
        return nc.dram_tensor(name, list(shape), dt, kind="ExternalInput").ap()

    def dscr(name, shape, dt=BF16):
        kind = "ExternalOutput" if name in dbg else "Internal"
        return nc.dram_tensor(name, list(shape), dt, kind=kind).ap()

    pos_all = din("pos_all", [64, S], I32); pos_own = din("pos_own", [64, T], I32)
    c_col = din("c_col", [128, 16])
    cpar = din("cpar", [128, 2])
    masks_d = din("masks", [128, 256], BF16)
    ident32_d = din("ident32", [128, 128]); identb_d = din("identb", [128, 128], BF16)
    onesb_d = din("onesb", [128, 128], BF16); ustr_d = din("ustrict", [128, 128], BF16)
    rot_d = din("rot", [64, 64], BF16); iota_d = din("iota_row", [128, 64])
    invf_d = din("invf", [64, 1])
    bmod_d = din("bmod_col", [128, 96])
    vec16_d = din("vec16", [128, 7 * 16])
    convw_d = din("convw_col", [128, 64])
    qan_d = din("qan_col", [128, 4]); kvan_d = din("kvan_col", [128, 2])
    qkn_d = din("qkn_col", [128, 4])
    rbias_d = din("rbias_row", [128, 64])
    BIG = dict(x_all=[S, D], x_own=[T, D], w_mod=[D, 6 * D], w_in=[D, 9024], w_a=[16, 128, 128], w_i=[16, 128, 128],
               w_uq=[512, 3072], w_ukv=[256, 4096], w_rnn_out=[D, D], w_mla_out=[D, D], w_out=[D, D], w_router=[D, NE],
               ws_gate=[D, DEXP], ws_up=[D, DEXP],
               ws_down=[DEXP, D])

    for e_ in range(NE // 4):
        BIG["wg%02d" % e_] = [4, D, DEXP]; BIG["wu%02d" % e_] = [4, D, DEXP]; BIG["wd%02d" % e_] = [4, DEXP, D]

    class _Lazy:
        def __init__(self):
            self.c = {}

        def __getattr__(self, name):
            c = self.__dict__["c"]
            if name not in c:
                c[name] = din(name, BIG[name])
            return c[name]
    I = _Lazy()
    out_d = nc.dram_tensor("out", [T, D], F32, kind="ExternalOutput").ap()

    UTA = dscr("UTA", [D, S]); UTO = dscr("UTO", [D, T])
    HT = dscr("HT", [D, T]); KT = dscr("KT", [16, 192, S]); VTK = dscr("VTK", [S, D]); QT = dscr("QT", [16, 192, T])
    YMT = dscr("YMT", [D, T]); YRT = dscr("YRT", [D, T]); GAT = dscr("GAT", [D, T]); GBT = dscr("GBT", [D, T])
    MA = dscr("MA", [D, T]); MT = dscr("MT", [D, T])
    X1 = dscr("X1", [T, D], F32)
    XG = dscr("XG", [NROW, D]); YG = dscr("YG", [NROW, D])
    XS = dscr("XS", [T, D]); YS = dscr("YS", [T, D])
    MODC = dscr("MODC", [128, 96], F32)

    with ExitStack() as st:
        ARN = 103424
        arena_t = st.enter_context(nc.sbuf_tensor("arena", [128, ARN], BF16))
        A = Arena(arena_t, ARN)
        ps = [st.enter_context(nc.psum_tensor("ps%d" % i, [128, 512], F32)) for i in range(8)]
        P = Prog(nc, st)
        ldq = ["sp"]

        def PS(i):
            return ("ps", i)

        ident32 = A.alloc(128, F32); identb = A.alloc(128); onesb = A.alloc(128); ustr = A.alloc(128)
        rot = A.alloc(64, parts=64); masks = A.alloc(256); iota = A.alloc(64, F32); rbias = A.alloc(64, F32)
        invf = A.alloc(1, F32, parts=64); cp = A.alloc(2, F32)
        bmod = A.alloc(96, F32); vec16 = A.alloc(112, F32); convw = A.alloc(64, F32)
        qan = A.alloc(4, F32); kvan = A.alloc(2, F32); qkn = A.alloc(4, F32)
        modc = A.alloc(96, F32); A1 = A.alloc(16, F32); A2 = A.alloc(16, F32); nsp8 = A.alloc(16, F32)
        gqs = A.alloc(2, F32); ccol = A.alloc(16, F32); scs = A.alloc(16, F32)
        negpi = A.alloc(1, F32); tmpc = A.alloc(64, F32)
        for dst, src in ((ident32, ident32_d), (identb, identb_d), (onesb, onesb_d), (ustr, ustr_d), (rot, rot_d),
                         (masks, masks_d), (iota, iota_d), (rbias, rbias_d), (invf, invf_d), (cp, cpar),
                         (bmod, bmod_d), (vec16, vec16_d), (convw, convw_d), (qan, qan_d), (kvan, kvan_d),
                         (qkn, qkn_d), (ccol, c_col)):
            P.dma("sp", dst, src, writes=["const"], semkey="const")
        norm1 = vec16[:, 0:16]; norm2 = vec16[:, 16:32]; convb = vec16[:, 32:48]
        b_a = vec16[:, 48:64]; b_i = vec16[:, 64:80]; lam = vec16[:, 80:96]
        cvec = cp[:, 0:1]; omc = cp[:, 1:2]
        A.mark()

        def phase0():
            P.op("act", lambda e: e.activation(scs, ccol, AF.Silu), reads=["const"], writes=["scs"])
            P.op("pool", lambda e: e.memset(negpi, -PI), writes=["negpi"])
            wm = [A.alloc(16 * 768, F32).rearrange("p (k c) -> p k c", c=768) for _ in range(2)]
            wsrc = I.w_mod.rearrange("(k p) c -> p k c", p=128)
            for blk in range(16):
                b = wm[blk % 2]
                for k0 in range(0, 16, 4):
                    P.dma("sp", b[:, k0:k0 + 4, :], wsrc[:, k0:k0 + 4, blk * 768:(blk + 1) * 768],
                          writes=[("wm", blk % 2)], semkey=("wm", blk % 2))
                for f in range(6):
                    fa = blk * 6 + f
                    for kc in range(16):
                        P.op("pe", lambda e, b=b, f=f, fa=fa, kc=kc: e.matmul(
                            ps[0][:, fa:fa + 1], b[:, kc, f * 128:(f + 1) * 128], scs[:, kc:kc + 1],
                            start=(kc == 0), stop=(kc == 15)),
                            reads=[("wm", blk % 2), "scs"], writes=[PS(0)])
            P.op("dve", lambda e: e.tensor_tensor(modc, ps[0][:, 0:96], bmod, ALU.add),
                 reads=[PS(0), "const"], writes=["modc"])
            P.op("dve", lambda e: e.scalar_tensor_tensor(A1, modc[:, 16:32], 1.0, norm1, ALU.add, ALU.mult),
                 reads=["modc"], writes=["A1"])
            P.op("dve", lambda e: e.scalar_tensor_tensor(A2, modc[:, 64:80], 1.0, norm2, ALU.add, ALU.mult),
                 reads=["modc"], writes=["A2"])
            t0 = tmpc[:, 0:16]; t1 = tmpc[:, 16:32]; t2 = tmpc[:, 32:48]; t3 = tmpc[:, 48:64]
            P.op("dve", lambda e: e.tensor_scalar(t0, lam, -1.0, None, ALU.mult), reads=["const"], writes=["t0"])
            P.op("dve", lambda e: e.tensor_tensor(t0, t0, lam, ALU.max), reads=["const", "t0"], writes=["t0"])
            P.op("act", lambda e: e.activation(t1, t0, AF.Exp, scale=-1.0), reads=["t0"], writes=["t1"])
            P.op("dve", lambda e: e.tensor_scalar(t2, t1, 2.0, None, ALU.add), reads=["t1"], writes=["t2"])
            P.op("dve", lambda e: e.reciprocal(t2, t2), reads=["t2"], writes=["t2"])
            P.op("dve", lambda e: e.tensor_tensor(t1, t1, t2, ALU.mult), reads=["t1", "t2"], writes=["t1"])
            P.op("dve", lambda e: e.tensor_tensor(t2, t1, t1, ALU.mult), reads=["t1"], writes=["t2"])
            P.op("dve", lambda e: e.tensor_scalar(t3, t2, 1.0 / 9, 1.0 / 7, ALU.mult, ALU.add), reads=["t2"], writes=["t3"])
            for cst in (1.0 / 5, 1.0 / 3, 1.0):
                P.op("dve", lambda e: e.tensor_tensor(t3, t3, t2, ALU.mult), reads=["t3", "t2"], writes=["t3"])
                P.op("dve", lambda e, cst=cst: e.tensor_scalar(t3, t3, cst, None, ALU.add), reads=["t3"], writes=["t3"])
            P.op("dve", lambda e: e.tensor_tensor(t3, t3, t1, ALU.mult), reads=["t3", "t1"], writes=["t3"])
            P.op("dve", lambda e: e.tensor_scalar(t0, lam, -1.0, 0.0, ALU.mult, ALU.max), reads=["const"], writes=["t0"])
            P.op("dve", lambda e: e.scalar_tensor_tensor(t3, t3, 2.0, t0, ALU.mult, ALU.add), reads=["t3", "t0"], writes=["t3"])
            P.op("dve", lambda e: e.tensor_scalar(nsp8, t3, -8.0, None, ALU.mult), reads=["t3"], writes=["nsp8"])
            P.op("dve", lambda e: e.tensor_scalar(gqs, qkn[:, 0:2], 192.0 ** -0.5, None, ALU.mult),
                 reads=["const"], writes=["gqs"])
            P.dma("sp", MODC, modc, reads=["modc"], semkey="modc_out")
            P.barrier()
            A.reset()

        def norm_T(x_src, ntok, dst, Acol, shcol):
            xt = [A.alloc(D, F32) for _ in range(2)]
            junk = A.alloc(D)
            ss = A.alloc(4, F32)
            ut = [A.alloc(16 * 512).rearrange("p (c t) -> p c t", t=512) for _ in range(2)]
            n = 0
            for g in range(ntok // 512):
                u = ut[g % 2]
                for sub in range(4):
                    b = xt[n % 2]; xk = ("xt", n % 2)
                    r0 = g * 512 + sub * 128
                    P.dma("sp", b, x_src[r0:r0 + 128, :], writes=[xk])
                    sc = ss[:, 0:1]; sc2 = ss[:, 1:2]
                    P.op("act", lambda e, b=b, sc=sc: e.activation(junk, b, AF.Square, accum_out=sc),
                         reads=[xk], writes=["junk", "ss"])
                    P.op("act", lambda e, sc=sc, sc2=sc2: e.activation(sc2, sc, AF.Sqrt, scale=1.0 / D, bias=EPS),
                         reads=["ss"], writes=["ss2"])
                    P.op("dve", lambda e, sc2=sc2: e.reciprocal(sc2, sc2), reads=["ss2"], writes=["ss2"])
                    P.op("pool", lambda e, b=b, sc2=sc2: e.tensor_scalar(b, b, sc2, 1.0, ALU.mult, ALU.mult),
                         reads=[xk, "ss2"], writes=[xk])
                    for q4 in range(4):
                        pi = (n * 4 + q4) % 4
                        for j in range(4):
                            fc = q4 * 4 + j
                            P.op("pe", lambda e, b=b, fc=fc, j=j, pi=pi: e.transpose(
                                ps[pi][:, j * 128:(j + 1) * 128], b[:, fc * 128:(fc + 1) * 128], ident32),
                                reads=[xk, "const"], writes=[PS(pi)])
                        for j in range(4):
                            fc = q4 * 4 + j
                            P.op("act", lambda e, u=u, fc=fc, j=j, pi=pi, sub=sub: e.activation(
                                u[:, fc, sub * 128:(sub + 1) * 128], ps[pi][:, j * 128:(j + 1) * 128], AF.Identity,
                                bias=shcol[:, fc:fc + 1], scale=Acol[:, fc:fc + 1]),
                                reads=[PS(pi), "modc", "A1", "A2"], writes=[("ut", g % 2)])
                    n += 1
                P.dma("act", dst.rearrange("(c p) t -> p c t", p=128)[:, :, g * 512:(g + 1) * 512], u,
                      reads=[("ut", g % 2)], semkey=("uts", g % 2))
            P.barrier()
            A.reset()

        wpar = [0]

        def load_w(wsrc, ncols, nk=NKC):
            wt = A.alloc(nk * ncols).rearrange("p (k c) -> p k c", c=ncols)
            key = ("w", wpar[0]); wpar[0] += 1
            src = wsrc.rearrange("(k p) c -> p k c", p=128)
            cw_ = min(ncols, 1024)
            step = max(1, 4096 // cw_)
            for c0 in range(0, ncols, cw_):
                c1 = min(ncols, c0 + cw_)
                for k0 in range(0, nk, step):
                    k1 = min(nk, k0 + step)
                    P.dma("pool", wt[:, k0:k1, c0:c1], src[:, k0:k1, c0:c1], writes=[key], semkey=key)
            return wt, key

        def fm_pass(wt, wkey, ncols, in_scr, ntok, epi, psbanks, xin_bufs, nk=NKC, fc0=0):
            pend = None
            n = 0
            for tt in range(ntok // 512):
                xb = xin_bufs[tt % 2]; xk = ("xin", tt % 2)
                P.dma("sp", xb[:, 0:nk, :], in_scr.rearrange("(k p) t -> p k t", p=128)[:, :, tt * 512:(tt + 1) * 512],
                      writes=[xk])
                for fc in range(ncols // 128):
                    pi = psbanks[n % len(psbanks)]; n += 1
                    for k in range(nk):
                        P.op("pe", lambda e, pi=pi, k=k, fc=fc, xb=xb: e.matmul(
                            ps[pi][:], wt[:, k, fc * 128:(fc + 1) * 128], xb[:, k, :], start=(k == 0), stop=(k == nk - 1)),
                            reads=[wkey, xk], writes=[PS(pi)])
                    if pend is not None:
                        pend()
                    pend = epi(fc0 + fc, tt, ps[pi], PS(pi))
            if pend is not None:
                pend()

        def phase_rnn():
            xin = [A.alloc(16 * 512).rearrange("p (k t) -> p k t", t=512) for _ in range(2)]
            wab = A.alloc(16 * 128).rearrange("p (h j) -> p h j", j=128)
            wib = A.alloc(16 * 128).rearrange("p (h j) -> p h j", j=128)
            P.dma("pool", wab, I.w_a.rearrange("h i j -> i h j"), writes=["wab"])
            P.dma("pool", wib, I.w_i.rearrange("h i j -> i h j"), writes=["wib"])
            xr = A.alloc(8 * 516, F32).rearrange("p (c t) -> p c t", t=516)
            carry = A.alloc(8, F32)
            xc = [A.alloc(512, F32) for _ in range(2)]
            xcb = [A.alloc(512) for _ in range(2)]
            rr = [A.alloc(512, F32) for _ in range(2)]
            ii = [A.alloc(512, F32) for _ in range(2)]
            aa = [A.alloc(512, F32) for _ in range(2)]
            bb = [A.alloc(512, F32) for _ in range(2)]
            hh = [A.alloc(512, F32) for _ in range(2)]
            tq = [A.alloc(256, F32) for _ in range(2)]
            hout = [A.alloc(8 * 256).rearrange("p (c t) -> p c t", t=256) for _ in range(2)]
            for half in range(2):
                wt, wkey = load_w(I.w_in[:, half * 1024:(half + 1) * 1024], 1024)
                P.op("pool", lambda e: e.memset(xr, 0.0), writes=["xr"] + [("xr", c) for c in range(8)])
                P.op("pool", lambda e: e.memset(carry, 0.0), writes=[("carry", c) for c in range(8)])
                cnt = [0]

                def epi(fc, tt, pst, pk, half=half, cnt=cnt):
                    gfc = half * 8 + fc
                    i = cnt[0] % 2; cnt[0] += 1
                    X = xr[:, fc, :]
                    P.op("act", lambda e: e.copy(X[:, 4:516], pst[:]), reads=[pk], writes=[("xr", fc)])
                    c0 = xc[i]; k0 = ("xc", i)
                    P.op("dve", lambda e: e.tensor_scalar(c0, X[:, 1:513], convw[:, gfc:gfc + 1], convb[:, gfc:gfc + 1],
                                                          ALU.mult, ALU.add), reads=[("xr", fc), "const"], writes=[k0])
                    for j in range(1, 4):
                        P.op("dve", lambda e, j=j: e.scalar_tensor_tensor(
                            c0, X[:, 1 + j:513 + j], convw[:, j * 16 + gfc:j * 16 + gfc + 1], c0, ALU.mult, ALU.add),
                            reads=[("xr", fc), k0], writes=[k0])
                    P.op("pool", lambda e: e.tensor_copy(X[:, 0:4], X[:, 512:516]), reads=[("xr", fc)], writes=[("xr", fc)])
                    P.op("act", lambda e: e.copy(xcb[i], c0), reads=[k0], writes=[("xcb", i)])

                    def stage2():
                        pr = 4 + i * 2; pq = 5 + i * 2
                        P.op("pe", lambda e: e.matmul(ps[pr][:], wab[:, gfc, :], xcb[i], start=True, stop=True),
                             reads=["wab", ("xcb", i)], writes=[PS(pr)])
                        P.op("pe", lambda e: e.matmul(ps[pq][:], wib[:, gfc, :], xcb[i], start=True, stop=True),
                             reads=["wib", ("xcb", i)], writes=[PS(pq)])
                        P.op("act", lambda e: e.activation(rr[i], ps[pr][:], AF.Sigmoid, bias=b_a[:, gfc:gfc + 1]),
                             reads=[PS(pr), "const"], writes=[("rr", i)])
                        P.op("act", lambda e: e.activation(ii[i], ps[pq][:], AF.Sigmoid, bias=b_i[:, gfc:gfc + 1]),
                             reads=[PS(pq), "const"], writes=[("ii", i)])
                        P.op("act", lambda e: e.activation(aa[i], rr[i], AF.Exp, scale=nsp8[:, gfc:gfc + 1]),
                             reads=[("rr", i), "nsp8"], writes=[("aa", i)])
                        P.op("pool", lambda e: e.tensor_tensor(rr[i], aa[i], aa[i], ALU.mult),
                             reads=[("aa", i), ("rr", i)], writes=[("rr", i)])
                        P.op("act", lambda e: e.activation(rr[i], rr[i], AF.Sqrt, scale=-1.0, bias=1.0),
                             reads=[("rr", i)], writes=[("rr", i)])
                        P.op("pool", lambda e: e.tensor_tensor(ii[i], ii[i], c0, ALU.mult),
                             reads=[("ii", i), k0], writes=[("ii", i)])
                        P.op("dve", lambda e: e.tensor_tensor(bb[i], rr[i], ii[i], ALU.mult),
                             reads=[("rr", i), ("ii", i)], writes=[("bb", i)])
                        P.op("dve", lambda e: e.tensor_tensor_scan(hh[i], aa[i], bb[i], carry[:, fc:fc + 1], ALU.mult, ALU.add),
                             reads=[("aa", i), ("bb", i), ("carry", fc)], writes=[("hh", i)])
                        P.op("pool", lambda e: e.tensor_copy(carry[:, fc:fc + 1], hh[i][:, 511:512]),
                             reads=[("hh", i)], writes=[("carry", fc)])
                        h4 = hh[i].rearrange("p (j two q) -> p j two q", two=2, q=128)
                        t4 = tq[i].rearrange("p (j q) -> p j q", q=128)
                        ho = hout[tt % 2]
                        P.op("dve", lambda e: e.tensor_scalar(t4, h4[:, :, 0, :], omc, None, ALU.mult),
                             reads=[("hh", i), "const"], writes=[("tq", i)])
                        P.op("dve", lambda e: e.scalar_tensor_tensor(
                            ho[:, fc, :].rearrange("p (j q) -> p j q", q=128), h4[:, :, 1, :], cvec, t4, ALU.mult, ALU.add),
                            reads=[("hh", i), ("tq", i), "const"], writes=[("hout", tt % 2)])
                        if fc == 7:
                            P.dma("act", HT.rearrange("(c p) t -> p c t", p=128)[:, half * 8:(half + 1) * 8, tt * 256:(tt + 1) * 256],
                                  ho, reads=[("hout", tt % 2)], semkey=("houts", tt % 2))
                    return stage2
                fm_pass(wt, wkey, 1024, UTA, S, epi, [0, 1, 2, 3], xin)
            P.barrier()
            A.reset()

        def rope_tables(pos_src, t0, posi, ang, kf, cs, sn, tag):
            P.dma("sp", posi, pos_src[:, t0:t0 + 512], writes=[tag + "posi"])
            P.op("dve", lambda e: e.tensor_copy(ang, posi), reads=[tag + "posi"], writes=[tag + "ang"])
            P.op("dve", lambda e: e.tensor_scalar(ang, ang, invf, None, ALU.mult), reads=[tag + "ang", "const"], writes=[tag + "ang"])
            ki = posi
            P.op("dve", lambda e: e.tensor_scalar(kf, ang, 1.0 / (2 * PI), None, ALU.mult), reads=[tag + "ang"], writes=[tag + "kf"])
            P.op("dve", lambda e: e.tensor_copy(ki, kf), reads=[tag + "kf"], writes=[tag + "posi"])
            P.op("dve", lambda e: e.tensor_copy(kf, ki), reads=[tag + "posi"], writes=[tag + "kf"])
            P.op("dve", lambda e: e.scalar_tensor_tensor(ang, kf, -2 * PI, ang, ALU.mult, ALU.add),
                 reads=[tag + "kf", tag + "ang"], writes=[tag + "ang"])
            for dst, shift in ((sn, 0.0), (cs, PI / 2)):
                P.op("dve", lambda e, shift=shift: e.tensor_scalar(kf, ang, shift, None, ALU.add),
                     reads=[tag + "ang", tag + "kf"], writes=[tag + "kf"])
                for _ in range(2):
                    P.op("dve", lambda e, dst=dst: e.tensor_scalar(dst, kf, PI, -2 * PI, ALU.is_gt, ALU.mult),
                         reads=[tag + "kf"], writes=[tag + "cs"])
                    P.op("dve", lambda e, dst=dst: e.tensor_tensor(kf, kf, dst, ALU.add),
                         reads=[tag + "kf", tag + "cs"], writes=[tag + "kf"])
                P.op("dve", lambda e, dst=dst: e.tensor_scalar(dst, kf, -PI, 2 * PI, ALU.is_lt, ALU.mult),
                     reads=[tag + "kf"], writes=[tag + "cs"])
                P.op("dve", lambda e, dst=dst: e.tensor_tensor(kf, kf, dst, ALU.add),
                     reads=[tag + "kf", tag + "cs"], writes=[tag + "kf"])
                P.op("act", lambda e, dst=dst: e.activation(dst, kf, AF.Sin), reads=[tag + "kf"], writes=[tag + "cs"])

        def rstd_bcast(dst, pst, pk, n, tagw):
            P.op("act", lambda e: e.activation(dst, pst[:], AF.Sqrt, scale=1.0 / n, bias=EPS), reads=[pk], writes=[tagw])
            P.op("dve", lambda e: e.reciprocal(dst, dst), reads=[tagw], writes=[tagw])

        def phase_kv():
            xin = [A.alloc(16 * 512).rearrange("p (k t) -> p k t", t=512) for _ in range(2)]
            wkv, wkvk = load_w(I.w_in[:, 4608:4928], 320)
            wuk = A.alloc(2 * 2048).rearrange("p (c h d) -> p c h d", c=2, d=128)
            wuv = A.alloc(2 * 2048).rearrange("p (c h d) -> p c h d", c=2, d=128)
            src = I.w_ukv.rearrange("(c p) (h two d) -> p c h two d", p=128, two=2, d=128)
            for c in range(2):
                P.dma("pool", wuk[:, c, :, :], src[:, c, :, 0, :], writes=["wuk"])
                P.dma("pool", wuv[:, c, :, :], src[:, c, :, 1, :], writes=["wuv"])
            ckv32 = A.alloc(2 * 512, F32).rearrange("p (c t) -> p c t", t=512)
            sqb = A.alloc(2 * 512).rearrange("p (c t) -> p c t", t=512)
            ckvn = A.alloc(2 * 512).rearrange("p (c t) -> p c t", t=512)
            rs = A.alloc(512, F32)
            kr32 = A.alloc(512, F32, parts=64); krsq = A.alloc(512, parts=64); krg32 = A.alloc(512, F32, parts=64)
            krgb = A.alloc(512, parts=64); kro = A.alloc(512, F32, parts=64); tmp64 = A.alloc(512, F32, parts=64)
            posi = A.alloc(512, I32, parts=64); ang = A.alloc(512, F32, parts=64); kf = A.alloc(512, F32, parts=64)
            cs = A.alloc(512, F32, parts=64); sn = A.alloc(512, F32, parts=64)
            kn32 = [A.alloc(512, F32) for _ in range(2)]
            knsq = [A.alloc(512) for _ in range(2)]
            rsh = [A.alloc(512, F32) for _ in range(2)]
            kon = [A.alloc(16 * 512).rearrange("p (h t) -> p h t", t=512) for _ in range(2)]
            kor = [A.alloc(16 * 512, parts=64).rearrange("p (h t) -> p h t", t=512) for _ in range(2)]
            vt = [A.alloc(D) for _ in range(2)]
            xT = UTA.rearrange("(k p) t -> p k t", p=128)
            nv = 0
            for tt in range(NTA):
                xb = xin[tt % 2]; xk = ("xin", tt % 2)
                P.dma("sp", xb, xT[:, :, tt * 512:(tt + 1) * 512], writes=[xk])
                rope_tables(pos_all, tt * 512, posi, ang, kf, cs, sn, "k")
                for fc in range(2):
                    for k in range(16):
                        P.op("pe", lambda e, fc=fc, k=k, xb=xb: e.matmul(ps[fc][:], wkv[:, k, fc * 128:(fc + 1) * 128], xb[:, k, :],
                                                                         start=(k == 0), stop=(k == 15)),
                             reads=[wkvk, xk], writes=[PS(fc)])
                    P.op("act", lambda e, fc=fc: e.copy(ckv32[:, fc, :], ps[fc][:]), reads=[PS(fc)], writes=[("ckv32", fc)])
                    P.op("act", lambda e, fc=fc: e.activation(sqb[:, fc, :], ps[fc][:], AF.Square), reads=[PS(fc)], writes=[("sqb", fc)])
                for k in range(16):
                    P.op("pe", lambda e, k=k, xb=xb: e.matmul(ps[2][0:64, :], wkv[:, k, 256:320], xb[:, k, :], start=(k == 0), stop=(k == 15)),
                         reads=[wkvk, xk], writes=[PS(2)])
                P.op("act", lambda e: e.copy(kr32, ps[2][0:64, :]), reads=[PS(2)], writes=["kr32"])
                P.op("act", lambda e: e.activation(krsq, ps[2][0:64, :], AF.Square), reads=[PS(2)], writes=["krsq"])
                for fc in range(2):
                    P.op("pe", lambda e, fc=fc: e.matmul(ps[3][:], onesb, sqb[:, fc, :], start=(fc == 0), stop=(fc == 1)),
                         reads=["const", ("sqb", fc)], writes=[PS(3)])
                rstd_bcast(rs, ps[3], PS(3), 256, "rs")
                for fc in range(2):
                    P.op("dve", lambda e, fc=fc: e.scalar_tensor_tensor(ckvn[:, fc, :], ckv32[:, fc, :], kvan[:, fc:fc + 1], rs, ALU.mult, ALU.mult),
                         reads=[("ckv32", fc), "rs", "const"], writes=[("ckvn", fc)])
                P.op("dve", lambda e: e.tensor_scalar(krg32, kr32, qkn[0:64, 3:4], None, ALU.mult), reads=["kr32", "const"], writes=["krg32"])
                P.op("act", lambda e: e.copy(krgb, krg32), reads=["krg32"], writes=["krgb"])
                P.op("pe", lambda e: e.matmul(ps[2][0:64, :], rot, krgb, start=True, stop=True), reads=["const", "krgb"], writes=[PS(2)])
                P.op("dve", lambda e: e.tensor_tensor(kro, krg32, cs, ALU.mult), reads=["krg32", "kcs"], writes=["kro"])
                P.op("dve", lambda e: e.tensor_tensor(tmp64, ps[2][0:64, :], sn, ALU.mult), reads=[PS(2), "kcs"], writes=["tmp64"])
                P.op("dve", lambda e: e.tensor_tensor(kro, kro, tmp64, ALU.add), reads=["kro", "tmp64"], writes=["kro"])
                KN = kon[tt % 2]; KR = kor[tt % 2]
                for h in range(16):
                    i = h % 2
                    pa = 4 + i; pb = 6 + i
                    for c in range(2):
                        P.op("pe", lambda e, h=h, c=c, pa=pa: e.matmul(ps[pa][:], wuk[:, c, h, :], ckvn[:, c, :], start=(c == 0), stop=(c == 1)),
                             reads=["wuk", ("ckvn", 0), ("ckvn", 1)], writes=[PS(pa)])
                    P.op("act", lambda e, i=i, pa=pa: e.copy(kn32[i], ps[pa][:]), reads=[PS(pa)], writes=[("kn32", i)])
                    P.op("act", lambda e, i=i, pa=pa: e.activation(knsq[i], ps[pa][:], AF.Square), reads=[PS(pa)], writes=[("knsq", i)])
                    P.op("pe", lambda e, i=i, pb=pb: e.matmul(ps[pb][:], onesb, knsq[i], start=True, stop=False),
                         reads=["const", ("knsq", i)], writes=[PS(pb)])
                    P.op("pe", lambda e, pb=pb: e.matmul(ps[pb][:], onesb[0:64, :], krsq, start=False, stop=True),
                         reads=["const", "krsq"], writes=[PS(pb)])
                    rstd_bcast(rsh[i], ps[pb], PS(pb), 192, ("rsh", i))
                    P.op("dve", lambda e, h=h, i=i: e.scalar_tensor_tensor(KN[:, h, :], kn32[i], qkn[:, 2:3], rsh[i], ALU.mult, ALU.mult),
                         reads=[("kn32", i), ("rsh", i), "const"], writes=[("kon", tt % 2)])
                    P.op("pool", lambda e, h=h, i=i: e.tensor_tensor(KR[:, h, :], kro, rsh[i][0:64, :], ALU.mult),
                         reads=["kro", ("rsh", i)], writes=[("kor", tt % 2)])
                P.dma("act", KT.rearrange("h p t -> p h t")[0:128, :, tt * 512:(tt + 1) * 512], KN, reads=[("kon", tt % 2)],
                      semkey=("kons", tt % 2))
                P.dma("act", KT.rearrange("h p t -> p h t")[128:192, :, tt * 512:(tt + 1) * 512], KR, reads=[("kor", tt % 2)],
                      semkey=("kors", tt % 2))
                for sub in range(4):
                    vb = vt[nv % 2]; vk = ("vt", nv % 2); nv += 1
                    for hc in range(4):
                        pi = hc
                        for c in range(2):
                            P.op("pe", lambda e, c=c, hc=hc, sub=sub, pi=pi: e.matmul(
                                ps[pi][:], ckvn[:, c, sub * 128:(sub + 1) * 128],
                                wuv[:, c, hc * 4:(hc + 1) * 4, :].rearrange("p h d -> p (h d)"), start=(c == 0), stop=(c == 1)),
                                reads=["wuv", ("ckvn", 0), ("ckvn", 1)], writes=[PS(pi)])
                        eng = "act" if hc % 2 == 0 else "dve"
                        if eng == "act":
                            P.op("act", lambda e, vb=vb, hc=hc, pi=pi: e.copy(vb[:, hc * 512:(hc + 1) * 512], ps[pi][:]),
                                 reads=[PS(pi)], writes=[vk])
                        else:
                            P.op("dve", lambda e, vb=vb, hc=hc, pi=pi: e.tensor_copy(vb[:, hc * 512:(hc + 1) * 512], ps[pi][:]),
                                 reads=[PS(pi)], writes=[vk])
                    r0 = tt * 512 + sub * 128
                    P.dma("act", VTK[r0:r0 + 128, :], vb, reads=[vk], semkey=("vts", (nv - 1) % 2))
            P.barrier()
            A.reset()

        def phase_q():
            xin = [A.alloc(16 * 512).rearrange("p (k t) -> p k t", t=512) for _ in range(2)]
            wq, wqk = load_w(I.w_in[:, 4096:4608], 512)
            wuq, wuqk = load_w(I.w_uq, 3072, nk=4)
            cq32 = A.alloc(4 * 512, F32).rearrange("p (c t) -> p c t", t=512)
            sqb = A.alloc(4 * 512).rearrange("p (c t) -> p c t", t=512)
            cqn = A.alloc(4 * 512).rearrange("p (c t) -> p c t", t=512)
            rs = A.alloc(512, F32)
            posi = A.alloc(512, I32, parts=64); ang = A.alloc(512, F32, parts=64); kf = A.alloc(512, F32, parts=64)
            cs = A.alloc(512, F32, parts=64); sn = A.alloc(512, F32, parts=64)
            qn32 = [A.alloc(512, F32) for _ in range(2)]
            qnsq = [A.alloc(512) for _ in range(2)]
            qr32 = [A.alloc(512, F32, parts=64) for _ in range(2)]
            qrsq = [A.alloc(512, parts=64) for _ in range(2)]
            qrgb = [A.alloc(512, parts=64) for _ in range(2)]
            qro = [A.alloc(512, F32, parts=64) for _ in range(2)]
            tmp64 = [A.alloc(512, F32, parts=64) for _ in range(2)]
            rsh = [A.alloc(512, F32) for _ in range(2)]
            qon = [A.alloc(16 * 512).rearrange("p (h t) -> p h t", t=512) for _ in range(2)]
            qor = [A.alloc(16 * 512, parts=64).rearrange("p (h t) -> p h t", t=512) for _ in range(2)]
            xT = UTO.rearrange("(k p) t -> p k t", p=128)
            for tt in range(NTO):
                xb = xin[tt % 2]; xk = ("xin", tt % 2)
                P.dma("sp", xb, xT[:, :, tt * 512:(tt + 1) * 512], writes=[xk])
                rope_tables(pos_own, tt * 512, posi, ang, kf, cs, sn, "q")
                for fc in range(4):
                    for k in range(16):
                        P.op("pe", lambda e, fc=fc, k=k, xb=xb: e.matmul(ps[fc][:], wq[:, k, fc * 128:(fc + 1) * 128], xb[:, k, :],
                                                                         start=(k == 0), stop=(k == 15)),
                             reads=[wqk, xk], writes=[PS(fc)])
                    P.op("act", lambda e, fc=fc: e.copy(cq32[:, fc, :], ps[fc][:]), reads=[PS(fc)], writes=[("cq32", fc)])
                    P.op("act", lambda e, fc=fc: e.activation(sqb[:, fc, :], ps[fc][:], AF.Square), reads=[PS(fc)], writes=[("sqb", fc)])
                for fc in range(4):
                    P.op("pe", lambda e, fc=fc: e.matmul(ps[4][:], onesb, sqb[:, fc, :], start=(fc == 0), stop=(fc == 3)),
                         reads=["const", ("sqb", fc)], writes=[PS(4)])
                rstd_bcast(rs, ps[4], PS(4), 512, "rs")
                for fc in range(4):
                    P.op("dve", lambda e, fc=fc: e.scalar_tensor_tensor(cqn[:, fc, :], cq32[:, fc, :], qan[:, fc:fc + 1], rs, ALU.mult, ALU.mult),
                         reads=[("cq32", fc), "rs", "const"], writes=["cqn"])
                QN = qon[tt % 2]; QR = qor[tt % 2]
                for h in range(16):
                    i = h % 2
                    pa = 0 + i; pb = 2 + i; pc = 4 + i; pd = 6 + i
                    for c in range(4):
                        P.op("pe", lambda e, h=h, c=c, pa=pa: e.matmul(ps[pa][:], wuq[:, c, h * 192:h * 192 + 128], cqn[:, c, :],
                                                                       start=(c == 0), stop=(c == 3)),
                             reads=[wuqk, "cqn"], writes=[PS(pa)])
                    for c in range(4):
                        P.op("pe", lambda e, h=h, c=c, pb=pb: e.matmul(ps[pb][0:64, :], wuq[:, c, h * 192 + 128:h * 192 + 192], cqn[:, c, :],
                                                                       start=(c == 0), stop=(c == 3)),
                             reads=[wuqk, "cqn"], writes=[PS(pb)])
                    P.op("act", lambda e, i=i, pa=pa: e.copy(qn32[i], ps[pa][:]), reads=[PS(pa)], writes=[("qn32", i)])
                    P.op("act", lambda e, i=i, pa=pa: e.activation(qnsq[i], ps[pa][:], AF.Square), reads=[PS(pa)], writes=[("qnsq", i)])
                    P.op("act", lambda e, i=i, pb=pb: e.activation(qr32[i], ps[pb][0:64, :], AF.Identity, scale=gqs[0:64, 1:2]),
                         reads=[PS(pb), "gqs"], writes=[("qr32", i)])
                    P.op("act", lambda e, i=i, pb=pb: e.activation(qrsq[i], ps[pb][0:64, :], AF.Square), reads=[PS(pb)], writes=[("qrsq", i)])
                    P.op("pe", lambda e, i=i, pc=pc: e.matmul(ps[pc][:], onesb, qnsq[i], start=True, stop=False),
                         reads=["const", ("qnsq", i)], writes=[PS(pc)])
                    P.op("pe", lambda e, i=i, pc=pc: e.matmul(ps[pc][:], onesb[0:64, :], qrsq[i], start=False, stop=True),
                         reads=["const", ("qrsq", i)], writes=[PS(pc)])
                    rstd_bcast(rsh[i], ps[pc], PS(pc), 192, ("rsh", i))
                    P.op("dve", lambda e, h=h, i=i: e.scalar_tensor_tensor(QN[:, h, :], qn32[i], gqs[:, 0:1], rsh[i], ALU.mult, ALU.mult),
                         reads=[("qn32", i), ("rsh", i), "gqs"], writes=[("qon", tt % 2)])
                    P.op("pool", lambda e, i=i: e.tensor_copy(qrgb[i], qr32[i]), reads=[("qr32", i)], writes=[("qrgb", i)])
                    P.op("pe", lambda e, i=i, pd=pd: e.matmul(ps[pd][0:64, :], rot, qrgb[i], start=True, stop=True),
                         reads=["const", ("qrgb", i)], writes=[PS(pd)])
                    P.op("pool", lambda e, i=i: e.tensor_tensor(qro[i], qr32[i], cs, ALU.mult), reads=[("qr32", i), "qcs"], writes=[("qro", i)])
                    P.op("dve", lambda e, i=i, pd=pd: e.tensor_tensor(tmp64[i], ps[pd][0:64, :], sn, ALU.mult), reads=[PS(pd), "qcs"], writes=[("tmp64", i)])
                    P.op("pool", lambda e, i=i: e.tensor_tensor(qro[i], qro[i], tmp64[i], ALU.add), reads=[("qro", i), ("tmp64", i)], writes=[("qro", i)])
                    P.op("pool", lambda e, h=h, i=i: e.tensor_tensor(QR[:, h, :], qro[i], rsh[i][0:64, :], ALU.mult),
                         reads=[("qro", i), ("rsh", i)], writes=[("qor", tt % 2)])
                P.dma("act", QT.rearrange("h p t -> p h t")[0:128, :, tt * 512:(tt + 1) * 512], QN, reads=[("qon", tt % 2)],
                      semkey=("qons", tt % 2))
                P.dma("act", QT.rearrange("h p t -> p h t")[128:192, :, tt * 512:(tt + 1) * 512], QR, reads=[("qor", tt % 2)],
                      semkey=("qors", tt % 2))
            P.barrier()
            A.reset()

        def phase_attn():
            NKB = S // 128
            ktn = [A.alloc(S) for _ in range(2)]
            ktr = [A.alloc(S, parts=64) for _ in range(2)]
            vh = [A.alloc(NKB * 128).rearrange("p (k d) -> p k d", d=128) for _ in range(2)]
            qtn = [A.alloc(T) for _ in range(2)]
            qtr = [A.alloc(T, parts=64) for _ in range(2)]
            pt = [A.alloc(512) for _ in range(3)]
            rl = A.alloc(512, F32)
            yo = [A.alloc(512) for _ in range(2)]
            def prefetch(h):
                hb = h % 2; hk = ("head", hb)
                P.dma("sp", ktn[hb], KT[h, 0:128, :], writes=[hk], semkey=hk)
                P.dma("sp", ktr[hb], KT[h, 128:192, :], writes=[hk], semkey=hk)
                P.dma("sp", vh[hb], VTK.rearrange("(k p) f -> p k f", p=128)[:, :, h * 128:(h + 1) * 128], writes=[hk], semkey=hk)
                P.dma("sp", qtn[hb], QT[h, 0:128, :], writes=[hk], semkey=hk)
                P.dma("sp", qtr[hb], QT[h, 128:192, :], writes=[hk], semkey=hk)

            steps = []
            nch = 0
            for h in range(16):
                for qc in range(NTO):
                    nkb = 8 * qc + 8
                    for kb in range(nkb):
                        steps.append((h, qc, kb, nkb, nch))
                    nch += 1

            def emit_S(i):
                h, qc, kb, nkb, nch_ = steps[i]
                hb = h % 2; hk = ("head", hb)
                jlo = max(0, kb // 2 - 4 * qc)
                Nv = (4 - jlo) * 128
                q0 = qc * 512 + jlo * 128
                si = i % 3
                P.op("pe", lambda e: e.matmul(ps[si][:, 0:Nv], ktn[hb][:, kb * 128:(kb + 1) * 128], qtn[hb][:, q0:q0 + Nv], start=True, stop=False),
                     reads=[hk], writes=[PS(si)])
                P.op("pe", lambda e: e.matmul(ps[si][:, 0:Nv], ktr[hb][:, kb * 128:(kb + 1) * 128], qtr[hb][:, q0:q0 + Nv], start=False, stop=True),
                     reads=[hk], writes=[PS(si)])

            prefetch(0)
            emit_S(0)
            for i, (h, qc, kb, nkb, nch_) in enumerate(steps):
                hb = h % 2; hk = ("head", hb)
                if qc == 0 and kb == 0 and h + 1 < 16:
                    prefetch(h + 1)
                if i + 1 < len(steps):
                    emit_S(i + 1)
                po = 3 + (nch_ % 2) * 2; pl = po + 1
                jlo = max(0, kb // 2 - 4 * qc)
                Nv = (4 - jlo) * 128
                si = i % 3
                pb = pt[si]; pkey = ("pt", si)
                if jlo > 0:
                    P.op("pool", lambda e: e.memset(pb[:, 0:jlo * 128], 0.0), writes=[pkey])
                P.op("act", lambda e: e.activation(pb[:, jlo * 128:512], ps[si][:, 0:Nv], AF.Exp), reads=[PS(si)], writes=[pkey])
                if kb // 2 >= 4 * qc:
                    par = kb % 2
                    P.op("pool", lambda e: e.tensor_tensor(
                        pb[:, jlo * 128:(jlo + 1) * 128], pb[:, jlo * 128:(jlo + 1) * 128], masks[:, par * 128:(par + 1) * 128], ALU.mult),
                        reads=[pkey, "const"], writes=[pkey])
                P.op("pe", lambda e: e.matmul(ps[po][:], vh[hb][:, kb, :], pb, start=(kb == 0), stop=(kb == nkb - 1)),
                     reads=[hk, pkey], writes=[PS(po)])
                P.op("pe", lambda e: e.matmul(ps[pl][:], onesb, pb, start=(kb == 0), stop=(kb == nkb - 1)),
                     reads=["const", pkey], writes=[PS(pl)])
                if kb == nkb - 1:
                    P.op("dve", lambda e: e.reciprocal(rl, ps[pl][:]), reads=[PS(pl)], writes=["rl"])
                    yb = yo[nch_ % 2]; yk = ("yo", nch_ % 2)
                    P.op("dve", lambda e: e.tensor_tensor(yb, ps[po][:], rl, ALU.mult), reads=[PS(po), "rl"], writes=[yk])
                    P.dma("act", YMT[h * 128:(h + 1) * 128, qc * 512:(qc + 1) * 512], yb, reads=[yk], semkey=("yos", nch_ % 2))
            P.barrier()
            A.reset()

        def simple_pass(wsrc, in_scr, mode, aux1, aux2, dst):
            xin = [A.alloc(16 * 512).rearrange("p (k t) -> p k t", t=512) for _ in range(2)]
            a1 = [A.alloc(8 * 512).rearrange("p (c t) -> p c t", t=512) for _ in range(2)] if aux1 is not None else None
            a2 = [A.alloc(8 * 512).rearrange("p (c t) -> p c t", t=512) for _ in range(2)] if aux2 is not None else None
            ob = [A.alloc(8 * 512).rearrange("p (c t) -> p c t", t=512) for _ in range(2)]
            t32 = [A.alloc(512, F32) for _ in range(2)]
            u32 = [A.alloc(512, F32) for _ in range(2)]
            wts = [load_w(wsrc[:, half * 1024:(half + 1) * 1024], 1024) for half in range(2)]
            for half in range(2):
                wt, wkey = wts[half]
                cnt = [0]
                last_tt = [-1]

                def epi(fc, tt, pst, pk, half=half, cnt=cnt, last_tt=last_tt):
                    i = cnt[0] % 2; cnt[0] += 1
                    o = ob[tt % 2]; ok = ("ob", tt % 2)
                    if tt != last_tt[0]:
                        last_tt[0] = tt
                        for aux, ab, nm in ((aux1, a1, "a1"), (aux2, a2, "a2")):
                            if aux is not None:
                                P.dma("sp", ab[tt % 2], aux.rearrange("(c p) t -> p c t", p=128)[:, half * 8:(half + 1) * 8, tt * 512:(tt + 1) * 512],
                                      writes=[(nm, tt % 2)])
                    if mode == "gelu_mul":
                        t = t32[i]; tk = ("t32", i); u = u32[i]; uk = ("u32", i)
                        P.op("act", lambda e: e.activation(t, pst[:], AF.Square), reads=[pk], writes=[tk])
                        P.op("dve", lambda e: e.tensor_scalar(t, t, 0.044715, 1.0, ALU.mult, ALU.add), reads=[tk], writes=[tk])
                        P.op("dve", lambda e: e.tensor_tensor(t, t, pst[:], ALU.mult), reads=[tk, pk], writes=[tk])
                        P.op("act", lambda e: e.activation(t, t, AF.Sigmoid, scale=2.0 * math.sqrt(2.0 / PI)), reads=[tk], writes=[tk])
                        P.op("dve", lambda e: e.tensor_tensor(u, t, pst[:], ALU.mult), reads=[tk, pk], writes=[uk])
                        P.op("pool", lambda e: e.tensor_tensor(o[:, fc, :], u, a1[tt % 2][:, fc, :], ALU.mult),
                             reads=[uk, ("a1", tt % 2)], writes=[ok])
                    elif mode == "sigmoid":
                        P.op("act", lambda e: e.activation(o[:, fc, :], pst[:], AF.Sigmoid), reads=[pk], writes=[ok])
                    elif mode == "mul":
                        P.op("dve", lambda e: e.tensor_tensor(o[:, fc, :], pst[:], a1[tt % 2][:, fc, :], ALU.mult),
                             reads=[pk, ("a1", tt % 2)], writes=[ok])
                    elif mode == "mul_add":
                        u = u32[i]; uk = ("u32", i)
                        P.op("dve", lambda e: e.tensor_tensor(u, pst[:], a1[tt % 2][:, fc, :], ALU.mult),
                             reads=[pk, ("a1", tt % 2)], writes=[uk])
                        P.op("pool", lambda e: e.tensor_tensor(o[:, fc, :], u, a2[tt % 2][:, fc, :], ALU.add),
                             reads=[uk, ("a2", tt % 2)], writes=[ok])
                    if fc == 7:
                        P.dma("act", dst.rearrange("(c p) t -> p c t", p=128)[:, half * 8:(half + 1) * 8, tt * 512:(tt + 1) * 512], o,
                              reads=[ok], semkey=("obs", tt % 2))
                    return None
                fm_pass(wt, wkey, 1024, in_scr, T, epi, [0, 1, 2, 3], xin)
            P.barrier()
            A.reset()

        def make_row(dst, col, tagr):
            bt = A.alloc(128, F32)
            for j in range(16):
                P.op("dve", lambda e, j=j: e.tensor_copy(bt, col[:, j:j + 1].to_broadcast([128, 128])),
                     reads=["modc", "A2"], writes=["bt"])
                P.op("pe", lambda e: e.transpose(ps[7][:, 0:128], bt, ident32), reads=["bt", "const"], writes=[PS(7)])
                P.op("act", lambda e, j=j: e.copy(dst[:, j * 128:(j + 1) * 128], ps[7][:, 0:128]), reads=[PS(7)], writes=[tagr])

        NT128 = T // 128

        regc = {}

        def phase_out_route(rwk, gidx):
            g1row = A.alloc(D, F32); a2row = A.alloc(D, F32); sh2row = A.alloc(D, F32)
            make_row(g1row, modc[:, 32:48], "g1row")
            make_row(a2row, A2, "a2row")
            make_row(sh2row, modc[:, 48:64], "sh2row")
            wo = [load_w(I.w_out[:, half * 1024:(half + 1) * 1024], 1024) for half in range(2)]
            wr32 = A.alloc(16 * 64, F32).rearrange("p (k e) -> p k e", e=64)
            P.dma("sp", wr32, I.w_router.rearrange("(k p) e -> p k e", p=128), writes=["wr32"])
            xin = [A.alloc(16 * 512).rearrange("p (k t) -> p k t", t=512) for _ in range(2)]
            xo = [A.alloc(D, F32) for _ in range(2)]
            vtk = A.alloc(D, F32)
            vbf = [A.alloc(D) for _ in range(2)]
            junk = A.alloc(D)
            vT = A.alloc(16 * 128, F32).rearrange("p (k t) -> p k t", t=128)
            sm = A.alloc(64, F32)
            sc_ = A.alloc(64, F32); sel = A.alloc(64, F32); msk = A.alloc(64, F32); rw = A.alloc(64, F32)
            mskb = A.alloc(64); pos = A.alloc(64, F32); carry = A.alloc(64, F32); oh = A.alloc(64, F32)
            mx8 = A.alloc(8, F32); ix8 = A.alloc(8, U32); ixf = A.alloc(8, F32); posk = A.alloc(8, F32)
            dst_f = A.alloc(8, F32); ovf = A.alloc(8, F32); dsti = [A.alloc(8, I32) for _ in range(2)]
            P.op("pool", lambda e: e.memset(carry, 0.0), writes=["carry"])
            mT = MT.rearrange("(k p) t -> p k t", p=128)
            for tt in range(NTO):
                xb = xin[tt % 2]; xk = ("xin", tt % 2)
                P.dma("sp", xb, mT[:, :, tt * 512:(tt + 1) * 512], writes=[xk])
                for sub in range(4):
                    ti = tt * 4 + sub
                    r0 = ti * 128
                    X = xo[ti % 2]; Xk = ("xo", ti % 2)
                    P.dma("sp", X, I.x_own[r0:r0 + 128, :], writes=[Xk])
                    for dc in range(4):
                        wt, wkey = wo[dc // 2]
                        for k in range(16):
                            P.op("pe", lambda e, dc=dc, k=k, wt=wt, xb=xb, sub=sub: e.matmul(
                                ps[dc][:], xb[:, k, sub * 128:(sub + 1) * 128], wt[:, k, (dc % 2) * 512:(dc % 2 + 1) * 512],
                                start=(k == 0), stop=(k == 15)), reads=[wkey, xk], writes=[PS(dc)])
                        P.op("dve", lambda e, dc=dc: e.tensor_tensor(vtk[:, dc * 512:(dc + 1) * 512], ps[dc][:], g1row[:, dc * 512:(dc + 1) * 512], ALU.mult),
                             reads=[PS(dc), "g1row"], writes=[("vtk", dc)])
                        P.op("pool", lambda e, dc=dc, X=X: e.tensor_tensor(X[:, dc * 512:(dc + 1) * 512], X[:, dc * 512:(dc + 1) * 512], vtk[:, dc * 512:(dc + 1) * 512], ALU.add),
                             reads=[Xk, ("vtk", dc)], writes=[Xk])
                    P.dma("act", X1[r0:r0 + 128, :], X, reads=[Xk], semkey=("x1s", ti % 2))
                    ss = sm[:, 0:1]; rs_ = sm[:, 1:2]
                    P.op("act", lambda e, X=X: e.activation(junk, X, AF.Square, accum_out=ss), reads=[Xk], writes=["junk", "ss"])
                    P.op("act", lambda e: e.activation(rs_, ss, AF.Sqrt, scale=1.0 / D, bias=EPS), reads=["ss"], writes=["rs_"])
                    P.op("dve", lambda e: e.reciprocal(rs_, rs_), reads=["rs_"], writes=["rs_"])
                    P.op("dve", lambda e, X=X: e.scalar_tensor_tensor(vtk, X, rs_, a2row, ALU.mult, ALU.mult),
                         reads=[Xk, "rs_", "a2row"] + [("vtk", d) for d in range(4)], writes=[("vtk", d) for d in range(4)] + ["vtkall"])
                    P.op("pool", lambda e: e.tensor_tensor(vtk, vtk, sh2row, ALU.add), reads=["vtkall", "sh2row"], writes=["vtkall"] + [("vtk", d) for d in range(4)])
                    VB = vbf[ti % 2]; VBk = ("vbf", ti % 2)
                    P.op("act", lambda e, VB=VB: e.copy(VB, vtk), reads=["vtkall"], writes=[VBk])
                    for q4 in range(4):
                        pi = 4 + q4 % 2
                        for j in range(4):
                            fc = q4 * 4 + j
                            P.op("pe", lambda e, fc=fc, j=j, pi=pi: e.transpose(ps[pi][:, j * 128:(j + 1) * 128], vtk[:, fc * 128:(fc + 1) * 128], ident32),
                                 reads=["vtkall", "const"], writes=[PS(pi)])
                        P.op("act", lambda e, q4=q4, pi=pi: e.copy(vT[:, q4 * 4:(q4 + 1) * 4, :].rearrange("p k t -> p (k t)"), ps[pi][:]),
                             reads=[PS(pi)], writes=["vT"])
                    for k in range(16):
                        P.op("pe", lambda e, k=k: e.matmul(ps[6][:, 0:64], vT[:, k, :], wr32[:, k, :], start=(k == 0), stop=(k == 15)),
                             reads=["vT", "wr32"], writes=[PS(6)])
                    P.op("act", lambda e: e.activation(sc_, ps[6][:, 0:64], AF.Sigmoid), reads=[PS(6)], writes=["sc_"])
                    P.op("dve", lambda e: e.tensor_tensor(sel, sc_, rbias, ALU.add), reads=["sc_", "const"], writes=["sel"])
                    P.op("dve", lambda e: e.max(mx8, sel), reads=["sel"], writes=["mx8"])
                    P.op("dve", lambda e: e.max_index(ix8, mx8, sel), reads=["sel", "mx8"], writes=["ix8"])
                    P.op("dve", lambda e: e.tensor_scalar(msk, sel, mx8[:, 5:6], None, ALU.is_ge), reads=["sel", "mx8"], writes=["msk"])
                    P.op("dve", lambda e: e.tensor_tensor(rw, sc_, msk, ALU.mult), reads=["sc_", "msk"], writes=["rw"])
                    den = sm[:, 2:3]
                    P.op("dve", lambda e: e.reduce_sum(den, rw, axis=AX.X), reads=["rw"], writes=["den"])
                    P.op("dve", lambda e: e.reciprocal(den, den), reads=["den"], writes=["den"])
                    P.op("dve", lambda e: e.tensor_scalar(rw, rw, den, 2.5, ALU.mult, ALU.mult), reads=["rw", "den"], writes=["rw"])
                    P.op("act", lambda e: e.copy(mskb, msk), reads=["msk"], writes=["mskb"])
                    P.op("pe", lambda e: e.matmul(ps[7][:, 0:64], ustr, mskb, start=True, stop=True), reads=["const", "mskb"], writes=[PS(7)])
                    P.op("pe", lambda e: e.matmul(ps[7][:, 64:128], onesb, mskb, start=True, stop=True), reads=["const", "mskb"], writes=[PS(7)])
                    P.op("dve", lambda e: e.tensor_tensor(pos, ps[7][:, 0:64], carry, ALU.add), reads=[PS(7), "carry"], writes=["pos"])
                    P.op("dve", lambda e: e.tensor_tensor(carry, ps[7][:, 64:128], carry, ALU.add), reads=[PS(7), "carry"], writes=["carry"])
                    P.op("dve", lambda e: e.tensor_copy(ixf, ix8), reads=["ix8"], writes=["ixf"])
                    for k in range(TOPK):
                        P.op("dve", lambda e, k=k: e.tensor_scalar(oh, iota, ixf[:, k:k + 1], None, ALU.is_equal), reads=["const", "ixf"], writes=["oh"])
                        P.op("dve", lambda e: e.tensor_tensor(msk, oh, pos, ALU.mult), reads=["oh", "pos", "msk"], writes=["msk"])
                        P.op("dve", lambda e, k=k: e.reduce_sum(posk[:, k:k + 1], msk, axis=AX.X), reads=["msk"], writes=["posk"])
                        P.op("dve", lambda e: e.tensor_tensor(msk, oh, rw, ALU.mult), reads=["oh", "rw", "msk"], writes=["msk"])
                        P.op("dve", lambda e, k=k, ti=ti: e.reduce_sum(rwk[:, ti * 8 + k:ti * 8 + k + 1], msk, axis=AX.X), reads=["msk"], writes=["rwk"])
                    P.op("dve", lambda e: e.scalar_tensor_tensor(dst_f[:, 0:6], ixf[:, 0:6], float(C), posk[:, 0:6], ALU.mult, ALU.add),
                         reads=["ixf", "posk"], writes=["dst_f"])
                    P.op("dve", lambda e: e.tensor_scalar(ovf[:, 0:6], posk[:, 0:6], float(C), None, ALU.is_ge), reads=["posk"], writes=["ovf"])
                    P.op("dve", lambda e: e.tensor_scalar(posk[:, 0:6], ovf[:, 0:6], -1.0, 1.0, ALU.mult, ALU.add), reads=["ovf", "posk"], writes=["posk"])
                    P.op("dve", lambda e, ti=ti: e.tensor_tensor(rwk[:, ti * 8:ti * 8 + 6], rwk[:, ti * 8:ti * 8 + 6], posk[:, 0:6], ALU.mult),
                         reads=["posk", "rwk"], writes=["rwk"])
                    P.op("dve", lambda e: e.tensor_tensor(dst_f[:, 0:6], dst_f[:, 0:6], posk[:, 0:6], ALU.mult), reads=["dst_f", "posk"], writes=["dst_f"])
                    DI = dsti[ti % 2]; DIk = ("dsti", ti % 2)
                    P.op("dve", lambda e: e.scalar_tensor_tensor(posk[:, 0:6], ovf[:, 0:6], 4194304.0, dst_f[:, 0:6], ALU.mult, ALU.add),
                         reads=["ovf", "dst_f", "posk"], writes=["posk"])
                    P.op("dve", lambda e, DI=DI: e.tensor_copy(DI[:, 0:6], posk[:, 0:6]), reads=["posk"], writes=[DIk])
                    P.op("dve", lambda e, DI=DI, ti=ti: e.tensor_copy(gidx[:, ti * 8:ti * 8 + 6], dst_f[:, 0:6]), reads=["dst_f"], writes=["gidx"])
                    for k in range(TOPK):
                        def scat(e, DI=DI, VB=VB, k=k):
                            if "r" not in regc:
                                regc["r"] = e.to_reg(NROW - 1)
                            return e.indirect_dma_start(
                                out=XG, out_offset=bass.IndirectOffsetOnAxis(ap=DI[:, k:k + 1], axis=0), in_=VB, in_offset=None,
                                bounds_check=regc["r"], oob_is_err=False)
                        P.raw("pool", scat,
                            reads=[DIk, VBk], writes=[], semkey=("sc", ti % 2))
                    P.dma("act", XS[r0:r0 + 128, :], VB, reads=[VBk], semkey=("xgs", ti % 2))
            P.barrier()
            A.reset()

        def phase_experts():
            NH = C // 512
            xg = [A.alloc(4 * D).rearrange("p (b f) -> p b f", f=D) for _ in range(2)]
            xT = A.alloc(16 * C).rearrange("p (k s) -> p k s", s=C)
            wg = [A.alloc(16 * 128).rearrange("p (k f) -> p k f", f=128) for _ in range(3)]
            wu = [A.alloc(16 * 128).rearrange("p (k f) -> p k f", f=128) for _ in range(3)]
            hT = A.alloc(NFC * C).rearrange("p (f s) -> p f s", s=C)
            wd = [A.alloc(NFC * 512).rearrange("p (f d) -> p f d", d=512) for _ in range(3)]
            sg = [A.alloc(512, F32) for _ in range(2)]
            yb = [A.alloc(512) for _ in range(4)]
            nxg = 0; nw = 0; nwd = 0; nsg = 0; ny = 0; npsA = 0; ntr = 0
            for ex in range(NX):
                if ex > 0 and ex % 8 == 0:
                    P.barrier()
                if ex < NE:
                    Wg = getattr(I, "wg%02d" % (ex // 4))[ex % 4]; Wu = getattr(I, "wu%02d" % (ex // 4))[ex % 4]
                    Wd = getattr(I, "wd%02d" % (ex // 4))[ex % 4]
                    wq_, wdeps = "pool", []
                else:
                    Wg = I.ws_gate; Wu = I.ws_up; Wd = I.ws_down
                    wq_, wdeps = "pool", []
                for hh_ in range(NH):
                    g = xg[nxg % 2]; gk = ("xg", nxg % 2); nxg += 1
                    r0 = (ex if ex < NE else ex - NE) * C + hh_ * 512
                    XSRC = XG if ex < NE else XS
                    P.dma("sp", g, XSRC[r0:r0 + 512, :].rearrange("(b p) f -> p b f", p=128), writes=[gk])
                    for k in range(16):
                        pi = 6 + ntr % 2; ntr += 1
                        pbf = ps[pi][:].bitcast(BF16)
                        for b4 in range(4):
                            P.op("pe", lambda e, g=g, b4=b4, k=k, pbf=pbf: e.transpose(pbf[:, b4 * 128:(b4 + 1) * 128], g[:, b4, k * 128:(k + 1) * 128], identb),
                                 reads=[gk, "const"], writes=[PS(pi)])
                        eng = "act" if k % 2 == 0 else "dve"
                        if eng == "act":
                            P.op("act", lambda e, k=k, hh_=hh_, pbf=pbf: e.copy(xT[:, k, hh_ * 512:(hh_ + 1) * 512], pbf[:, 0:512]),
                                 reads=[PS(pi)], writes=["xT"])
                        else:
                            P.op("dve", lambda e, k=k, hh_=hh_, pbf=pbf: e.tensor_copy(xT[:, k, hh_ * 512:(hh_ + 1) * 512], pbf[:, 0:512]),
                                 reads=[PS(pi)], writes=["xT"])
                Wgv = Wg.rearrange("(k p) f -> p k f", p=128); Wuv = Wu.rearrange("(k p) f -> p k f", p=128)
                for fc in range(NFC):
                    wi = nw % 3; nw += 1
                    P.dma(wq_, wg[wi], Wgv[:, :, fc * 128:(fc + 1) * 128], reads=wdeps[0:1], writes=[("wg", wi)])
                    P.dma(wq_, wu[wi], Wuv[:, :, fc * 128:(fc + 1) * 128], reads=wdeps[1:2], writes=[("wu", wi)])
                    for hh_ in range(NH):
                        pg = (npsA % 3) * 2; pu = pg + 1; npsA += 1
                        for k in range(16):
                            P.op("pe", lambda e, wi=wi, k=k, hh_=hh_, pg=pg: e.matmul(ps[pg][:], wg[wi][:, k, :], xT[:, k, hh_ * 512:(hh_ + 1) * 512],
                                                                                    start=(k == 0), stop=(k == 15)),
                                 reads=[("wg", wi), "xT"], writes=[PS(pg)])
                        for k in range(16):
                            P.op("pe", lambda e, wi=wi, k=k, hh_=hh_, pu=pu: e.matmul(ps[pu][:], wu[wi][:, k, :], xT[:, k, hh_ * 512:(hh_ + 1) * 512],
                                                                                    start=(k == 0), stop=(k == 15)),
                                 reads=[("wu", wi), "xT"], writes=[PS(pu)])
                        s = sg[nsg % 2]; sk_ = ("sg", nsg % 2); nsg += 1
                        P.op("act", lambda e, s=s, pg=pg: e.activation(s, ps[pg][:], AF.Silu), reads=[PS(pg)], writes=[sk_])
                        P.op("dve", lambda e, s=s, pu=pu, fc=fc, hh_=hh_: e.tensor_tensor(hT[:, fc, hh_ * 512:(hh_ + 1) * 512], s, ps[pu][:], ALU.mult),
                             reads=[sk_, PS(pu)], writes=["hT"])
                Wdv = Wd.rearrange("(f p) d -> p f d", p=128)
                for dc in range(4):
                    wi = nwd % 3; nwd += 1
                    P.dma(wq_, wd[wi], Wdv[:, :, dc * 512:(dc + 1) * 512], reads=wdeps[2:3], writes=[("wd", wi)])
                    for sb in range(C // 128):
                        pi = (npsA % 3) * 2 + (sb % 2);
                        if sb % 2 == 1:
                            npsA += 1
                        for fc in range(NFC):
                            P.op("pe", lambda e, wi=wi, fc=fc, sb=sb, pi=pi: e.matmul(ps[pi][:], hT[:, fc, sb * 128:(sb + 1) * 128], wd[wi][:, fc, :],
                                                                                    start=(fc == 0), stop=(fc == NFC - 1)),
                                 reads=[("wd", wi), "hT"], writes=[PS(pi)])
                        y = yb[ny % 4]; yk = ("yb", ny % 4)
                        if ny % 2 == 0:
                            P.op("act", lambda e, y=y, pi=pi: e.copy(y, ps[pi][:]), reads=[PS(pi)], writes=[yk])
                        else:
                            P.op("dve", lambda e, y=y, pi=pi: e.tensor_copy(y, ps[pi][:]), reads=[PS(pi)], writes=[yk])
                        r0 = (ex if ex < NE else ex - NE) * C + sb * 128
                        YDST = YG if ex < NE else YS
                        P.dma("act", YDST[r0:r0 + 128, dc * 512:(dc + 1) * 512], y, reads=[yk], semkey=("ybs", ny % 4))
                        ny += 1
                    if C // 128 % 2 == 1:
                        npsA += 1
            P.barrier()
            A.reset()

        def phase_combine(rwk, gidx):
            g2row = A.alloc(D, F32)
            make_row(g2row, modc[:, 80:96], "g2row")
            yg = [A.alloc(7 * D).rearrange("p (k f) -> p k f", f=D) for _ in range(2)]
            acc = [A.alloc(D, F32) for _ in range(2)]
            x1 = [A.alloc(D, F32) for _ in range(2)]
            for ti in range(NT128):
                r0 = ti * 128
                Y = yg[ti % 2]; Yk = ("yg", ti % 2)
                for k in range(TOPK):
                    P.raw("pool", lambda e, Y=Y, k=k, ti=ti: e.indirect_dma_start(
                        out=Y[:, k, :], out_offset=None, in_=YG, in_offset=bass.IndirectOffsetOnAxis(ap=gidx[:, ti * 8 + k:ti * 8 + k + 1], axis=0)),
                        reads=["gidx"], writes=[Yk], semkey=Yk)
                P.dma("sp", Y[:, 6, :], YS[r0:r0 + 128, :], writes=[Yk], semkey=Yk)
                X = x1[ti % 2]; Xk = ("x1", ti % 2)
                P.dma("sp", X, X1[r0:r0 + 128, :], writes=[Xk])
                a = acc[ti % 2]; ak = ("acc", ti % 2)
                P.op("dve", lambda e, a=a, Y=Y, ti=ti: e.scalar_tensor_tensor(a, Y[:, 0, :], rwk[:, ti * 8:ti * 8 + 1], Y[:, 6, :], ALU.mult, ALU.add),
                     reads=[Yk, "rwk"], writes=[ak])
                for k in range(1, TOPK):
                    P.op("dve", lambda e, a=a, Y=Y, ti=ti, k=k: e.scalar_tensor_tensor(a, Y[:, k, :], rwk[:, ti * 8 + k:ti * 8 + k + 1], a, ALU.mult, ALU.add),
                         reads=[Yk, "rwk", ak], writes=[ak])
                P.op("dve", lambda e, a=a: e.tensor_tensor(a, a, g2row, ALU.mult), reads=[ak, "g2row"], writes=[ak])
                P.op("pool", lambda e, a=a, X=X: e.tensor_tensor(X, X, a, ALU.add), reads=[ak, Xk], writes=[Xk])
                P.dma("act", out_d[r0:r0 + 128, :], X, reads=[Xk], semkey=("outs", ti % 2))
            P.barrier()

        def zero_scratch():
            z = A.alloc(4 * D).rearrange("p (b f) -> p b f", f=D)
            P.op("pool", lambda e: e.memset(z, 0.0), writes=["z"])
            for r in range(0, NROW, 512):
                P.dma("sp", XG[r:r + 512, :].rearrange("(b p) f -> p b f", p=128), z, reads=["z"], semkey="zx")
            P.barrier()
            A.reset()

        rwk = A.alloc(NT128 * 8, F32)
        gidx = A.alloc(NT128 * 8, I32)
        A.mark()

        phases = dbg_phases if (dbg_phases := getattr(build, "phases", None)) else None
        def want(nm):
            return phases is None or nm in phases
        if want("zero"):
            zero_scratch()
        if want("p0"):
            phase0()
        else:
            P.dma("sp", modc, MODC, writes=["modc"])
        if want("p1"):
            norm_T(I.x_all, S, UTA, A1, modc[:, 0:16])
            norm_T(I.x_own, T, UTO, A1, modc[:, 0:16])
        if want("rnn"):
            phase_rnn()
        if want("kv"):
            phase_kv()
        if want("q"):
            phase_q()
        if want("attn"):
            phase_attn()
        if want("merge"):
            simple_pass(I.w_in[:, 2048:4096], UTO, "gelu_mul", HT, None, YRT)
            simple_pass(I.w_in[:, 4928:6976], UTO, "sigmoid", None, None, GAT)
            simple_pass(I.w_in[:, 6976:9024], UTO, "sigmoid", None, None, GBT)
            simple_pass(I.w_rnn_out, YRT, "mul", GAT, None, MA)
            simple_pass(I.w_mla_out, YMT, "mul_add", GBT, MA, MT)
        if want("route"):
            phase_out_route(rwk, gidx)
        if want("experts"):
            phase_experts()
        if want("combine"):
            phase_combine(rwk, gidx)
        P.barrier()
        P.emit()
    nc._declared = declared
    return nc


def _col(v, n):
    return np.ascontiguousarray(np.asarray(v, np.float32).reshape(n, 128).T)


def make_in_maps(S, inp):
    bf = ml_dtypes.bfloat16
    x = np.asarray(inp["x"]); B = x.shape[0]
    pos = np.asarray(inp["positions"]).astype(np.int32)
    sq = lambda k: np.ascontiguousarray(np.asarray(inp[k])[0])
    ident = np.eye(128, dtype=np.float32)
    ustr = np.triu(np.ones((128, 128), np.float32), 1)
    rot = np.zeros((64, 64), np.float32)
    for m in range(32):
        rot[m + 32, m] = -1.0
        rot[m, m + 32] = 1.0
    invf = (np.float32(10000.0) ** (-np.arange(0, 64, 2, dtype=np.float32) / np.float32(64))).astype(np.float32)
    invf2 = np.concatenate([invf, invf]).reshape(64, 1).astype(np.float32)
    iota = np.broadcast_to(np.arange(64, dtype=np.float32)[None, :], (128, 64)).copy()
    vec16 = np.concatenate([_col(sq("norm1"), 16), _col(sq("norm2"), 16), _col(sq("conv_b"), 16), _col(sq("b_a"), 16),
                            _col(sq("b_i"), 16), _col(sq("lru_lambda"), 16), np.zeros((128, 16), np.float32)], axis=1)
    cw = sq("conv_w")
    convw = np.concatenate([_col(cw[j], 16) for j in range(4)], axis=1)
    qn = sq("q_norm"); kn = sq("k_norm")
    qkn = np.zeros((128, 4), np.float32)
    qkn[:, 0] = qn[:128]; qkn[:64, 1] = qn[128:]; qkn[:, 2] = kn[:128]; qkn[:64, 3] = kn[128:]
    shared = dict(
        ident32=ident, identb=ident.astype(bf), onesb=np.ones((128, 128), bf), ustrict=ustr.astype(bf),
        rot=rot.astype(bf), iota_row=iota, invf=invf2, bmod_col=_col(sq("b_mod"), 96), vec16=vec16, convw_col=convw,
        qan_col=_col(sq("q_a_norm"), 4), kvan_col=_col(sq("kv_a_norm"), 2), qkn_col=qkn,
        rbias_row=np.broadcast_to(sq("router_bias")[None, :], (128, 64)).copy(),
        w_mod=sq("w_mod"), w_in=sq("w_in"), w_a=sq("w_a"), w_i=sq("w_i"),
        w_uq=sq("w_uq").reshape(512, 3072), w_ukv=sq("w_ukv").reshape(256, 4096),
        w_rnn_out=sq("w_rnn_out"), w_mla_out=sq("w_mla_out"), w_out=sq("w_out"), w_router=sq("w_router"),
        ws_gate=sq("ws_gate"), ws_up=sq("ws_up"), ws_down=sq("ws_down"),
    )
    wg_ = np.asarray(inp["w_gate"])[0]; wu_ = np.asarray(inp["w_up"])[0]; wd_ = np.asarray(inp["w_down"])[0]
    for e_ in range(NE // 4):
        shared["wg%02d" % e_] = wg_[4 * e_:4 * e_ + 4]; shared["wu%02d" % e_] = wu_[4 * e_:4 * e_ + 4]
        shared["wd%02d" % e_] = wd_[4 * e_:4 * e_ + 4]
    maps = []
    for core in range(2 * B):
        b = core // 2; c = core % 2
        xb = np.ascontiguousarray(x[b])
        xo = np.ascontiguousarray(xb.reshape(S // 128, 128, D)[c::2].reshape(S // 2, D))
        pa = pos[b]
        po = np.ascontiguousarray(pa.reshape(S // 128, 128)[c::2].reshape(S // 2))
        q = np.arange(128)[None, :] // 64; k = np.arange(128)[:, None] // 64
        diag = (k <= q).astype(np.float32)
        if c == 0:
            m_even = diag; m_odd = np.zeros((128, 128), np.float32)
        else:
            m_even = np.ones((128, 128), np.float32); m_odd = diag
        m = dict(shared)
        m.update(

            x_all=xb, x_own=xo,
            pos_all=np.broadcast_to(pa[None, :], (64, S)).copy(), pos_own=np.broadcast_to(po[None, :], (64, S // 2)).copy(),
            c_col=_col(np.asarray(inp["c"])[b], 16),
            cpar=np.broadcast_to(np.array([[c, 1 - c]], np.float32), (128, 2)).copy(),
            masks=np.concatenate([m_even, m_odd], axis=1).astype(bf),
        )
        maps.append(m)
    return maps


_CACHE = {}


def run(inp, S, C, dbg=(), ncores=None):
    key = (S, C, tuple(dbg))
    if key not in _CACHE:
        _CACHE[key] = build(S, C, dbg)
    nc = _CACHE[key]
    maps = make_in_maps(S, inp)
    if ncores is not None:
        maps = maps[:ncores]
    maps = [{k: m[k] for k in nc._declared} for m in maps]
    res = run_bass_kernel_spmd(nc, maps, core_ids=list(range(len(maps))))
    return res


def kernel(**inputs):
    x = np.asarray(inputs["x"])
    B, S, _ = x.shape
    res = run(inputs, S, 1024)
    out = np.empty((B, S, D), np.float32)
    for core, r in enumerate(res.results):
        b = core // 2; c = core % 2
        out[b].reshape(S // 128, 128, D)[c::2] = r["out"].reshape(S // 256, 128, D)
    return out
```

```python
import math
import types
import numpy as np
import ml_dtypes
import concourse.bass as bass
import concourse.mybir as mybir
from concourse.bass_utils import run_bass_kernel_spmd
from contextlib import ExitStack

F32 = mybir.dt.float32
BF16 = mybir.dt.bfloat16
I32 = mybir.dt.int32
U32 = mybir.dt.uint32
AF = mybir.ActivationFunctionType
ALU = mybir.AluOpType
AX = mybir.AxisListType

ENGS = ("pe", "act", "dve", "pool", "sp")
D = 2048
NKC = 16
EPS = 1e-6
NE = 64
DEXP = 1408
NFC = 11
TOPK = 6
PI = math.pi


def _freeze(fn):
    if fn is None or fn.__closure__ is None:
        return fn
    cells = []
    for c in fn.__closure__:
        try:
            cells.append(types.CellType(c.cell_contents))
        except ValueError:
            cells.append(c)
    return types.FunctionType(fn.__code__, fn.__globals__, fn.__name__, fn.__defaults__, tuple(cells))


class Prog:
    def __init__(self, nc, stack):
        self.nc = nc
        self.stack = stack
        self.q = {e: [] for e in ENGS}
        self.cnt = {e: 0 for e in ENGS}
        self.waited = {e: {} for e in ENGS}
        self.res = {}
        self.dsems = {}
        self.semobj = {}
        self.gen = {e: 0 for e in ENGS}
        self.dfree = []
        self.nds = 0
        self.epoch = 0
        for e in ENGS:
            self.semobj[("e", e, 0)] = stack.enter_context(nc.semaphore("s_" + e))

    def _need(self, eng, tok, waits):
        if tok is None:
            return
        sk, val, src = tok
        if src == "pe" and eng == "pe":
            return
        if self.waited[eng].get(sk, 0) >= val:
            return
        self.waited[eng][sk] = val
        waits.append((sk, val))

    def _deps(self, eng, reads, writes):
        waits = []
        for r in reads:
            st = self.res.get(r)
            if st:
                self._need(eng, st[0], waits)
        for w in writes:
            st = self.res.get(w)
            if st:
                self._need(eng, st[0], waits)
                for t in st[1]:
                    self._need(eng, t, waits)
        return waits

    def _commit(self, tok, reads, writes):
        for r in reads:
            st = self.res.setdefault(r, [None, []])
            st[1].append(tok)
        for w in writes:
            self.res[w] = [tok, []]

    def op(self, eng, fn, reads=(), writes=()):
        waits = self._deps(eng, reads, writes)
        self.cnt[eng] += 1
        sk = ("e", eng, self.gen[eng])
        tok = (sk, self.cnt[eng], eng)
        self.q[eng].append((waits, _freeze(fn), sk, 1))
        self._commit(tok, reads, writes)
        return tok

    def _dsem(self, semkey):
        if semkey not in self.dsems:
            if self.dfree:
                ent = self.dfree.pop()
            else:
                self.nds += 1
                ent = [self.stack.enter_context(self.nc.semaphore("d%d" % self.nds)), 0]
            self.dsems[semkey] = ent
            self.semobj[semkey] = ent[0]
        self.dsems[semkey][1] += 16
        return self.dsems[semkey][1]

    def raw(self, queue, fn, reads=(), writes=(), semkey=None):
        waits = self._deps(queue, reads, writes)
        semkey = ("d", semkey, self.epoch)
        val = self._dsem(semkey)
        tok = (semkey, val, None)
        self.q[queue].append((waits, _freeze(fn), semkey, 16))
        self._commit(tok, reads, writes)
        return tok

    def dma(self, queue, out, in_, reads=(), writes=(), semkey=None, **kw):
        if semkey is None:
            semkey = writes[0] if writes else reads[0]

        def fn(e, out=out, in_=in_, kw=kw):
            return e.dma_start(out=out, in_=in_, **kw)
        return self.raw(queue, fn, reads, writes, semkey)

    def barrier(self):
        toks = [(("e", e, self.gen[e]), self.cnt[e], e) for e in ENGS if self.cnt[e] > 0]
        toks += [(k, v[1], None) for k, v in self.dsems.items() if v[1] > 0]
        for eng in ENGS:
            waits = []
            for sk, val, src in toks:
                if src == eng:
                    continue
                if self.waited[eng].get(sk, 0) >= val:
                    continue
                self.waited[eng][sk] = val
                waits.append((sk, val))
            if waits:
                self.q[eng].append((waits, None, None, 0))
        self.res = {}
        self.dfree.extend(self.dsems.values())
        self.dsems = {}
        self.epoch += 1
        for e in ENGS:
            if self.cnt[e] > 30000:
                self.gen[e] += 1
                self.cnt[e] = 0
                self.semobj[("e", e, self.gen[e])] = self.stack.enter_context(
                    self.nc.semaphore("s_%s_%d" % (e, self.gen[e])))

    def emit(self):
        nc = self.nc
        with nc.Block() as block:
            def run(engname, e):
                for waits, fn, sk, inc in self.q[engname]:
                    for wk, wv in waits:
                        e.wait_ge(self.semobj[wk], wv)
                    if fn is not None:
                        fn(e).then_inc(self.semobj[sk], inc)

            @block.tensor
            def _(e):
                run("pe", e)

            @block.scalar
            def _(e):
                run("act", e)

            @block.vector
            def _(e):
                run("dve", e)

            @block.gpsimd
            def _(e):
                run("pool", e)

            @block.sync
            def _(e):
                run("sp", e)


class Arena:
    def __init__(self, ap, nelem):
        self.ap = ap
        self.n = nelem
        self.off = 0
        self.base = 0

    def alloc(self, n, dt=BF16, parts=128):
        mul = 2 if dt in (F32, I32, U32) else 1
        sz = n * mul
        sz = (sz + 15) // 16 * 16
        assert self.off + sz <= self.n, "arena overflow %d + %d > %d" % (self.off, sz, self.n)
        a = self.ap[:, self.off:self.off + n * mul]
        self.off += sz
        if dt != BF16:
            a = a.bitcast(dt)
        if parts != 128:
            a = a[0:parts, :]
        return a

    def mark(self):
        self.base = self.off

    def reset(self):
        self.off = self.base


def build(S, C, dbg=()):
    T = S // 2
    NTA = S // 512
    NTO = T // 512
    assert T % C == 0 and C % 512 == 0
    NPS = T // C
    NX = NE + NPS
    NROW = NE * C
    nc = bass.Bass("TRN2", target_bir_lowering=False)

    declared = []

    def din(name, shape, dt=F32):
        declared.append(name)
        return nc.dram_tensor(name, list(shape), dt, kind="ExternalInput").ap()

    def dscr(name, shape, dt=BF16):
        kind = "ExternalOutput" if name in dbg else "Internal"
        return nc.dram_tensor(name, list(shape), dt, kind=kind).ap()

    pos_all = din("pos_all", [64, S], I32); pos_own = din("pos_own", [64, T], I32)
    c_col = din("c_col", [128, 16])
    cpar = din("cpar", [128, 2])
    masks_d = din("masks", [128, 256], BF16)
    ident32_d = din("ident32", [128, 128]); identb_d = din("identb", [128, 128], BF16)
    onesb_d = din("onesb", [128, 128], BF16); ustr_d = din("ustrict", [128, 128], BF16)
    rot_d = din("rot", [64, 64], BF16); iota_d = din("iota_row", [128, 64])
    invf_d = din("invf", [64, 1])
    bmod_d = din("bmod_col", [128, 96])
    vec16_d = din("vec16", [128, 7 * 16])
    convw_d = din("convw_col", [128, 64])
    qan_d = din("qan_col", [128, 4]); kvan_d = din("kvan_col", [128, 2])
    qkn_d = din("qkn_col", [128, 4])
    rbias_d = din("rbias_row", [128, 64])
    BIG = dict(x_all=[S, D], x_own=[T, D], w_mod=[D, 6 * D], w_in=[D, 9024], w_a=[16, 128, 128], w_i=[16, 128, 128],
               w_uq=[512, 3072], w_ukv=[256, 4096], w_rnn_out=[D, D], w_mla_out=[D, D], w_out=[D, D], w_router=[D, NE],
               ws_gate=[D, DEXP], ws_up=[D, DEXP],
               ws_down=[DEXP, D])

    for e_ in range(NE // 4):
        BIG["wg%02d" % e_] = [4, D, DEXP]; BIG["wu%02d" % e_] = [4, D, DEXP]; BIG["wd%02d" % e_] = [4, DEXP, D]

    class _Lazy:
        def __init__(self):
            self.c = {}

        def __getattr__(self, name):
            c = self.__dict__["c"]
            if name not in c:
                c[name] = din(name, BIG[name])
            return c[name]
    I = _Lazy()
    out_d = nc.dram_tensor("out", [T, D], F32, kind="ExternalOutput").ap()

    UTA = dscr("UTA", [D, S]); UTO = dscr("UTO", [D, T])
    HT = dscr("HT", [D, T]); KT = dscr("KT", [16, 192, S]); VTK = dscr("VTK", [S, D]); QT = dscr("QT", [16, 192, T])
    YMT = dscr("YMT", [D, T]); YRT = dscr("YRT", [D, T]); GAT = dscr("GAT", [D, T]); GBT = dscr("GBT", [D, T])
    MA = dscr("MA", [D, T]); MT = dscr("MT", [D, T])
    X1 = dscr("X1", [T, D], F32)
    XG = dscr("XG", [NROW, D]); YG = dscr("YG", [NROW, D])
    XS = dscr("XS", [T, D]); YS = dscr("YS", [T, D])
    MODC = dscr("MODC", [128, 96], F32)

    with ExitStack() as st:
        ARN = 103424
        arena_t = st.enter_context(nc.sbuf_tensor("arena", [128, ARN], BF16))
        A = Arena(arena_t, ARN)
        ps = [st.enter_context(nc.psum_tensor("ps%d" % i, [128, 512], F32)) for i in range(8)]
        P = Prog(nc, st)
        ldq = ["sp"]

        def PS(i):
            return ("ps", i)

        ident32 = A.alloc(128, F32); identb = A.alloc(128); onesb = A.alloc(128); ustr = A.alloc(128)
        rot = A.alloc(64, parts=64); masks = A.alloc(256); iota = A.alloc(64, F32); rbias = A.alloc(64, F32)
        invf = A.alloc(1, F32, parts=64); cp = A.alloc(2, F32)
        bmod = A.alloc(96, F32); vec16 = A.alloc(112, F32); convw = A.alloc(64, F32)
        qan = A.alloc(4, F32); kvan = A.alloc(2, F32); qkn = A.alloc(4, F32)
        modc = A.alloc(96, F32); A1 = A.alloc(16, F32); A2 = A.alloc(16, F32); nsp8 = A.alloc(16, F32)
        gqs = A.alloc(2, F32); ccol = A.alloc(16, F32); scs = A.alloc(16, F32)
        negpi = A.alloc(1, F32); tmpc = A.alloc(64, F32)
        for dst, src in ((ident32, ident32_d), (identb, identb_d), (onesb, onesb_d), (ustr, ustr_d), (rot, rot_d),
                         (masks, masks_d), (iota, iota_d), (rbias, rbias_d), (invf, invf_d), (cp, cpar),
                         (bmod, bmod_d), (vec16, vec16_d), (convw, convw_d), (qan, qan_d), (kvan, kvan_d),
                         (qkn, qkn_d), (ccol, c_col)):
            P.dma("sp", dst, src, writes=["const"], semkey="const")
        norm1 = vec16[:, 0:16]; norm2 = vec16[:, 16:32]; convb = vec16[:, 32:48]
        b_a = vec16[:, 48:64]; b_i = vec16[:, 64:80]; lam = vec16[:, 80:96]
        cvec = cp[:, 0:1]; omc = cp[:, 1:2]
        A.mark()

        def phase0():
            P.op("act", lambda e: e.activation(scs, ccol, AF.Silu), reads=["const"], writes=["scs"])
            P.op("pool", lambda e: e.memset(negpi, -PI), writes=["negpi"])
            wm = [A.alloc(16 * 768, F32).rearrange("p (k c) -> p k c", c=768) for _ in range(2)]
            wsrc = I.w_mod.rearrange("(k p) c -> p k c", p=128)
            for blk in range(16):
                b = wm[blk % 2]
                for k0 in range(0, 16, 4):
                    P.dma("sp", b[:, k0:k0 + 4, :], wsrc[:, k0:k0 + 4, blk * 768:(blk + 1) * 768],
                          writes=[("wm", blk % 2)], semkey=("wm", blk % 2))
                for f in range(6):
                    fa = blk * 6 + f
                    for kc in range(16):
                        P.op("pe", lambda e, b=b, f=f, fa=fa, kc=kc: e.matmul(
                            ps[0][:, fa:fa + 1], b[:, kc, f * 128:(f + 1) * 128], scs[:, kc:kc + 1],
                            start=(kc == 0), stop=(kc == 15)),
                            reads=[("wm", blk % 2), "scs"], writes=[PS(0)])
            P.op("dve", lambda e: e.tensor_tensor(modc, ps[0][:, 0:96], bmod, ALU.add),
                 reads=[PS(0), "const"], writes=["modc"])
            P.op("dve", lambda e: e.scalar_tensor_tensor(A1, modc[:, 16:32], 1.0, norm1, ALU.add, ALU.mult),
                 reads=["modc"], writes=["A1"])
            P.op("dve", lambda e: e.scalar_tensor_tensor(A2, modc[:, 64:80], 1.0, norm2, ALU.add, ALU.mult),
                 reads=["modc"], writes=["A2"])
            t0 = tmpc[:, 0:16]; t1 = tmpc[:, 16:32]; t2 = tmpc[:, 32:48]; t3 = tmpc[:, 48:64]
            P.op("dve", lambda e: e.tensor_scalar(t0, lam, -1.0, None, ALU.mult), reads=["const"], writes=["t0"])
            P.op("dve", lambda e: e.tensor_tensor(t0, t0, lam, ALU.max), reads=["const", "t0"], writes=["t0"])
            P.op("act", lambda e: e.activation(t1, t0, AF.Exp, scale=-1.0), reads=["t0"], writes=["t1"])
            P.op("dve", lambda e: e.tensor_scalar(t2, t1, 2.0, None, ALU.add), reads=["t1"], writes=["t2"])
            P.op("dve", lambda e: e.reciprocal(t2, t2), reads=["t2"], writes=["t2"])
            P.op("dve", lambda e: e.tensor_tensor(t1, t1, t2, ALU.mult), reads=["t1", "t2"], writes=["t1"])
            P.op("dve", lambda e: e.tensor_tensor(t2, t1, t1, ALU.mult), reads=["t1"], writes=["t2"])
            P.op("dve", lambda e: e.tensor_scalar(t3, t2, 1.0 / 9, 1.0 / 7, ALU.mult, ALU.add), reads=["t2"], writes=["t3"])
            for cst in (1.0 / 5, 1.0 / 3, 1.0):
                P.op("dve", lambda e: e.tensor_tensor(t3, t3, t2, ALU.mult), reads=["t3", "t2"], writes=["t3"])
                P.op("dve", lambda e, cst=cst: e.tensor_scalar(t3, t3, cst, None, ALU.add), reads=["t3"], writes=["t3"])
            P.op("dve", lambda e: e.tensor_tensor(t3, t3, t1, ALU.mult), reads=["t3", "t1"], writes=["t3"])
            P.op("dve", lambda e: e.tensor_scalar(t0, lam, -1.0, 0.0, ALU.mult, ALU.max), reads=["const"], writes=["t0"])
            P.op("dve", lambda e: e.scalar_tensor_tensor(t3, t3, 2.0, t0, ALU.mult, ALU.add), reads=["t3", "t0"], writes=["t3"])
            P.op("dve", lambda e: e.tensor_scalar(nsp8, t3, -8.0, None, ALU.mult), reads=["t3"], writes=["nsp8"])
            P.op("dve", lambda e: e.tensor_scalar(gqs, qkn[:, 0:2], 192.0 ** -0.5, None, ALU.mult),
                 reads=["const"], writes=["gqs"])
            P.dma("sp", MODC, modc, reads=["modc"], semkey="modc_out")
            P.barrier()
            A.reset()

        def norm_T(x_src, ntok, dst, Acol, shcol):
            xt = [A.alloc(D, F32) for _ in range(2)]
            junk = A.alloc(D)
            ss = A.alloc(4, F32)
            ut = [A.alloc(16 * 512).rearrange("p (c t) -> p c t", t=512) for _ in range(2)]
            n = 0
            for g in range(ntok // 512):
                u = ut[g % 2]
                for sub in range(4):
                    b = xt[n % 2]; xk = ("xt", n % 2)
                    r0 = g * 512 + sub * 128
                    P.dma("sp", b, x_src[r0:r0 + 128, :], writes=[xk])
                    sc = ss[:, 0:1]; sc2 = ss[:, 1:2]
                    P.op("act", lambda e, b=b, sc=sc: e.activation(junk, b, AF.Square, accum_out=sc),
                         reads=[xk], writes=["junk", "ss"])
                    P.op("act", lambda e, sc=sc, sc2=sc2: e.activation(sc2, sc, AF.Sqrt, scale=1.0 / D, bias=EPS),
                         reads=["ss"], writes=["ss2"])
                    P.op("dve", lambda e, sc2=sc2: e.reciprocal(sc2, sc2), reads=["ss2"], writes=["ss2"])
                    P.op("pool", lambda e, b=b, sc2=sc2: e.tensor_scalar(b, b, sc2, 1.0, ALU.mult, ALU.mult),
                         reads=[xk, "ss2"], writes=[xk])
                    for q4 in range(4):
                        pi = (n * 4 + q4) % 4
                        for j in range(4):
                            fc = q4 * 4 + j
                            P.op("pe", lambda e, b=b, fc=fc, j=j, pi=pi: e.transpose(
                                ps[pi][:, j * 128:(j + 1) * 128], b[:, fc * 128:(fc + 1) * 128], ident32),
                                reads=[xk, "const"], writes=[PS(pi)])
                        for j in range(4):
                            fc = q4 * 4 + j
                            P.op("act", lambda e, u=u, fc=fc, j=j, pi=pi, sub=sub: e.activation(
                                u[:, fc, sub * 128:(sub + 1) * 128], ps[pi][:, j * 128:(j + 1) * 128], AF.Identity,
                                bias=shcol[:, fc:fc + 1], scale=Acol[:, fc:fc + 1]),
                                reads=[PS(pi), "modc", "A1", "A2"], writes=[("ut", g % 2)])
                    n += 1
                P.dma("act", dst.rearrange("(c p) t -> p c t", p=128)[:, :, g * 512:(g + 1) * 512], u,
                      reads=[("ut", g % 2)], semkey=("uts", g % 2))
            P.barrier()
            A.reset()

        wpar = [0]

        def load_w(wsrc, ncols, nk=NKC):
            wt = A.alloc(nk * ncols).rearrange("p (k c) -> p k c", c=ncols)
            key = ("w", wpar[0]); wpar[0] += 1
            src = wsrc.rearrange("(k p) c -> p k c", p=128)
            cw_ = min(ncols, 1024)
            step = max(1, 4096 // cw_)
            for c0 in range(0, ncols, cw_):
                c1 = min(ncols, c0 + cw_)
                for k0 in range(0, nk, step):
                    k1 = min(nk, k0 + step)
                    P.dma("pool", wt[:, k0:k1, c0:c1], src[:, k0:k1, c0:c1], writes=[key], semkey=key)
            return wt, key

        def fm_pass(wt, wkey, ncols, in_scr, ntok, epi, psbanks, xin_bufs, nk=NKC, fc0=0):
            pend = None
            n = 0
            for tt in range(ntok // 512):
                xb = xin_bufs[tt % 2]; xk = ("xin", tt % 2)
                P.dma("sp", xb[:, 0:nk, :], in_scr.rearrange("(k p) t -> p k t", p=128)[:, :, tt * 512:(tt + 1) * 512],
                      writes=[xk])
                for fc in range(ncols // 128):
                    pi = psbanks[n % len(psbanks)]; n += 1
                    for k in range(nk):
                        P.op("pe", lambda e, pi=pi, k=k, fc=fc, xb=xb: e.matmul(
                            ps[pi][:], wt[:, k, fc * 128:(fc + 1) * 128], xb[:, k, :], start=(k == 0), stop=(k == nk - 1)),
                            reads=[wkey, xk], writes=[PS(pi)])
                    if pend is not None:
                        pend()
                    pend = epi(fc0 + fc, tt, ps[pi], PS(pi))
            if pend is not None:
                pend()

        def phase_rnn():
            xin = [A.alloc(16 * 512).rearrange("p (k t) -> p k t", t=512) for _ in range(2)]
            wab = A.alloc(16 * 128).rearrange("p (h j) -> p h j", j=128)
            wib = A.alloc(16 * 128).rearrange("p (h j) -> p h j", j=128)
            P.dma("pool", wab, I.w_a.rearrange("h i j -> i h j"), writes=["wab"])
            P.dma("pool", wib, I.w_i.rearrange("h i j -> i h j"), writes=["wib"])
            xr = A.alloc(8 * 516, F32).rearrange("p (c t) -> p c t", t=516)
            carry = A.alloc(8, F32)
            xc = [A.alloc(512, F32) for _ in range(2)]
            xcb = [A.alloc(512) for _ in range(2)]
            rr = [A.alloc(512, F32) for _ in range(2)]
            ii = [A.alloc(512, F32) for _ in range(2)]
            aa = [A.alloc(512, F32) for _ in range(2)]
            bb = [A.alloc(512, F32) for _ in range(2)]
            hh = [A.alloc(512, F32) for _ in range(2)]
            tq = [A.alloc(256, F32) for _ in range(2)]
            hout = [A.alloc(8 * 256).rearrange("p (c t) -> p c t", t=256) for _ in range(2)]
            for half in range(2):
                wt, wkey = load_w(I.w_in[:, half * 1024:(half + 1) * 1024], 1024)
                P.op("pool", lambda e: e.memset(xr, 0.0), writes=["xr"] + [("xr", c) for c in range(8)])
                P.op("pool", lambda e: e.memset(carry, 0.0), writes=[("carry", c) for c in range(8)])
                cnt = [0]

                def epi(fc, tt, pst, pk, half=half, cnt=cnt):
                    gfc = half * 8 + fc
                    i = cnt[0] % 2; cnt[0] += 1
                    X = xr[:, fc, :]
                    P.op("act", lambda e: e.copy(X[:, 4:516], pst[:]), reads=[pk], writes=[("xr", fc)])
                    c0 = xc[i]; k0 = ("xc", i)
                    P.op("dve", lambda e: e.tensor_scalar(c0, X[:, 1:513], convw[:, gfc:gfc + 1], convb[:, gfc:gfc + 1],
                                                          ALU.mult, ALU.add), reads=[("xr", fc), "const"], writes=[k0])
                    for j in range(1, 4):
                        P.op("dve", lambda e, j=j: e.scalar_tensor_tensor(
                            c0, X[:, 1 + j:513 + j], convw[:, j * 16 + gfc:j * 16 + gfc + 1], c0, ALU.mult, ALU.add),
                            reads=[("xr", fc), k0], writes=[k0])
                    P.op("pool", lambda e: e.tensor_copy(X[:, 0:4], X[:, 512:516]), reads=[("xr", fc)], writes=[("xr", fc)])
                    P.op("act", lambda e: e.copy(xcb[i], c0), reads=[k0], writes=[("xcb", i)])

                    def stage2():
                        pr = 4 + i * 2; pq = 5 + i * 2
                        P.op("pe", lambda e: e.matmul(ps[pr][:], wab[:, gfc, :], xcb[i], start=True, stop=True),
                             reads=["wab", ("xcb", i)], writes=[PS(pr)])
                        P.op("pe", lambda e: e.matmul(ps[pq][:], wib[:, gfc, :], xcb[i], start=True, stop=True),
                             reads=["wib", ("xcb", i)], writes=[PS(pq)])
                        P.op("act", lambda e: e.activation(rr[i], ps[pr][:], AF.Sigmoid, bias=b_a[:, gfc:gfc + 1]),
                             reads=[PS(pr), "const"], writes=[("rr", i)])
                        P.op("act", lambda e: e.activation(ii[i], ps[pq][:], AF.Sigmoid, bias=b_i[:, gfc:gfc + 1]),
                             reads=[PS(pq), "const"], writes=[("ii", i)])
                        P.op("act", lambda e: e.activation(aa[i], rr[i], AF.Exp, scale=nsp8[:, gfc:gfc + 1]),
                             reads=[("rr", i), "nsp8"], writes=[("aa", i)])
                        P.op("pool", lambda e: e.tensor_tensor(rr[i], aa[i], aa[i], ALU.mult),
                             reads=[("aa", i), ("rr", i)], writes=[("rr", i)])
                        P.op("act", lambda e: e.activation(rr[i], rr[i], AF.Sqrt, scale=-1.0, bias=1.0),
                             reads=[("rr", i)], writes=[("rr", i)])
                        P.op("pool", lambda e: e.tensor_tensor(ii[i], ii[i], c0, ALU.mult),
                             reads=[("ii", i), k0], writes=[("ii", i)])
                        P.op("dve", lambda e: e.tensor_tensor(bb[i], rr[i], ii[i], ALU.mult),
                             reads=[("rr", i), ("ii", i)], writes=[("bb", i)])
                        P.op("dve", lambda e: e.tensor_tensor_scan(hh[i], aa[i], bb[i], carry[:, fc:fc + 1], ALU.mult, ALU.add),
                             reads=[("aa", i), ("bb", i), ("carry", fc)], writes=[("hh", i)])
                        P.op("pool", lambda e: e.tensor_copy(carry[:, fc:fc + 1], hh[i][:, 511:512]),
                             reads=[("hh", i)], writes=[("carry", fc)])
                        h4 = hh[i].rearrange("p (j two q) -> p j two q", two=2, q=128)
                        t4 = tq[i].rearrange("p (j q) -> p j q", q=128)
                        ho = hout[tt % 2]
                        P.op("dve", lambda e: e.tensor_scalar(t4, h4[:, :, 0, :], omc, None, ALU.mult),
                             reads=[("hh", i), "const"], writes=[("tq", i)])
                        P.op("dve", lambda e: e.scalar_tensor_tensor(
                            ho[:, fc, :].rearrange("p (j q) -> p j q", q=128), h4[:, :, 1, :], cvec, t4, ALU.mult, ALU.add),
                            reads=[("hh", i), ("tq", i), "const"], writes=[("hout", tt % 2)])
                        if fc == 7:
                            P.dma("act", HT.rearrange("(c p) t -> p c t", p=128)[:, half * 8:(half + 1) * 8, tt * 256:(tt + 1) * 256],
                                  ho, reads=[("hout", tt % 2)], semkey=("houts", tt % 2))
                    return stage2
                fm_pass(wt, wkey, 1024, UTA, S, epi, [0, 1, 2, 3], xin)
            P.barrier()
            A.reset()

        def rope_tables(pos_src, t0, posi, ang, kf, cs, sn, tag):
            P.dma("sp", posi, pos_src[:, t0:t0 + 512], writes=[tag + "posi"])
            P.op("dve", lambda e: e.tensor_copy(ang, posi), reads=[tag + "posi"], writes=[tag + "ang"])
            P.op("dve", lambda e: e.tensor_scalar(ang, ang, invf, None, ALU.mult), reads=[tag + "ang", "const"], writes=[tag + "ang"])
            ki = posi
            P.op("dve", lambda e: e.tensor_scalar(kf, ang, 1.0 / (2 * PI), None, ALU.mult), reads=[tag + "ang"], writes=[tag + "kf"])
            P.op("dve", lambda e: e.tensor_copy(ki, kf), reads=[tag + "kf"], writes=[tag + "posi"])
            P.op("dve", lambda e: e.tensor_copy(kf, ki), reads=[tag + "posi"], writes=[tag + "kf"])
            P.op("dve", lambda e: e.scalar_tensor_tensor(ang, kf, -2 * PI, ang, ALU.mult, ALU.add),
                 reads=[tag + "kf", tag + "ang"], writes=[tag + "ang"])
            for dst, shift in ((sn, 0.0), (cs, PI / 2)):
                P.op("dve", lambda e, shift=shift: e.tensor_scalar(kf, ang, shift, None, ALU.add),
                     reads=[tag + "ang", tag + "kf"], writes=[tag + "kf"])
                for _ in range(2):
                    P.op("dve", lambda e, dst=dst: e.tensor_scalar(dst, kf, PI, -2 * PI, ALU.is_gt, ALU.mult),
                         reads=[tag + "kf"], writes=[tag + "cs"])
                    P.op("dve", lambda e, dst=dst: e.tensor_tensor(kf, kf, dst, ALU.add),
                         reads=[tag + "kf", tag + "cs"], writes=[tag + "kf"])
                P.op("dve", lambda e, dst=dst: e.tensor_scalar(dst, kf, -PI, 2 * PI, ALU.is_lt, ALU.mult),
                     reads=[tag + "kf"], writes=[tag + "cs"])
                P.op("dve", lambda e, dst=dst: e.tensor_tensor(kf, kf, dst, ALU.add),
                     reads=[tag + "kf", tag + "cs"], writes=[tag + "kf"])
                P.op("act", lambda e, dst=dst: e.activation(dst, kf, AF.Sin), reads=[tag + "kf"], writes=[tag + "cs"])

        def rstd_bcast(dst, pst, pk, n, tagw):
            P.op("act", lambda e: e.activation(dst, pst[:], AF.Sqrt, scale=1.0 / n, bias=EPS), reads=[pk], writes=[tagw])
            P.op("dve", lambda e: e.reciprocal(dst, dst), reads=[tagw], writes=[tagw])

        def phase_kv():
            xin = [A.alloc(16 * 512).rearrange("p (k t) -> p k t", t=512) for _ in range(2)]
            wkv, wkvk = load_w(I.w_in[:, 4608:4928], 320)
            wuk = A.alloc(2 * 2048).rearrange("p (c h d) -> p c h d", c=2, d=128)
            wuv = A.alloc(2 * 2048).rearrange("p (c h d) -> p c h d", c=2, d=128)
            src = I.w_ukv.rearrange("(c p) (h two d) -> p c h two d", p=128, two=2, d=128)
            for c in range(2):
                P.dma("pool", wuk[:, c, :, :], src[:, c, :, 0, :], writes=["wuk"])
                P.dma("pool", wuv[:, c, :, :], src[:, c, :, 1, :], writes=["wuv"])
            ckv32 = A.alloc(2 * 512, F32).rearrange("p (c t) -> p c t", t=512)
            sqb = A.alloc(2 * 512).rearrange("p (c t) -> p c t", t=512)
            ckvn = A.alloc(2 * 512).rearrange("p (c t) -> p c t", t=512)
            rs = A.alloc(512, F32)
            kr32 = A.alloc(512, F32, parts=64); krsq = A.alloc(512, parts=64); krg32 = A.alloc(512, F32, parts=64)
            krgb = A.alloc(512, parts=64); kro = A.alloc(512, F32, parts=64); tmp64 = A.alloc(512, F32, parts=64)
            posi = A.alloc(512, I32, parts=64); ang = A.alloc(512, F32, parts=64); kf = A.alloc(512, F32, parts=64)
            cs = A.alloc(512, F32, parts=64); sn = A.alloc(512, F32, parts=64)
            kn32 = [A.alloc(512, F32) for _ in range(2)]
            knsq = [A.alloc(512) for _ in range(2)]
            rsh = [A.alloc(512, F32) for _ in range(2)]
            kon = [A.alloc(16 * 512).rearrange("p (h t) -> p h t", t=512) for _ in range(2)]
            kor = [A.alloc(16 * 512, parts=64).rearrange("p (h t) -> p h t", t=512) for _ in range(2)]
            vt = [A.alloc(D) for _ in range(2)]
            xT = UTA.rearrange("(k p) t -> p k t", p=128)
            nv = 0
            for tt in range(NTA):
                xb = xin[tt % 2]; xk = ("xin", tt % 2)
                P.dma("sp", xb, xT[:, :, tt * 512:(tt + 1) * 512], writes=[xk])
                rope_tables(pos_all, tt * 512, posi, ang, kf, cs, sn, "k")
                for fc in range(2):
                    for k in range(16):
                        P.op("pe", lambda e, fc=fc, k=k, xb=xb: e.matmul(ps[fc][:], wkv[:, k, fc * 128:(fc + 1) * 128], xb[:, k, :],
                                                                         start=(k == 0), stop=(k == 15)),
                             reads=[wkvk, xk], writes=[PS(fc)])
                    P.op("act", lambda e, fc=fc: e.copy(ckv32[:, fc, :], ps[fc][:]), reads=[PS(fc)], writes=[("ckv32", fc)])
                    P.op("act", lambda e, fc=fc: e.activation(sqb[:, fc, :], ps[fc][:], AF.Square), reads=[PS(fc)], writes=[("sqb", fc)])
                for k in range(16):
                    P.op("pe", lambda e, k=k, xb=xb: e.matmul(ps[2][0:64, :], wkv[:, k, 256:320], xb[:, k, :], start=(k == 0), stop=(k == 15)),
                         reads=[wkvk, xk], writes=[PS(2)])
                P.op("act", lambda e: e.copy(kr32, ps[2][0:64, :]), reads=[PS(2)], writes=["kr32"])
                P.op("act", lambda e: e.activation(krsq, ps[2][0:64, :], AF.Square), reads=[PS(2)], writes=["krsq"])
                for fc in range(2):
                    P.op("pe", lambda e, fc=fc: e.matmul(ps[3][:], onesb, sqb[:, fc, :], start=(fc == 0), stop=(fc == 1)),
                         reads=["const", ("sqb", fc)], writes=[PS(3)])
                rstd_bcast(rs, ps[3], PS(3), 256, "rs")
                for fc in range(2):
                    P.op("dve", lambda e, fc=fc: e.scalar_tensor_tensor(ckvn[:, fc, :], ckv32[:, fc, :], kvan[:, fc:fc + 1], rs, ALU.mult, ALU.mult),
                         reads=[("ckv32", fc), "rs", "const"], writes=[("ckvn", fc)])
                P.op("dve", lambda e: e.tensor_scalar(krg32, kr32, qkn[0:64, 3:4], None, ALU.mult), reads=["kr32", "const"], writes=["krg32"])
                P.op("act", lambda e: e.copy(krgb, krg32), reads=["krg32"], writes=["krgb"])
                P.op("pe", lambda e: e.matmul(ps[2][0:64, :], rot, krgb, start=True, stop=True), reads=["const", "krgb"], writes=[PS(2)])
                P.op("dve", lambda e: e.tensor_tensor(kro, krg32, cs, ALU.mult), reads=["krg32", "kcs"], writes=["kro"])
                P.op("dve", lambda e: e.tensor_tensor(tmp64, ps[2][0:64, :], sn, ALU.mult), reads=[PS(2), "kcs"], writes=["tmp64"])
                P.op("dve", lambda e: e.tensor_tensor(kro, kro, tmp64, ALU.add), reads=["kro", "tmp64"], writes=["kro"])
                KN = kon[tt % 2]; KR = kor[tt % 2]
                for h in range(16):
                    i = h % 2
                    pa = 4 + i; pb = 6 + i
                    for c in range(2):
                        P.op("pe", lambda e, h=h, c=c, pa=pa: e.matmul(ps[pa][:], wuk[:, c, h, :], ckvn[:, c, :], start=(c == 0), stop=(c == 1)),
                             reads=["wuk", ("ckvn", 0), ("ckvn", 1)], writes=[PS(pa)])
                    P.op("act", lambda e, i=i, pa=pa: e.copy(kn32[i], ps[pa][:]), reads=[PS(pa)], writes=[("kn32", i)])
                    P.op("act", lambda e, i=i, pa=pa: e.activation(knsq[i], ps[pa][:], AF.Square), reads=[PS(pa)], writes=[("knsq", i)])
                    P.op("pe", lambda e, i=i, pb=pb: e.matmul(ps[pb][:], onesb, knsq[i], start=True, stop=False),
                         reads=["const", ("knsq", i)], writes=[PS(pb)])
                    P.op("pe", lambda e, pb=pb: e.matmul(ps[pb][:], onesb[0:64, :], krsq, start=False, stop=True),
                         reads=["const", "krsq"], writes=[PS(pb)])
                    rstd_bcast(rsh[i], ps[pb], PS(pb), 192, ("rsh", i))
                    P.op("dve", lambda e, h=h, i=i: e.scalar_tensor_tensor(KN[:, h, :], kn32[i], qkn[:, 2:3], rsh[i], ALU.mult, ALU.mult),
                         reads=[("kn32", i), ("rsh", i), "const"], writes=[("kon", tt % 2)])
                    P.op("pool", lambda e, h=h, i=i: e.tensor_tensor(KR[:, h, :], kro, rsh[i][0:64, :], ALU.mult),
                         reads=["kro", ("rsh", i)], writes=[("kor", tt % 2)])
                P.dma("act", KT.rearrange("h p t -> p h t")[0:128, :, tt * 512:(tt + 1) * 512], KN, reads=[("kon", tt % 2)],
                      semkey=("kons", tt % 2))
                P.dma("act", KT.rearrange("h p t -> p h t")[128:192, :, tt * 512:(tt + 1) * 512], KR, reads=[("kor", tt % 2)],
                      semkey=("kors", tt % 2))
                for sub in range(4):
                    vb = vt[nv % 2]; vk = ("vt", nv % 2); nv += 1
                    for hc in range(4):
                        pi = hc
                        for c in range(2):
                            P.op("pe", lambda e, c=c, hc=hc, sub=sub, pi=pi: e.matmul(
                                ps[pi][:], ckvn[:, c, sub * 128:(sub + 1) * 128],
                                wuv[:, c, hc * 4:(hc + 1) * 4, :].rearrange("p h d -> p (h d)"), start=(c == 0), stop=(c == 1)),
                                reads=["wuv", ("ckvn", 0), ("ckvn", 1)], writes=[PS(pi)])
                        eng = "act" if hc % 2 == 0 else "dve"
                        if eng == "act":
                            P.op("act", lambda e, vb=vb, hc=hc, pi=pi: e.copy(vb[:, hc * 512:(hc + 1) * 512], ps[pi][:]),
                                 reads=[PS(pi)], writes=[vk])
                        else:
                            P.op("dve", lambda e, vb=vb, hc=hc, pi=pi: e.tensor_copy(vb[:, hc * 512:(hc + 1) * 512], ps[pi][:]),
                                 reads=[PS(pi)], writes=[vk])
                    r0 = tt * 512 + sub * 128
                    P.dma("act", VTK[r0:r0 + 128, :], vb, reads=[vk], semkey=("vts", (nv - 1) % 2))
            P.barrier()
            A.reset()

        def phase_q():
            xin = [A.alloc(16 * 512).rearrange("p (k t) -> p k t", t=512) for _ in range(2)]
            wq, wqk = load_w(I.w_in[:, 4096:4608], 512)
            wuq, wuqk = load_w(I.w_uq, 3072, nk=4)
            cq32 = A.alloc(4 * 512, F32).rearrange("p (c t) -> p c t", t=512)
            sqb = A.alloc(4 * 512).rearrange("p (c t) -> p c t", t=512)
            cqn = A.alloc(4 * 512).rearrange("p (c t) -> p c t", t=512)
            rs = A.alloc(512, F32)
            posi = A.alloc(512, I32, parts=64); ang = A.alloc(512, F32, parts=64); kf = A.alloc(512, F32, parts=64)
            cs = A.alloc(512, F32, parts=64); sn = A.alloc(512, F32, parts=64)
            qn32 = [A.alloc(512, F32) for _ in range(2)]
            qnsq = [A.alloc(512) for _ in range(2)]
            qr32 = [A.alloc(512, F32, parts=64) for _ in range(2)]
            qrsq = [A.alloc(512, parts=64) for _ in range(2)]
            qrgb = [A.alloc(512, parts=64) for _ in range(2)]
            qro = [A.alloc(512, F32, parts=64) for _ in range(2)]
            tmp64 = [A.alloc(512, F32, parts=64) for _ in range(2)]
            rsh = [A.alloc(512, F32) for _ in range(2)]
            qon = [A.alloc(16 * 512).rearrange("p (h t) -> p h t", t=512) for _ in range(2)]
            qor = [A.alloc(16 * 512, parts=64).rearrange("p (h t) -> p h t", t=512) for _ in range(2)]
            xT = UTO.rearrange("(k p) t -> p k t", p=128)
            for tt in range(NTO):
                xb = xin[tt % 2]; xk = ("xin", tt % 2)
                P.dma("sp", xb, xT[:, :, tt * 512:(tt + 1) * 512], writes=[xk])
                rope_tables(pos_own, tt * 512, posi, ang, kf, cs, sn, "q")
                for fc in range(4):
                    for k in range(16):
                        P.op("pe", lambda e, fc=fc, k=k, xb=xb: e.matmul(ps[fc][:], wq[:, k, fc * 128:(fc + 1) * 128], xb[:, k, :],
                                                                         start=(k == 0), stop=(k == 15)),
                             reads=[wqk, xk], writes=[PS(fc)])
                    P.op("act", lambda e, fc=fc: e.copy(cq32[:, fc, :], ps[fc][:]), reads=[PS(fc)], writes=[("cq32", fc)])
                    P.op("act", lambda e, fc=fc: e.activation(sqb[:, fc, :], ps[fc][:], AF.Square), reads=[PS(fc)], writes=[("sqb", fc)])
                for fc in range(4):
                    P.op("pe", lambda e, fc=fc: e.matmul(ps[4][:], onesb, sqb[:, fc, :], start=(fc == 0), stop=(fc == 3)),
                         reads=["const", ("sqb", fc)], writes=[PS(4)])
                rstd_bcast(rs, ps[4], PS(4), 512, "rs")
                for fc in range(4):
                    P.op("dve", lambda e, fc=fc: e.scalar_tensor_tensor(cqn[:, fc, :], cq32[:, fc, :], qan[:, fc:fc + 1], rs, ALU.mult, ALU.mult),
                         reads=[("cq32", fc), "rs", "const"], writes=["cqn"])
                QN = qon[tt % 2]; QR = qor[tt % 2]
                for h in range(16):
                    i = h % 2
                    pa = 0 + i; pb = 2 + i; pc = 4 + i; pd = 6 + i
                    for c in range(4):
                        P.op("pe", lambda e, h=h, c=c, pa=pa: e.matmul(ps[pa][:], wuq[:, c, h * 192:h * 192 + 128], cqn[:, c, :],
                                                                       start=(c == 0), stop=(c == 3)),
                             reads=[wuqk, "cqn"], writes=[PS(pa)])
                    for c in range(4):
                        P.op("pe", lambda e, h=h, c=c, pb=pb: e.matmul(ps[pb][0:64, :], wuq[:, c, h * 192 + 128:h * 192 + 192], cqn[:, c, :],
                                                                       start=(c == 0), stop=(c == 3)),
                             reads=[wuqk, "cqn"], writes=[PS(pb)])
                    P.op("act", lambda e, i=i, pa=pa: e.copy(qn32[i], ps[pa][:]), reads=[PS(pa)], writes=[("qn32", i)])
                    P.op("act", lambda e, i=i, pa=pa: e.activation(qnsq[i], ps[pa][:], AF.Square), reads=[PS(pa)], writes=[("qnsq", i)])
                    P.op("act", lambda e, i=i, pb=pb: e.activation(qr32[i], ps[pb][0:64, :], AF.Identity, scale=gqs[0:64, 1:2]),
                         reads=[PS(pb), "gqs"], writes=[("qr32", i)])
                    P.op("act", lambda e, i=i, pb=pb: e.activation(qrsq[i], ps[pb][0:64, :], AF.Square), reads=[PS(pb)], writes=[("qrsq", i)])
                    P.op("pe", lambda e, i=i, pc=pc: e.matmul(ps[pc][:], onesb, qnsq[i], start=True, stop=False),
                         reads=["const", ("qnsq", i)], writes=[PS(pc)])
                    P.op("pe", lambda e, i=i, pc=pc: e.matmul(ps[pc][:], onesb[0:64, :], qrsq[i], start=False, stop=True),
                         reads=["const", ("qrsq", i)], writes=[PS(pc)])
                    rstd_bcast(rsh[i], ps[pc], PS(pc), 192, ("rsh", i))
                    P.op("dve", lambda e, h=h, i=i: e.scalar_tensor_tensor(QN[:, h, :], qn32[i], gqs[:, 0:1], rsh[i], ALU.mult, ALU.mult),
                         reads=[("qn32", i), ("rsh", i), "gqs"], writes=[("qon", tt % 2)])
                    P.op("pool", lambda e, i=i: e.tensor_copy(qrgb[i], qr32[i]), reads=[("qr32", i)], writes=[("qrgb", i)])
                    P.op("pe", lambda e, i=i, pd=pd: e.matmul(ps[pd][0:64, :], rot, qrgb[i], start=True, stop=True),
                         reads=["const", ("qrgb", i)], writes=[PS(pd)])
                    P.op("pool", lambda e, i=i: e.tensor_tensor(qro[i], qr32[i], cs, ALU.mult), reads=[("qr32", i), "qcs"], writes=[("qro", i)])
                    P.op("dve", lambda e, i=i, pd=pd: e.tensor_tensor(tmp64[i], ps[pd][0:64, :], sn, ALU.mult), reads=[PS(pd), "qcs"], writes=[("tmp64", i)])
                    P.op("pool", lambda e, i=i: e.tensor_tensor(qro[i], qro[i], tmp64[i], ALU.add), reads=[("qro", i), ("tmp64", i)], writes=[("qro", i)])
                    P.op("pool", lambda e, h=h, i=i: e.tensor_tensor(QR[:, h, :], qro[i], rsh[i][0:64, :], ALU.mult),
                         reads=[("qro", i), ("rsh", i)], writes=[("qor", tt % 2)])
                P.dma("act", QT.rearrange("h p t -> p h t")[0:128, :, tt * 512:(tt + 1) * 512], QN, reads=[("qon", tt % 2)],
                      semkey=("qons", tt % 2))
                P.dma("act", QT.rearrange("h p t -> p h t")[128:192, :, tt * 512:(tt + 1) * 512], QR, reads=[("qor", tt % 2)],
                      semkey=("qors", tt % 2))
            P.barrier()
            A.reset()

        def phase_attn():
            NKB = S // 128
            ktn = [A.alloc(S) for _ in range(2)]
            ktr = [A.alloc(S, parts=64) for _ in range(2)]
            vh = [A.alloc(NKB * 128).rearrange("p (k d) -> p k d", d=128) for _ in range(2)]
            qtn = [A.alloc(T) for _ in range(2)]
            qtr = [A.alloc(T, parts=64) for _ in range(2)]
            pt = [A.alloc(512) for _ in range(3)]
            rl = A.alloc(512, F32)
            yo = [A.alloc(512) for _ in range(2)]
            lacc = [A.alloc(512, F32) for _ in range(2)]
            ones32 = A.alloc(128, F32)
            P.op("pool", lambda e: e.memset(ones32, 1.0), writes=["ones32"])
            def prefetch(h):
                hb = h % 2; hk = ("head", hb)
                P.dma("sp", ktn[hb], KT[h, 0:128, :], writes=[hk], semkey=hk)
                P.dma("sp", ktr[hb], KT[h, 128:192, :], writes=[hk], semkey=hk)
                P.dma("sp", vh[hb], VTK.rearrange("(k p) f -> p k f", p=128)[:, :, h * 128:(h + 1) * 128], writes=[hk], semkey=hk)
                P.dma("sp", qtn[hb], QT[h, 0:128, :], writes=[hk], semkey=hk)
                P.dma("sp", qtr[hb], QT[h, 128:192, :], writes=[hk], semkey=hk)

            steps = []
            nch = 0
            for h in range(16):
                for qc in range(NTO):
                    nkb = 8 * qc + 8
                    for kb in range(nkb):
                        steps.append((h, qc, kb, nkb, nch))
                    nch += 1

            def emit_S(i):
                h, qc, kb, nkb, nch_ = steps[i]
                hb = h % 2; hk = ("head", hb)
                jlo = max(0, kb // 2 - 4 * qc)
                Nv = (4 - jlo) * 128
                q0 = qc * 512 + jlo * 128
                si = i % 3
                P.op("pe", lambda e: e.matmul(ps[si][:, 0:Nv], ktn[hb][:, kb * 128:(kb + 1) * 128], qtn[hb][:, q0:q0 + Nv], start=True, stop=False),
                     reads=[hk], writes=[PS(si)])
                P.op("pe", lambda e: e.matmul(ps[si][:, 0:Nv], ktr[hb][:, kb * 128:(kb + 1) * 128], qtr[hb][:, q0:q0 + Nv], start=False, stop=True),
                     reads=[hk], writes=[PS(si)])

            prefetch(0)
            emit_S(0)
            for i, (h, qc, kb, nkb, nch_) in enumerate(steps):
                hb = h % 2; hk = ("head", hb)
                if qc == 0 and kb == 0 and h + 1 < 16:
                    prefetch(h + 1)
                if i + 1 < len(steps):
                    emit_S(i + 1)
                po = 3 + (nch_ % 2) * 2; pl = po + 1
                jlo = max(0, kb // 2 - 4 * qc)
                Nv = (4 - jlo) * 128
                si = i % 3
                pb = pt[si]; pkey = ("pt", si)
                if jlo > 0:
                    P.op("pool", lambda e: e.memset(pb[:, 0:jlo * 128], 0.0), writes=[pkey])
                P.op("act", lambda e: e.activation(pb[:, jlo * 128:512], ps[si][:, 0:Nv], AF.Exp), reads=[PS(si)], writes=[pkey])
                if kb // 2 >= 4 * qc:
                    par = kb % 2
                    P.op("pool", lambda e: e.tensor_tensor(
                        pb[:, jlo * 128:(jlo + 1) * 128], pb[:, jlo * 128:(jlo + 1) * 128], masks[:, par * 128:(par + 1) * 128], ALU.mult),
                        reads=[pkey, "const"], writes=[pkey])
                P.op("pe", lambda e: e.matmul(ps[po][:], vh[hb][:, kb, :], pb, start=(kb == 0), stop=(kb == nkb - 1)),
                     reads=[hk, pkey], writes=[PS(po)])
                la = lacc[nch_ % 2]; lk = ("lacc", nch_ % 2)
                if kb == 0:
                    P.op("dve", lambda e: e.tensor_copy(la, pb), reads=[pkey], writes=[lk])
                else:
                    P.op("dve", lambda e: e.tensor_tensor(la, la, pb, ALU.add), reads=[pkey, lk], writes=[lk])
                if kb == nkb - 1:
                    P.op("pe", lambda e: e.matmul(ps[pl][:], ones32, la, start=True, stop=True),
                         reads=["ones32", lk], writes=[PS(pl)])
                    P.op("dve", lambda e: e.reciprocal(rl, ps[pl][:]), reads=[PS(pl)], writes=["rl"])
                    yb = yo[nch_ % 2]; yk = ("yo", nch_ % 2)
                    P.op("dve", lambda e: e.tensor_tensor(yb, ps[po][:], rl, ALU.mult), reads=[PS(po), "rl"], writes=[yk])
                    P.dma("act", YMT[h * 128:(h + 1) * 128, qc * 512:(qc + 1) * 512], yb, reads=[yk], semkey=("yos", nch_ % 2))
            P.barrier()
            A.reset()

        def simple_pass(wsrc, in_scr, mode, aux1, aux2, dst):
            xin = [A.alloc(16 * 512).rearrange("p (k t) -> p k t", t=512) for _ in range(2)]
            a1 = [A.alloc(8 * 512).rearrange("p (c t) -> p c t", t=512) for _ in range(2)] if aux1 is not None else None
            a2 = [A.alloc(8 * 512).rearrange("p (c t) -> p c t", t=512) for _ in range(2)] if aux2 is not None else None
            ob = [A.alloc(8 * 512).rearrange("p (c t) -> p c t", t=512) for _ in range(2)]
            t32 = [A.alloc(512, F32) for _ in range(2)]
            u32 = [A.alloc(512, F32) for _ in range(2)]
            wts = [load_w(wsrc[:, half * 1024:(half + 1) * 1024], 1024) for half in range(2)]
            for half in range(2):
                wt, wkey = wts[half]
                cnt = [0]
                last_tt = [-1]

                def epi(fc, tt, pst, pk, half=half, cnt=cnt, last_tt=last_tt):
                    i = cnt[0] % 2; cnt[0] += 1
                    o = ob[tt % 2]; ok = ("ob", tt % 2)
                    if tt != last_tt[0]:
                        last_tt[0] = tt
                        for aux, ab, nm in ((aux1, a1, "a1"), (aux2, a2, "a2")):
                            if aux is not None:
                                P.dma("sp", ab[tt % 2], aux.rearrange("(c p) t -> p c t", p=128)[:, half * 8:(half + 1) * 8, tt * 512:(tt + 1) * 512],
                                      writes=[(nm, tt % 2)])
                    if mode == "gelu_mul":
                        t = t32[i]; tk = ("t32", i); u = u32[i]; uk = ("u32", i)
                        P.op("act", lambda e: e.activation(t, pst[:], AF.Square), reads=[pk], writes=[tk])
                        P.op("dve", lambda e: e.tensor_scalar(t, t, 0.044715, 1.0, ALU.mult, ALU.add), reads=[tk], writes=[tk])
                        P.op("dve", lambda e: e.tensor_tensor(t, t, pst[:], ALU.mult), reads=[tk, pk], writes=[tk])
                        P.op("act", lambda e: e.activation(t, t, AF.Sigmoid, scale=2.0 * math.sqrt(2.0 / PI)), reads=[tk], writes=[tk])
                        P.op("dve", lambda e: e.tensor_tensor(u, t, pst[:], ALU.mult), reads=[tk, pk], writes=[uk])
                        P.op("pool", lambda e: e.tensor_tensor(o[:, fc, :], u, a1[tt % 2][:, fc, :], ALU.mult),
                             reads=[uk, ("a1", tt % 2)], writes=[ok])
                    elif mode == "sigmoid":
                        P.op("act", lambda e: e.activation(o[:, fc, :], pst[:], AF.Sigmoid), reads=[pk], writes=[ok])
                    elif mode == "mul":
                        P.op("dve", lambda e: e.tensor_tensor(o[:, fc, :], pst[:], a1[tt % 2][:, fc, :], ALU.mult),
                             reads=[pk, ("a1", tt % 2)], writes=[ok])
                    elif mode == "mul_add":
                        u = u32[i]; uk = ("u32", i)
                        P.op("dve", lambda e: e.tensor_tensor(u, pst[:], a1[tt % 2][:, fc, :], ALU.mult),
                             reads=[pk, ("a1", tt % 2)], writes=[uk])
                        P.op("pool", lambda e: e.tensor_tensor(o[:, fc, :], u, a2[tt % 2][:, fc, :], ALU.add),
                             reads=[uk, ("a2", tt % 2)], writes=[ok])
                    if fc == 7:
                        P.dma("act", dst.rearrange("(c p) t -> p c t", p=128)[:, half * 8:(half + 1) * 8, tt * 512:(tt + 1) * 512], o,
                              reads=[ok], semkey=("obs", tt % 2))
                    return None
                fm_pass(wt, wkey, 1024, in_scr, T, epi, [0, 1, 2, 3], xin)
            P.barrier()
            A.reset()

        def make_row(dst, col, tagr):
            bt = A.alloc(128, F32)
            for j in range(16):
                P.op("dve", lambda e, j=j: e.tensor_copy(bt, col[:, j:j + 1].to_broadcast([128, 128])),
                     reads=["modc", "A2"], writes=["bt"])
                P.op("pe", lambda e: e.transpose(ps[7][:, 0:128], bt, ident32), reads=["bt", "const"], writes=[PS(7)])
                P.op("act", lambda e, j=j: e.copy(dst[:, j * 128:(j + 1) * 128], ps[7][:, 0:128]), reads=[PS(7)], writes=[tagr])

        NT128 = T // 128

        regc = {}

        def phase_out_route(rwk, gidx):
            g1row = A.alloc(D, F32); a2row = A.alloc(D, F32); sh2row = A.alloc(D, F32)
            make_row(g1row, modc[:, 32:48], "g1row")
            make_row(a2row, A2, "a2row")
            make_row(sh2row, modc[:, 48:64], "sh2row")
            wo = [load_w(I.w_out[:, half * 1024:(half + 1) * 1024], 1024) for half in range(2)]
            wr32 = A.alloc(16 * 64, F32).rearrange("p (k e) -> p k e", e=64)
            P.dma("sp", wr32, I.w_router.rearrange("(k p) e -> p k e", p=128), writes=["wr32"])
            xin = [A.alloc(16 * 512).rearrange("p (k t) -> p k t", t=512) for _ in range(2)]
            xo = [A.alloc(D, F32) for _ in range(2)]
            vtk = A.alloc(D, F32)
            vbf = [A.alloc(D) for _ in range(2)]
            junk = A.alloc(D)
            vT = A.alloc(16 * 128, F32).rearrange("p (k t) -> p k t", t=128)
            sm = A.alloc(64, F32)
            sc_ = A.alloc(64, F32); sel = A.alloc(64, F32); msk = A.alloc(64, F32); rw = A.alloc(64, F32)
            mskb = A.alloc(64); pos = A.alloc(64, F32); carry = A.alloc(64, F32); oh = A.alloc(64, F32)
            mx8 = A.alloc(8, F32); ix8 = A.alloc(8, U32); ixf = A.alloc(8, F32); posk = A.alloc(8, F32)
            dst_f = A.alloc(8, F32); ovf = A.alloc(8, F32); dsti = [A.alloc(8, I32) for _ in range(2)]
            P.op("pool", lambda e: e.memset(carry, 0.0), writes=["carry"])
            mT = MT.rearrange("(k p) t -> p k t", p=128)
            for tt in range(NTO):
                xb = xin[tt % 2]; xk = ("xin", tt % 2)
                P.dma("sp", xb, mT[:, :, tt * 512:(tt + 1) * 512], writes=[xk])
                for sub in range(4):
                    ti = tt * 4 + sub
                    r0 = ti * 128
                    X = xo[ti % 2]; Xk = ("xo", ti % 2)
                    P.dma("sp", X, I.x_own[r0:r0 + 128, :], writes=[Xk])
                    for dc in range(4):
                        wt, wkey = wo[dc // 2]
                        for k in range(16):
                            P.op("pe", lambda e, dc=dc, k=k, wt=wt, xb=xb, sub=sub: e.matmul(
                                ps[dc][:], xb[:, k, sub * 128:(sub + 1) * 128], wt[:, k, (dc % 2) * 512:(dc % 2 + 1) * 512],
                                start=(k == 0), stop=(k == 15)), reads=[wkey, xk], writes=[PS(dc)])
                        P.op("dve", lambda e, dc=dc: e.tensor_tensor(vtk[:, dc * 512:(dc + 1) * 512], ps[dc][:], g1row[:, dc * 512:(dc + 1) * 512], ALU.mult),
                             reads=[PS(dc), "g1row"], writes=[("vtk", dc)])
                        P.op("pool", lambda e, dc=dc, X=X: e.tensor_tensor(X[:, dc * 512:(dc + 1) * 512], X[:, dc * 512:(dc + 1) * 512], vtk[:, dc * 512:(dc + 1) * 512], ALU.add),
                             reads=[Xk, ("vtk", dc)], writes=[Xk])
                    P.dma("act", X1[r0:r0 + 128, :], X, reads=[Xk], semkey=("x1s", ti % 2))
                    ss = sm[:, 0:1]; rs_ = sm[:, 1:2]
                    P.op("act", lambda e, X=X: e.activation(junk, X, AF.Square, accum_out=ss), reads=[Xk], writes=["junk", "ss"])
                    P.op("act", lambda e: e.activation(rs_, ss, AF.Sqrt, scale=1.0 / D, bias=EPS), reads=["ss"], writes=["rs_"])
                    P.op("dve", lambda e: e.reciprocal(rs_, rs_), reads=["rs_"], writes=["rs_"])
                    P.op("dve", lambda e, X=X: e.scalar_tensor_tensor(vtk, X, rs_, a2row, ALU.mult, ALU.mult),
                         reads=[Xk, "rs_", "a2row"] + [("vtk", d) for d in range(4)], writes=[("vtk", d) for d in range(4)] + ["vtkall"])
                    P.op("pool", lambda e: e.tensor_tensor(vtk, vtk, sh2row, ALU.add), reads=["vtkall", "sh2row"], writes=["vtkall"] + [("vtk", d) for d in range(4)])
                    VB = vbf[ti % 2]; VBk = ("vbf", ti % 2)
                    P.op("act", lambda e, VB=VB: e.copy(VB, vtk), reads=["vtkall"], writes=[VBk])
                    for q4 in range(4):
                        pi = 4 + q4 % 2
                        for j in range(4):
                            fc = q4 * 4 + j
                            P.op("pe", lambda e, fc=fc, j=j, pi=pi: e.transpose(ps[pi][:, j * 128:(j + 1) * 128], vtk[:, fc * 128:(fc + 1) * 128], ident32),
                                 reads=["vtkall", "const"], writes=[PS(pi)])
                        P.op("act", lambda e, q4=q4, pi=pi: e.copy(vT[:, q4 * 4:(q4 + 1) * 4, :].rearrange("p k t -> p (k t)"), ps[pi][:]),
                             reads=[PS(pi)], writes=["vT"])
                    for k in range(16):
                        P.op("pe", lambda e, k=k: e.matmul(ps[6][:, 0:64], vT[:, k, :], wr32[:, k, :], start=(k == 0), stop=(k == 15)),
                             reads=["vT", "wr32"], writes=[PS(6)])
                    P.op("act", lambda e: e.activation(sc_, ps[6][:, 0:64], AF.Sigmoid), reads=[PS(6)], writes=["sc_"])
                    P.op("dve", lambda e: e.tensor_tensor(sel, sc_, rbias, ALU.add), reads=["sc_", "const"], writes=["sel"])
                    P.op("dve", lambda e: e.max(mx8, sel), reads=["sel"], writes=["mx8"])
                    P.op("dve", lambda e: e.max_index(ix8, mx8, sel), reads=["sel", "mx8"], writes=["ix8"])
                    P.op("dve", lambda e: e.tensor_scalar(msk, sel, mx8[:, 5:6], None, ALU.is_ge), reads=["sel", "mx8"], writes=["msk"])
                    P.op("dve", lambda e: e.tensor_tensor(rw, sc_, msk, ALU.mult), reads=["sc_", "msk"], writes=["rw"])
                    den = sm[:, 2:3]
                    P.op("dve", lambda e: e.reduce_sum(den, rw, axis=AX.X), reads=["rw"], writes=["den"])
                    P.op("dve", lambda e: e.reciprocal(den, den), reads=["den"], writes=["den"])
                    P.op("dve", lambda e: e.tensor_scalar(rw, rw, den, 2.5, ALU.mult, ALU.mult), reads=["rw", "den"], writes=["rw"])
                    P.op("act", lambda e: e.copy(mskb, msk), reads=["msk"], writes=["mskb"])
                    P.op("pe", lambda e: e.matmul(ps[7][:, 0:64], ustr, mskb, start=True, stop=True), reads=["const", "mskb"], writes=[PS(7)])
                    P.op("pe", lambda e: e.matmul(ps[7][:, 64:128], onesb, mskb, start=True, stop=True), reads=["const", "mskb"], writes=[PS(7)])
                    P.op("dve", lambda e: e.tensor_tensor(pos, ps[7][:, 0:64], carry, ALU.add), reads=[PS(7), "carry"], writes=["pos"])
                    P.op("dve", lambda e: e.tensor_tensor(carry, ps[7][:, 64:128], carry, ALU.add), reads=[PS(7), "carry"], writes=["carry"])
                    P.op("dve", lambda e: e.tensor_copy(ixf, ix8), reads=["ix8"], writes=["ixf"])
                    for k in range(TOPK):
                        P.op("dve", lambda e, k=k: e.tensor_scalar(oh, iota, ixf[:, k:k + 1], None, ALU.is_equal), reads=["const", "ixf"], writes=["oh"])
                        P.op("dve", lambda e: e.tensor_tensor(msk, oh, pos, ALU.mult), reads=["oh", "pos", "msk"], writes=["msk"])
                        P.op("dve", lambda e, k=k: e.reduce_sum(posk[:, k:k + 1], msk, axis=AX.X), reads=["msk"], writes=["posk"])
                        P.op("dve", lambda e: e.tensor_tensor(msk, oh, rw, ALU.mult), reads=["oh", "rw", "msk"], writes=["msk"])
                        P.op("dve", lambda e, k=k, ti=ti: e.reduce_sum(rwk[:, ti * 8 + k:ti * 8 + k + 1], msk, axis=AX.X), reads=["msk"], writes=["rwk"])
                    P.op("dve", lambda e: e.scalar_tensor_tensor(dst_f[:, 0:6], ixf[:, 0:6], float(C), posk[:, 0:6], ALU.mult, ALU.add),
                         reads=["ixf", "posk"], writes=["dst_f"])
                    P.op("dve", lambda e: e.tensor_scalar(ovf[:, 0:6], posk[:, 0:6], float(C), None, ALU.is_ge), reads=["posk"], writes=["ovf"])
                    P.op("dve", lambda e: e.tensor_scalar(posk[:, 0:6], ovf[:, 0:6], -1.0, 1.0, ALU.mult, ALU.add), reads=["ovf", "posk"], writes=["posk"])
                    P.op("dve", lambda e, ti=ti: e.tensor_tensor(rwk[:, ti * 8:ti * 8 + 6], rwk[:, ti * 8:ti * 8 + 6], posk[:, 0:6], ALU.mult),
                         reads=["posk", "rwk"], writes=["rwk"])
                    P.op("dve", lambda e: e.tensor_tensor(dst_f[:, 0:6], dst_f[:, 0:6], posk[:, 0:6], ALU.mult), reads=["dst_f", "posk"], writes=["dst_f"])
                    DI = dsti[ti % 2]; DIk = ("dsti", ti % 2)
                    P.op("dve", lambda e: e.scalar_tensor_tensor(posk[:, 0:6], ovf[:, 0:6], 4194304.0, dst_f[:, 0:6], ALU.mult, ALU.add),
                         reads=["ovf", "dst_f", "posk"], writes=["posk"])
                    P.op("dve", lambda e, DI=DI: e.tensor_copy(DI[:, 0:6], posk[:, 0:6]), reads=["posk"], writes=[DIk])
                    P.op("dve", lambda e, DI=DI, ti=ti: e.tensor_copy(gidx[:, ti * 8:ti * 8 + 6], dst_f[:, 0:6]), reads=["dst_f"], writes=["gidx"])
                    for k in range(TOPK):
                        def scat(e, DI=DI, VB=VB, k=k):
                            if "r" not in regc:
                                regc["r"] = e.to_reg(NROW - 1)
                            return e.indirect_dma_start(
                                out=XG, out_offset=bass.IndirectOffsetOnAxis(ap=DI[:, k:k + 1], axis=0), in_=VB, in_offset=None,
                                bounds_check=regc["r"], oob_is_err=False)
                        P.raw("pool", scat,
                            reads=[DIk, VBk], writes=[], semkey=("sc", ti % 2))
                    P.dma("act", XS[r0:r0 + 128, :], VB, reads=[VBk], semkey=("xgs", ti % 2))
            P.barrier()
            A.reset()

        def phase_experts():
            NH = C // 512
            xg = [A.alloc(4 * D).rearrange("p (b f) -> p b f", f=D) for _ in range(2)]
            xT = A.alloc(16 * C).rearrange("p (k s) -> p k s", s=C)
            wg = [A.alloc(16 * 128).rearrange("p (k f) -> p k f", f=128) for _ in range(3)]
            wu = [A.alloc(16 * 128).rearrange("p (k f) -> p k f", f=128) for _ in range(3)]
            hT = A.alloc(NFC * C).rearrange("p (f s) -> p f s", s=C)
            wd = [A.alloc(NFC * 512).rearrange("p (f d) -> p f d", d=512) for _ in range(3)]
            sg = [A.alloc(512, F32) for _ in range(2)]
            yb = [A.alloc(512) for _ in range(4)]
            nxg = 0; nw = 0; nwd = 0; nsg = 0; ny = 0; npsA = 0; ntr = 0
            for ex in range(NX):
                if ex > 0 and ex % 8 == 0:
                    P.barrier()
                if ex < NE:
                    Wg = getattr(I, "wg%02d" % (ex // 4))[ex % 4]; Wu = getattr(I, "wu%02d" % (ex // 4))[ex % 4]
                    Wd = getattr(I, "wd%02d" % (ex // 4))[ex % 4]
                    wq_, wdeps = "pool", []
                else:
                    Wg = I.ws_gate; Wu = I.ws_up; Wd = I.ws_down
                    wq_, wdeps = "pool", []
                for hh_ in range(NH):
                    g = xg[nxg % 2]; gk = ("xg", nxg % 2); nxg += 1
                    r0 = (ex if ex < NE else ex - NE) * C + hh_ * 512
                    XSRC = XG if ex < NE else XS
                    P.dma("sp", g, XSRC[r0:r0 + 512, :].rearrange("(b p) f -> p b f", p=128), writes=[gk])
                    for k in range(16):
                        pi = 6 + ntr % 2; ntr += 1
                        pbf = ps[pi][:].bitcast(BF16)
                        for b4 in range(4):
                            P.op("pe", lambda e, g=g, b4=b4, k=k, pbf=pbf: e.transpose(pbf[:, b4 * 128:(b4 + 1) * 128], g[:, b4, k * 128:(k + 1) * 128], identb),
                                 reads=[gk, "const"], writes=[PS(pi)])
                        eng = "act" if k % 2 == 0 else "dve"
                        if eng == "act":
                            P.op("act", lambda e, k=k, hh_=hh_, pbf=pbf: e.copy(xT[:, k, hh_ * 512:(hh_ + 1) * 512], pbf[:, 0:512]),
                                 reads=[PS(pi)], writes=["xT"])
                        else:
                            P.op("dve", lambda e, k=k, hh_=hh_, pbf=pbf: e.tensor_copy(xT[:, k, hh_ * 512:(hh_ + 1) * 512], pbf[:, 0:512]),
                                 reads=[PS(pi)], writes=["xT"])
                Wgv = Wg.rearrange("(k p) f -> p k f", p=128); Wuv = Wu.rearrange("(k p) f -> p k f", p=128)
                for fc in range(NFC):
                    wi = nw % 3; nw += 1
                    P.dma(wq_, wg[wi], Wgv[:, :, fc * 128:(fc + 1) * 128], reads=wdeps[0:1], writes=[("wg", wi)])
                    P.dma(wq_, wu[wi], Wuv[:, :, fc * 128:(fc + 1) * 128], reads=wdeps[1:2], writes=[("wu", wi)])
                    for hh_ in range(NH):
                        pg = (npsA % 3) * 2; pu = pg + 1; npsA += 1
                        for k in range(16):
                            P.op("pe", lambda e, wi=wi, k=k, hh_=hh_, pg=pg: e.matmul(ps[pg][:], wg[wi][:, k, :], xT[:, k, hh_ * 512:(hh_ + 1) * 512],
                                                                                    start=(k == 0), stop=(k == 15)),
                                 reads=[("wg", wi), "xT"], writes=[PS(pg)])
                        for k in range(16):
                            P.op("pe", lambda e, wi=wi, k=k, hh_=hh_, pu=pu: e.matmul(ps[pu][:], wu[wi][:, k, :], xT[:, k, hh_ * 512:(hh_ + 1) * 512],
                                                                                    start=(k == 0), stop=(k == 15)),
                                 reads=[("wu", wi), "xT"], writes=[PS(pu)])
                        s = sg[nsg % 2]; sk_ = ("sg", nsg % 2); nsg += 1
                        P.op("act", lambda e, s=s, pg=pg: e.activation(s, ps[pg][:], AF.Silu), reads=[PS(pg)], writes=[sk_])
                        P.op("dve", lambda e, s=s, pu=pu, fc=fc, hh_=hh_: e.tensor_tensor(hT[:, fc, hh_ * 512:(hh_ + 1) * 512], s, ps[pu][:], ALU.mult),
                             reads=[sk_, PS(pu)], writes=["hT"])
                Wdv = Wd.rearrange("(f p) d -> p f d", p=128)
                for dc in range(4):
                    wi = nwd % 3; nwd += 1
                    P.dma(wq_, wd[wi], Wdv[:, :, dc * 512:(dc + 1) * 512], reads=wdeps[2:3], writes=[("wd", wi)])
                    for sb in range(C // 128):
                        pi = (npsA % 3) * 2 + (sb % 2);
                        if sb % 2 == 1:
                            npsA += 1
                        for fc in range(NFC):
                            P.op("pe", lambda e, wi=wi, fc=fc, sb=sb, pi=pi: e.matmul(ps[pi][:], hT[:, fc, sb * 128:(sb + 1) * 128], wd[wi][:, fc, :],
                                                                                    start=(fc == 0), stop=(fc == NFC - 1)),
                                 reads=[("wd", wi), "hT"], writes=[PS(pi)])
                        y = yb[ny % 4]; yk = ("yb", ny % 4)
                        if ny % 2 == 0:
                            P.op("act", lambda e, y=y, pi=pi: e.copy(y, ps[pi][:]), reads=[PS(pi)], writes=[yk])
                        else:
                            P.op("dve", lambda e, y=y, pi=pi: e.tensor_copy(y, ps[pi][:]), reads=[PS(pi)], writes=[yk])
                        r0 = (ex if ex < NE else ex - NE) * C + sb * 128
                        YDST = YG if ex < NE else YS
                        P.dma("act", YDST[r0:r0 + 128, dc * 512:(dc + 1) * 512], y, reads=[yk], semkey=("ybs", ny % 4))
                        ny += 1
                    if C // 128 % 2 == 1:
                        npsA += 1
            P.barrier()
            A.reset()

        def phase_combine(rwk, gidx):
            g2row = A.alloc(D, F32)
            make_row(g2row, modc[:, 80:96], "g2row")
            yg = [A.alloc(7 * D).rearrange("p (k f) -> p k f", f=D) for _ in range(2)]
            acc = [A.alloc(D, F32) for _ in range(2)]
            x1 = [A.alloc(D, F32) for _ in range(2)]
            for ti in range(NT128):
                r0 = ti * 128
                Y = yg[ti % 2]; Yk = ("yg", ti % 2)
                for k in range(TOPK):
                    P.raw("pool", lambda e, Y=Y, k=k, ti=ti: e.indirect_dma_start(
                        out=Y[:, k, :], out_offset=None, in_=YG, in_offset=bass.IndirectOffsetOnAxis(ap=gidx[:, ti * 8 + k:ti * 8 + k + 1], axis=0)),
                        reads=["gidx"], writes=[Yk], semkey=Yk)
                P.dma("sp", Y[:, 6, :], YS[r0:r0 + 128, :], writes=[Yk], semkey=Yk)
                X = x1[ti % 2]; Xk = ("x1", ti % 2)
                P.dma("sp", X, X1[r0:r0 + 128, :], writes=[Xk])
                a = acc[ti % 2]; ak = ("acc", ti % 2)
                P.op("dve", lambda e, a=a, Y=Y, ti=ti: e.scalar_tensor_tensor(a, Y[:, 0, :], rwk[:, ti * 8:ti * 8 + 1], Y[:, 6, :], ALU.mult, ALU.add),
                     reads=[Yk, "rwk"], writes=[ak])
                for k in range(1, TOPK):
                    P.op("dve", lambda e, a=a, Y=Y, ti=ti, k=k: e.scalar_tensor_tensor(a, Y[:, k, :], rwk[:, ti * 8 + k:ti * 8 + k + 1], a, ALU.mult, ALU.add),
                         reads=[Yk, "rwk", ak], writes=[ak])
                P.op("dve", lambda e, a=a: e.tensor_tensor(a, a, g2row, ALU.mult), reads=[ak, "g2row"], writes=[ak])
                P.op("pool", lambda e, a=a, X=X: e.tensor_tensor(X, X, a, ALU.add), reads=[ak, Xk], writes=[Xk])
                P.dma("act", out_d[r0:r0 + 128, :], X, reads=[Xk], semkey=("outs", ti % 2))
            P.barrier()

        def zero_scratch():
            z = A.alloc(4 * D).rearrange("p (b f) -> p b f", f=D)
            P.op("pool", lambda e: e.memset(z, 0.0), writes=["z"])
            for r in range(0, 512, 512):
                P.dma("sp", XG[r:r + 512, :].rearrange("(b p) f -> p b f", p=128), z, reads=["z"], semkey="zx")
            P.barrier()
            A.reset()

        rwk = A.alloc(NT128 * 8, F32)
        gidx = A.alloc(NT128 * 8, I32)
        A.mark()

        phases = dbg_phases if (dbg_phases := getattr(build, "phases", None)) else None
        def want(nm):
            return phases is None or nm in phases
        if want("zero"):
            zero_scratch()
        if want("p0"):
            phase0()
        else:
            P.dma("sp", modc, MODC, writes=["modc"])
        if want("p1"):
            norm_T(I.x_all, S, UTA, A1, modc[:, 0:16])
            norm_T(I.x_own, T, UTO, A1, modc[:, 0:16])
        if want("rnn"):
            phase_rnn()
        if want("kv"):
            phase_kv()
        if want("q"):
            phase_q()
        if want("attn"):
            phase_attn()
        if want("merge"):
            simple_pass(I.w_in[:, 2048:4096], UTO, "gelu_mul", HT, None, YRT)
            simple_pass(I.w_in[:, 4928:6976], UTO, "sigmoid", None, None, GAT)
            simple_pass(I.w_in[:, 6976:9024], UTO, "sigmoid", None, None, GBT)
            simple_pass(I.w_rnn_out, YRT, "mul", GAT, None, MA)
            simple_pass(I.w_mla_out, YMT, "mul_add", GBT, MA, MT)
        if want("route"):
            phase_out_route(rwk, gidx)
        if want("experts"):
            phase_experts()
        if want("combine"):
            phase_combine(rwk, gidx)
        P.barrier()
        P.emit()
    nc._declared = declared
    return nc


def _col(v, n):
    return np.ascontiguousarray(np.asarray(v, np.float32).reshape(n, 128).T)


def make_in_maps(S, inp):
    bf = ml_dtypes.bfloat16
    x = np.asarray(inp["x"]); B = x.shape[0]
    pos = np.asarray(inp["positions"]).astype(np.int32)
    sq = lambda k: np.ascontiguousarray(np.asarray(inp[k])[0])
    ident = np.eye(128, dtype=np.float32)
    ustr = np.triu(np.ones((128, 128), np.float32), 1)
    rot = np.zeros((64, 64), np.float32)
    for m in range(32):
        rot[m + 32, m] = -1.0
        rot[m, m + 32] = 1.0
    invf = (np.float32(10000.0) ** (-np.arange(0, 64, 2, dtype=np.float32) / np.float32(64))).astype(np.float32)
    invf2 = np.concatenate([invf, invf]).reshape(64, 1).astype(np.float32)
    iota = np.broadcast_to(np.arange(64, dtype=np.float32)[None, :], (128, 64)).copy()
    vec16 = np.concatenate([_col(sq("norm1"), 16), _col(sq("norm2"), 16), _col(sq("conv_b"), 16), _col(sq("b_a"), 16),
                            _col(sq("b_i"), 16), _col(sq("lru_lambda"), 16), np.zeros((128, 16), np.float32)], axis=1)
    cw = sq("conv_w")
    convw = np.concatenate([_col(cw[j], 16) for j in range(4)], axis=1)
    qn = sq("q_norm"); kn = sq("k_norm")
    qkn = np.zeros((128, 4), np.float32)
    qkn[:, 0] = qn[:128]; qkn[:64, 1] = qn[128:]; qkn[:, 2] = kn[:128]; qkn[:64, 3] = kn[128:]
    shared = dict(
        ident32=ident, identb=ident.astype(bf), onesb=np.ones((128, 128), bf), ustrict=ustr.astype(bf),
        rot=rot.astype(bf), iota_row=iota, invf=invf2, bmod_col=_col(sq("b_mod"), 96), vec16=vec16, convw_col=convw,
        qan_col=_col(sq("q_a_norm"), 4), kvan_col=_col(sq("kv_a_norm"), 2), qkn_col=qkn,
        rbias_row=np.broadcast_to(sq("router_bias")[None, :], (128, 64)).copy(),
        w_mod=sq("w_mod"), w_in=sq("w_in"), w_a=sq("w_a"), w_i=sq("w_i"),
        w_uq=sq("w_uq").reshape(512, 3072), w_ukv=sq("w_ukv").reshape(256, 4096),
        w_rnn_out=sq("w_rnn_out"), w_mla_out=sq("w_mla_out"), w_out=sq("w_out"), w_router=sq("w_router"),
        ws_gate=sq("ws_gate"), ws_up=sq("ws_up"), ws_down=sq("ws_down"),
    )
    wg_ = np.asarray(inp["w_gate"])[0]; wu_ = np.asarray(inp["w_up"])[0]; wd_ = np.asarray(inp["w_down"])[0]
    for e_ in range(NE // 4):
        shared["wg%02d" % e_] = wg_[4 * e_:4 * e_ + 4]; shared["wu%02d" % e_] = wu_[4 * e_:4 * e_ + 4]
        shared["wd%02d" % e_] = wd_[4 * e_:4 * e_ + 4]
    maps = []
    for core in range(2 * B):
        b = core // 2; c = core % 2
        xb = np.ascontiguousarray(x[b])
        xo = np.ascontiguousarray(xb.reshape(S // 128, 128, D)[c::2].reshape(S // 2, D))
        pa = pos[b]
        po = np.ascontiguousarray(pa.reshape(S // 128, 128)[c::2].reshape(S // 2))
        q = np.arange(128)[None, :] // 64; k = np.arange(128)[:, None] // 64
        diag = (k <= q).astype(np.float32)
        if c == 0:
            m_even = diag; m_odd = np.zeros((128, 128), np.float32)
        else:
            m_even = np.ones((128, 128), np.float32); m_odd = diag
        m = dict(shared)
        m.update(

            x_all=xb, x_own=xo,
            pos_all=np.broadcast_to(pa[None, :], (64, S)).copy(), pos_own=np.broadcast_to(po[None, :], (64, S // 2)).copy(),
            c_col=_col(np.asarray(inp["c"])[b], 16),
            cpar=np.broadcast_to(np.array([[c, 1 - c]], np.float32), (128, 2)).copy(),
            masks=np.concatenate([m_even, m_odd], axis=1).astype(bf),
        )
        maps.append(m)
    return maps


_CACHE = {}


def run(inp, S, C, dbg=(), ncores=None):
    key = (S, C, tuple(dbg))
    if key not in _CACHE:
        _CACHE[key] = build(S, C, dbg)
    nc = _CACHE[key]
    maps = make_in_maps(S, inp)
    if ncores is not None:
        maps = maps[:ncores]
    maps = [{k: m[k] for k in nc._declared} for m in maps]
    res = run_bass_kernel_spmd(nc, maps, core_ids=list(range(len(maps))))
    return res


def kernel(**inputs):
    x = np.asarray(inputs["x"])
    B, S, _ = x.shape
    res = run(inputs, S, 1024)
    out = np.empty((B, S, D), np.float32)
    for core, r in enumerate(res.results):
        b = core // 2; c = core % 2
        out[b].reshape(S // 128, 128, D)[c::2] = r["out"].reshape(S // 256, 128, D)
    return out
```

```python
import math
import types
import numpy as np
import ml_dtypes
import concourse.bass as bass
import concourse.mybir as mybir
from concourse.bass_utils import run_bass_kernel_spmd
from contextlib import ExitStack

F32 = mybir.dt.float32
BF16 = mybir.dt.bfloat16
I32 = mybir.dt.int32
U32 = mybir.dt.uint32
AF = mybir.ActivationFunctionType
ALU = mybir.AluOpType
AX = mybir.AxisListType

ENGS = ("pe", "act", "dve", "pool", "sp")
D = 2048
NKC = 16
EPS = 1e-6
NE = 64
DEXP = 1408
NFC = 11
TOPK = 6
PI = math.pi


def _freeze(fn):
    if fn is None or fn.__closure__ is None:
        return fn
    cells = []
    for c in fn.__closure__:
        try:
            cells.append(types.CellType(c.cell_contents))
        except ValueError:
            cells.append(c)
    return types.FunctionType(fn.__code__, fn.__globals__, fn.__name__, fn.__defaults__, tuple(cells))


class Prog:
    def __init__(self, nc, stack):
        self.nc = nc
        self.stack = stack
        self.q = {e: [] for e in ENGS}
        self.cnt = {e: 0 for e in ENGS}
        self.waited = {e: {} for e in ENGS}
        self.res = {}
        self.dsems = {}
        self.semobj = {}
        self.gen = {e: 0 for e in ENGS}
        self.dfree = []
        self.nds = 0
        self.epoch = 0
        for e in ENGS:
            self.semobj[("e", e, 0)] = stack.enter_context(nc.semaphore("s_" + e))

    def _need(self, eng, tok, waits):
        if tok is None:
            return
        sk, val, src = tok
        if src == "pe" and eng == "pe":
            return
        if self.waited[eng].get(sk, 0) >= val:
            return
        self.waited[eng][sk] = val
        waits.append((sk, val))

    def _deps(self, eng, reads, writes):
        waits = []
        for r in reads:
            st = self.res.get(r)
            if st:
                self._need(eng, st[0], waits)
        for w in writes:
            st = self.res.get(w)
            if st:
                self._need(eng, st[0], waits)
                for t in st[1]:
                    self._need(eng, t, waits)
        return waits

    def _commit(self, tok, reads, writes):
        for r in reads:
            st = self.res.setdefault(r, [None, []])
            st[1].append(tok)
        for w in writes:
            self.res[w] = [tok, []]

    def op(self, eng, fn, reads=(), writes=()):
        waits = self._deps(eng, reads, writes)
        self.cnt[eng] += 1
        sk = ("e", eng, self.gen[eng])
        tok = (sk, self.cnt[eng], eng)
        self.q[eng].append((waits, _freeze(fn), sk, 1))
        self._commit(tok, reads, writes)
        return tok

    def _dsem(self, semkey):
        if semkey not in self.dsems:
            if self.dfree:
                ent = self.dfree.pop()
            else:
                self.nds += 1
                ent = [self.stack.enter_context(self.nc.semaphore("d%d" % self.nds)), 0]
            self.dsems[semkey] = ent
            self.semobj[semkey] = ent[0]
        self.dsems[semkey][1] += 16
        return self.dsems[semkey][1]

    def raw(self, queue, fn, reads=(), writes=(), semkey=None):
        waits = self._deps(queue, reads, writes)
        semkey = ("d", semkey, self.epoch)
        val = self._dsem(semkey)
        tok = (semkey, val, None)
        self.q[queue].append((waits, _freeze(fn), semkey, 16))
        self._commit(tok, reads, writes)
        return tok

    def dma(self, queue, out, in_, reads=(), writes=(), semkey=None, **kw):
        if semkey is None:
            semkey = writes[0] if writes else reads[0]

        def fn(e, out=out, in_=in_, kw=kw):
            return e.dma_start(out=out, in_=in_, **kw)
        return self.raw(queue, fn, reads, writes, semkey)

    def barrier(self):
        toks = [(("e", e, self.gen[e]), self.cnt[e], e) for e in ENGS if self.cnt[e] > 0]
        toks += [(k, v[1], None) for k, v in self.dsems.items() if v[1] > 0]
        for eng in ENGS:
            waits = []
            for sk, val, src in toks:
                if src == eng:
                    continue
                if self.waited[eng].get(sk, 0) >= val:
                    continue
                self.waited[eng][sk] = val
                waits.append((sk, val))
            if waits:
                self.q[eng].append((waits, None, None, 0))
        self.res = {}
        self.dfree.extend(self.dsems.values())
        self.dsems = {}
        self.epoch += 1
        for e in ENGS:
            if self.cnt[e] > 30000:
                self.gen[e] += 1
                self.cnt[e] = 0
                self.semobj[("e", e, self.gen[e])] = self.stack.enter_context(
                    self.nc.semaphore("s_%s_%d" % (e, self.gen[e])))

    def emit(self):
        nc = self.nc
        with nc.Block() as block:
            def run(engname, e):
                for waits, fn, sk, inc in self.q[engname]:
                    for wk, wv in waits:
                        e.wait_ge(self.semobj[wk], wv)
                    if fn is not None:
                        fn(e).then_inc(self.semobj[sk], inc)

            @block.tensor
            def _(e):
                run("pe", e)

            @block.scalar
            def _(e):
                run("act", e)

            @block.vector
            def _(e):
                run("dve", e)

            @block.gpsimd
            def _(e):
                run("pool", e)

            @block.sync
            def _(e):
                run("sp", e)


class Arena:
    def __init__(self, ap, nelem):
        self.ap = ap
        self.n = nelem
        self.off = 0
        self.base = 0

    def alloc(self, n, dt=BF16, parts=128):
        mul = 2 if dt in (F32, I32, U32) else 1
        sz = n * mul
        sz = (sz + 15) // 16 * 16
        assert self.off + sz <= self.n, "arena overflow %d + %d > %d" % (self.off, sz, self.n)
        a = self.ap[:, self.off:self.off + n * mul]
        self.off += sz
        if dt != BF16:
            a = a.bitcast(dt)
        if parts != 128:
            a = a[0:parts, :]
        return a

    def mark(self):
        self.base = self.off

    def reset(self):
        self.off = self.base


def build(S, C, dbg=()):
    T = S // 2
    NTA = S // 512
    NTO = T // 512
    assert T % C == 0 and C % 512 == 0
    NPS = T // C
    NX = NE + NPS
    NROW = NE * C
    nc = bass.Bass("TRN2", target_bir_lowering=False)

    declared = []

    def din(name, shape, dt=F32):
        declared.append(name)
        return nc.dram_tensor(name, list(shape), dt, kind="ExternalInput").ap()

    def dscr(name, shape, dt=BF16):
        kind = "ExternalOutput" if name in dbg else "Internal"
        return nc.dram_tensor(name, list(shape), dt, kind=kind).ap()

    pos_all = din("pos_all", [64, S], I32); pos_own = din("pos_own", [64, T], I32)
    c_col = din("c_col", [128, 16])
    cpar = din("cpar", [128, 2])
    masks_d = din("masks", [128, 256], BF16)
    ident32_d = din("ident32", [128, 128]); identb_d = din("identb", [128, 128], BF16)
    onesb_d = din("onesb", [128, 128], BF16); ustr_d = din("ustrict", [128, 128], BF16)
    rot_d = din("rot", [64, 64], BF16); iota_d = din("iota_row", [128, 64])
    invf_d = din("invf", [64, 1])
    bmod_d = din("bmod_col", [128, 96])
    vec16_d = din("vec16", [128, 7 * 16])
    convw_d = din("convw_col", [128, 64])
    qan_d = din("qan_col", [128, 4]); kvan_d = din("kvan_col", [128, 2])
    qkn_d = din("qkn_col", [128, 4])
    rbias_d = din("rbias_row", [128, 64])
    BIG = dict(x_all=[S, D], x_own=[T, D], w_mod=[D, 6 * D], w_in=[D, 9024], w_a=[16, 128, 128], w_i=[16, 128, 128],
               w_uq=[512, 3072], w_ukv=[256, 4096], w_rnn_out=[D, D], w_mla_out=[D, D], w_out=[D, D], w_router=[D, NE],
               ws_gate=[D, DEXP], ws_up=[D, DEXP],
               ws_down=[DEXP, D])

    for e_ in range(NE // 4):
        BIG["wg%02d" % e_] = [4, D, DEXP]; BIG["wu%02d" % e_] = [4, D, DEXP]; BIG["wd%02d" % e_] = [4, DEXP, D]

    class _Lazy:
        def __init__(self):
            self.c = {}

        def __getattr__(self, name):
            c = self.__dict__["c"]
            if name not in c:
                c[name] = din(name, BIG[name])
            return c[name]
    I = _Lazy()
    out_d = nc.dram_tensor("out", [T, D], F32, kind="ExternalOutput").ap()

    UTA = dscr("UTA", [D, S]); UTO = dscr("UTO", [D, T])
    HT = dscr("HT", [D, T]); KT = dscr("KT", [16, 192, S]); VTK = dscr("VTK", [S, D]); QT = dscr("QT", [16, 192, T])
    YMT = dscr("YMT", [D, T]); YRT = dscr("YRT", [D, T]); GAT = dscr("GAT", [D, T]); GBT = dscr("GBT", [D, T])
    MA = dscr("MA", [D, T]); MT = dscr("MT", [D, T])
    X1 = dscr("X1", [T, D], F32)
    XG = dscr("XG", [NROW, D]); YG = dscr("YG", [NROW, D])
    XS = dscr("XS", [T, D]); YS = dscr("YS", [T, D])
    MODC = dscr("MODC", [128, 96], F32)

    with ExitStack() as st:
        ARN = 103424
        arena_t = st.enter_context(nc.sbuf_tensor("arena", [128, ARN], BF16))
        A = Arena(arena_t, ARN)
        ps = [st.enter_context(nc.psum_tensor("ps%d" % i, [128, 512], F32)) for i in range(8)]
        P = Prog(nc, st)
        ldq = ["sp"]

        def PS(i):
            return ("ps", i)

        ident32 = A.alloc(128, F32); identb = A.alloc(128); onesb = A.alloc(128); ustr = A.alloc(128)
        rot = A.alloc(64, parts=64); masks = A.alloc(256); iota = A.alloc(64, F32); rbias = A.alloc(64, F32)
        invf = A.alloc(1, F32, parts=64); cp = A.alloc(2, F32)
        bmod = A.alloc(96, F32); vec16 = A.alloc(112, F32); convw = A.alloc(64, F32)
        qan = A.alloc(4, F32); kvan = A.alloc(2, F32); qkn = A.alloc(4, F32)
        modc = A.alloc(96, F32); A1 = A.alloc(16, F32); A2 = A.alloc(16, F32); nsp8 = A.alloc(16, F32)
        gqs = A.alloc(2, F32); ccol = A.alloc(16, F32); scs = A.alloc(16, F32)
        negpi = A.alloc(1, F32); tmpc = A.alloc(64, F32)
        for dst, src in ((ident32, ident32_d), (identb, identb_d), (onesb, onesb_d), (ustr, ustr_d), (rot, rot_d),
                         (masks, masks_d), (iota, iota_d), (rbias, rbias_d), (invf, invf_d), (cp, cpar),
                         (bmod, bmod_d), (vec16, vec16_d), (convw, convw_d), (qan, qan_d), (kvan, kvan_d),
                         (qkn, qkn_d), (ccol, c_col)):
            P.dma("sp", dst, src, writes=["const"], semkey="const")
        norm1 = vec16[:, 0:16]; norm2 = vec16[:, 16:32]; convb = vec16[:, 32:48]
        b_a = vec16[:, 48:64]; b_i = vec16[:, 64:80]; lam = vec16[:, 80:96]
        cvec = cp[:, 0:1]; omc = cp[:, 1:2]
        A.mark()

        def phase0():
            P.op("act", lambda e: e.activation(scs, ccol, AF.Silu), reads=["const"], writes=["scs"])
            P.op("pool", lambda e: e.memset(negpi, -PI), writes=["negpi"])
            wm = [A.alloc(16 * 768, F32).rearrange("p (k c) -> p k c", c=768) for _ in range(2)]
            wsrc = I.w_mod.rearrange("(k p) c -> p k c", p=128)
            for blk in range(16):
                b = wm[blk % 2]
                for k0 in range(0, 16, 4):
                    P.dma("sp", b[:, k0:k0 + 4, :], wsrc[:, k0:k0 + 4, blk * 768:(blk + 1) * 768],
                          writes=[("wm", blk % 2)], semkey=("wm", blk % 2))
                for f in range(6):
                    fa = blk * 6 + f
                    for kc in range(16):
                        P.op("pe", lambda e, b=b, f=f, fa=fa, kc=kc: e.matmul(
                            ps[0][:, fa:fa + 1], b[:, kc, f * 128:(f + 1) * 128], scs[:, kc:kc + 1],
                            start=(kc == 0), stop=(kc == 15)),
                            reads=[("wm", blk % 2), "scs"], writes=[PS(0)])
            P.op("dve", lambda e: e.tensor_tensor(modc, ps[0][:, 0:96], bmod, ALU.add),
                 reads=[PS(0), "const"], writes=["modc"])
            P.op("dve", lambda e: e.scalar_tensor_tensor(A1, modc[:, 16:32], 1.0, norm1, ALU.add, ALU.mult),
                 reads=["modc"], writes=["A1"])
            P.op("dve", lambda e: e.scalar_tensor_tensor(A2, modc[:, 64:80], 1.0, norm2, ALU.add, ALU.mult),
                 reads=["modc"], writes=["A2"])
            t0 = tmpc[:, 0:16]; t1 = tmpc[:, 16:32]; t2 = tmpc[:, 32:48]; t3 = tmpc[:, 48:64]
            P.op("dve", lambda e: e.tensor_scalar(t0, lam, -1.0, None, ALU.mult), reads=["const"], writes=["t0"])
            P.op("dve", lambda e: e.tensor_tensor(t0, t0, lam, ALU.max), reads=["const", "t0"], writes=["t0"])
            P.op("act", lambda e: e.activation(t1, t0, AF.Exp, scale=-1.0), reads=["t0"], writes=["t1"])
            P.op("dve", lambda e: e.tensor_scalar(t2, t1, 2.0, None, ALU.add), reads=["t1"], writes=["t2"])
            P.op("dve", lambda e: e.reciprocal(t2, t2), reads=["t2"], writes=["t2"])
            P.op("dve", lambda e: e.tensor_tensor(t1, t1, t2, ALU.mult), reads=["t1", "t2"], writes=["t1"])
            P.op("dve", lambda e: e.tensor_tensor(t2, t1, t1, ALU.mult), reads=["t1"], writes=["t2"])
            P.op("dve", lambda e: e.tensor_scalar(t3, t2, 1.0 / 9, 1.0 / 7, ALU.mult, ALU.add), reads=["t2"], writes=["t3"])
            for cst in (1.0 / 5, 1.0 / 3, 1.0):
                P.op("dve", lambda e: e.tensor_tensor(t3, t3, t2, ALU.mult), reads=["t3", "t2"], writes=["t3"])
                P.op("dve", lambda e, cst=cst: e.tensor_scalar(t3, t3, cst, None, ALU.add), reads=["t3"], writes=["t3"])
            P.op("dve", lambda e: e.tensor_tensor(t3, t3, t1, ALU.mult), reads=["t3", "t1"], writes=["t3"])
            P.op("dve", lambda e: e.tensor_scalar(t0, lam, -1.0, 0.0, ALU.mult, ALU.max), reads=["const"], writes=["t0"])
            P.op("dve", lambda e: e.scalar_tensor_tensor(t3, t3, 2.0, t0, ALU.mult, ALU.add), reads=["t3", "t0"], writes=["t3"])
            P.op("dve", lambda e: e.tensor_scalar(nsp8, t3, -8.0, None, ALU.mult), reads=["t3"], writes=["nsp8"])
            P.op("dve", lambda e: e.tensor_scalar(gqs, qkn[:, 0:2], 192.0 ** -0.5, None, ALU.mult),
                 reads=["const"], writes=["gqs"])
            P.dma("sp", MODC, modc, reads=["modc"], semkey="modc_out")
            P.barrier()
            A.reset()

        def norm_T(x_src, ntok, dst, Acol, shcol):
            xt = [A.alloc(D, F32) for _ in range(2)]
            junk = A.alloc(D)
            ss = A.alloc(4, F32)
            ut = [A.alloc(16 * 512).rearrange("p (c t) -> p c t", t=512) for _ in range(2)]
            n = 0
            for g in range(ntok // 512):
                u = ut[g % 2]
                for sub in range(4):
                    b = xt[n % 2]; xk = ("xt", n % 2)
                    r0 = g * 512 + sub * 128
                    P.dma("sp", b, x_src[r0:r0 + 128, :], writes=[xk])
                    sc = ss[:, 0:1]; sc2 = ss[:, 1:2]
                    P.op("act", lambda e, b=b, sc=sc: e.activation(junk, b, AF.Square, accum_out=sc),
                         reads=[xk], writes=["junk", "ss"])
                    P.op("act", lambda e, sc=sc, sc2=sc2: e.activation(sc2, sc, AF.Sqrt, scale=1.0 / D, bias=EPS),
                         reads=["ss"], writes=["ss2"])
                    P.op("dve", lambda e, sc2=sc2: e.reciprocal(sc2, sc2), reads=["ss2"], writes=["ss2"])
                    P.op("pool", lambda e, b=b, sc2=sc2: e.tensor_scalar(b, b, sc2, 1.0, ALU.mult, ALU.mult),
                         reads=[xk, "ss2"], writes=[xk])
                    for q4 in range(4):
                        pi = (n * 4 + q4) % 4
                        for j in range(4):
                            fc = q4 * 4 + j
                            P.op("pe", lambda e, b=b, fc=fc, j=j, pi=pi: e.transpose(
                                ps[pi][:, j * 128:(j + 1) * 128], b[:, fc * 128:(fc + 1) * 128], ident32),
                                reads=[xk, "const"], writes=[PS(pi)])
                        for j in range(4):
                            fc = q4 * 4 + j
                            P.op("act", lambda e, u=u, fc=fc, j=j, pi=pi, sub=sub: e.activation(
                                u[:, fc, sub * 128:(sub + 1) * 128], ps[pi][:, j * 128:(j + 1) * 128], AF.Identity,
                                bias=shcol[:, fc:fc + 1], scale=Acol[:, fc:fc + 1]),
                                reads=[PS(pi), "modc", "A1", "A2"], writes=[("ut", g % 2)])
                    n += 1
                P.dma("act", dst.rearrange("(c p) t -> p c t", p=128)[:, :, g * 512:(g + 1) * 512], u,
                      reads=[("ut", g % 2)], semkey=("uts", g % 2))
            P.barrier()
            A.reset()

        wpar = [0]

        def load_w(wsrc, ncols, nk=NKC):
            wt = A.alloc(nk * ncols).rearrange("p (k c) -> p k c", c=ncols)
            key = ("w", wpar[0]); wpar[0] += 1
            src = wsrc.rearrange("(k p) c -> p k c", p=128)
            cw_ = min(ncols, 1024)
            step = max(1, 4096 // cw_)
            for c0 in range(0, ncols, cw_):
                c1 = min(ncols, c0 + cw_)
                for k0 in range(0, nk, step):
                    k1 = min(nk, k0 + step)
                    P.dma("pool", wt[:, k0:k1, c0:c1], src[:, k0:k1, c0:c1], writes=[key], semkey=key)
            return wt, key

        def fm_pass(wt, wkey, ncols, in_scr, ntok, epi, psbanks, xin_bufs, nk=NKC, fc0=0):
            pend = None
            n = 0
            for tt in range(ntok // 512):
                xb = xin_bufs[tt % 2]; xk = ("xin", tt % 2)
                P.dma("sp", xb[:, 0:nk, :], in_scr.rearrange("(k p) t -> p k t", p=128)[:, :, tt * 512:(tt + 1) * 512],
                      writes=[xk])
                for fc in range(ncols // 128):
                    pi = psbanks[n % len(psbanks)]; n += 1
                    for k in range(nk):
                        P.op("pe", lambda e, pi=pi, k=k, fc=fc, xb=xb: e.matmul(
                            ps[pi][:], wt[:, k, fc * 128:(fc + 1) * 128], xb[:, k, :], start=(k == 0), stop=(k == nk - 1)),
                            reads=[wkey, xk], writes=[PS(pi)])
                    if pend is not None:
                        pend()
                    pend = epi(fc0 + fc, tt, ps[pi], PS(pi))
            if pend is not None:
                pend()

        def phase_rnn():
            xin = [A.alloc(16 * 512).rearrange("p (k t) -> p k t", t=512) for _ in range(2)]
            wab = A.alloc(16 * 128).rearrange("p (h j) -> p h j", j=128)
            wib = A.alloc(16 * 128).rearrange("p (h j) -> p h j", j=128)
            P.dma("pool", wab, I.w_a.rearrange("h i j -> i h j"), writes=["wab"])
            P.dma("pool", wib, I.w_i.rearrange("h i j -> i h j"), writes=["wib"])
            xr = A.alloc(8 * 516, F32).rearrange("p (c t) -> p c t", t=516)
            carry = A.alloc(8, F32)
            xc = [A.alloc(512, F32) for _ in range(2)]
            xcb = [A.alloc(512) for _ in range(2)]
            rr = [A.alloc(512, F32) for _ in range(2)]
            ii = [A.alloc(512, F32) for _ in range(2)]
            aa = [A.alloc(512, F32) for _ in range(2)]
            bb = [A.alloc(512, F32) for _ in range(2)]
            hh = [A.alloc(512, F32) for _ in range(2)]
            tq = [A.alloc(256, F32) for _ in range(2)]
            hout = [A.alloc(8 * 256).rearrange("p (c t) -> p c t", t=256) for _ in range(2)]
            for half in range(2):
                wt, wkey = load_w(I.w_in[:, half * 1024:(half + 1) * 1024], 1024)
                P.op("pool", lambda e: e.memset(xr, 0.0), writes=["xr"] + [("xr", c) for c in range(8)])
                P.op("pool", lambda e: e.memset(carry, 0.0), writes=[("carry", c) for c in range(8)])
                cnt = [0]

                def epi(fc, tt, pst, pk, half=half, cnt=cnt):
                    gfc = half * 8 + fc
                    i = cnt[0] % 2; cnt[0] += 1
                    X = xr[:, fc, :]
                    P.op("act", lambda e: e.copy(X[:, 4:516], pst[:]), reads=[pk], writes=[("xr", fc)])
                    c0 = xc[i]; k0 = ("xc", i)
                    P.op("dve", lambda e: e.tensor_scalar(c0, X[:, 1:513], convw[:, gfc:gfc + 1], convb[:, gfc:gfc + 1],
                                                          ALU.mult, ALU.add), reads=[("xr", fc), "const"], writes=[k0])
                    for j in range(1, 4):
                        P.op("dve", lambda e, j=j: e.scalar_tensor_tensor(
                            c0, X[:, 1 + j:513 + j], convw[:, j * 16 + gfc:j * 16 + gfc + 1], c0, ALU.mult, ALU.add),
                            reads=[("xr", fc), k0], writes=[k0])
                    P.op("pool", lambda e: e.tensor_copy(X[:, 0:4], X[:, 512:516]), reads=[("xr", fc)], writes=[("xr", fc)])
                    P.op("act", lambda e: e.copy(xcb[i], c0), reads=[k0], writes=[("xcb", i)])

                    def stage2():
                        pr = 4 + i * 2; pq = 5 + i * 2
                        P.op("pe", lambda e: e.matmul(ps[pr][:], wab[:, gfc, :], xcb[i], start=True, stop=True),
                             reads=["wab", ("xcb", i)], writes=[PS(pr)])
                        P.op("pe", lambda e: e.matmul(ps[pq][:], wib[:, gfc, :], xcb[i], start=True, stop=True),
                             reads=["wib", ("xcb", i)], writes=[PS(pq)])
                        P.op("act", lambda e: e.activation(rr[i], ps[pr][:], AF.Sigmoid, bias=b_a[:, gfc:gfc + 1]),
                             reads=[PS(pr), "const"], writes=[("rr", i)])
                        P.op("act", lambda e: e.activation(ii[i], ps[pq][:], AF.Sigmoid, bias=b_i[:, gfc:gfc + 1]),
                             reads=[PS(pq), "const"], writes=[("ii", i)])
                        P.op("act", lambda e: e.activation(aa[i], rr[i], AF.Exp, scale=nsp8[:, gfc:gfc + 1]),
                             reads=[("rr", i), "nsp8"], writes=[("aa", i)])
                        P.op("pool", lambda e: e.tensor_tensor(rr[i], aa[i], aa[i], ALU.mult),
                             reads=[("aa", i), ("rr", i)], writes=[("rr", i)])
                        P.op("act", lambda e: e.activation(rr[i], rr[i], AF.Sqrt, scale=-1.0, bias=1.0),
                             reads=[("rr", i)], writes=[("rr", i)])
                        P.op("pool", lambda e: e.tensor_tensor(ii[i], ii[i], c0, ALU.mult),
                             reads=[("ii", i), k0], writes=[("ii", i)])
                        P.op("dve", lambda e: e.tensor_tensor(bb[i], rr[i], ii[i], ALU.mult),
                             reads=[("rr", i), ("ii", i)], writes=[("bb", i)])
                        P.op("dve", lambda e: e.tensor_tensor_scan(hh[i], aa[i], bb[i], carry[:, fc:fc + 1], ALU.mult, ALU.add),
                             reads=[("aa", i), ("bb", i), ("carry", fc)], writes=[("hh", i)])
                        P.op("pool", lambda e: e.tensor_copy(carry[:, fc:fc + 1], hh[i][:, 511:512]),
                             reads=[("hh", i)], writes=[("carry", fc)])
                        h4 = hh[i].rearrange("p (j two q) -> p j two q", two=2, q=128)
                        t4 = tq[i].rearrange("p (j q) -> p j q", q=128)
                        ho = hout[tt % 2]
                        P.op("dve", lambda e: e.tensor_scalar(t4, h4[:, :, 0, :], omc, None, ALU.mult),
                             reads=[("hh", i), "const"], writes=[("tq", i)])
                        P.op("dve", lambda e: e.scalar_tensor_tensor(
                            ho[:, fc, :].rearrange("p (j q) -> p j q", q=128), h4[:, :, 1, :], cvec, t4, ALU.mult, ALU.add),
                            reads=[("hh", i), ("tq", i), "const"], writes=[("hout", tt % 2)])
                        if fc == 7:
                            P.dma("act", HT.rearrange("(c p) t -> p c t", p=128)[:, half * 8:(half + 1) * 8, tt * 256:(tt + 1) * 256],
                                  ho, reads=[("hout", tt % 2)], semkey=("houts", tt % 2))
                    return stage2
                fm_pass(wt, wkey, 1024, UTA, S, epi, [0, 1, 2, 3], xin)
            P.barrier()
            A.reset()

        def rope_tables(pos_src, t0, posi, ang, kf, cs, sn, tag):
            P.dma("sp", posi, pos_src[:, t0:t0 + 512], writes=[tag + "posi"])
            P.op("dve", lambda e: e.tensor_copy(ang, posi), reads=[tag + "posi"], writes=[tag + "ang"])
            P.op("dve", lambda e: e.tensor_scalar(ang, ang, invf, None, ALU.mult), reads=[tag + "ang", "const"], writes=[tag + "ang"])
            ki = posi
            P.op("dve", lambda e: e.tensor_scalar(kf, ang, 1.0 / (2 * PI), None, ALU.mult), reads=[tag + "ang"], writes=[tag + "kf"])
            P.op("dve", lambda e: e.tensor_copy(ki, kf), reads=[tag + "kf"], writes=[tag + "posi"])
            P.op("dve", lambda e: e.tensor_copy(kf, ki), reads=[tag + "posi"], writes=[tag + "kf"])
            P.op("dve", lambda e: e.scalar_tensor_tensor(ang, kf, -2 * PI, ang, ALU.mult, ALU.add),
                 reads=[tag + "kf", tag + "ang"], writes=[tag + "ang"])
            for dst, shift in ((sn, 0.0), (cs, PI / 2)):
                P.op("dve", lambda e, shift=shift: e.tensor_scalar(kf, ang, shift, None, ALU.add),
                     reads=[tag + "ang", tag + "kf"], writes=[tag + "kf"])
                for _ in range(2):
                    P.op("dve", lambda e, dst=dst: e.tensor_scalar(dst, kf, PI, -2 * PI, ALU.is_gt, ALU.mult),
                         reads=[tag + "kf"], writes=[tag + "cs"])
                    P.op("dve", lambda e, dst=dst: e.tensor_tensor(kf, kf, dst, ALU.add),
                         reads=[tag + "kf", tag + "cs"], writes=[tag + "kf"])
                P.op("dve", lambda e, dst=dst: e.tensor_scalar(dst, kf, -PI, 2 * PI, ALU.is_lt, ALU.mult),
                     reads=[tag + "kf"], writes=[tag + "cs"])
                P.op("dve", lambda e, dst=dst: e.tensor_tensor(kf, kf, dst, ALU.add),
                     reads=[tag + "kf", tag + "cs"], writes=[tag + "kf"])
                P.op("act", lambda e, dst=dst: e.activation(dst, kf, AF.Sin), reads=[tag + "kf"], writes=[tag + "cs"])

        def rstd_bcast(dst, pst, pk, n, tagw):
            P.op("act", lambda e: e.activation(dst, pst[:], AF.Sqrt, scale=1.0 / n, bias=EPS), reads=[pk], writes=[tagw])
            P.op("dve", lambda e: e.reciprocal(dst, dst), reads=[tagw], writes=[tagw])

        def phase_kv():
            xin = [A.alloc(16 * 512).rearrange("p (k t) -> p k t", t=512) for _ in range(2)]
            wkv, wkvk = load_w(I.w_in[:, 4608:4928], 320)
            wuk = A.alloc(2 * 2048).rearrange("p (c h d) -> p c h d", c=2, d=128)
            wuv = A.alloc(2 * 2048).rearrange("p (c h d) -> p c h d", c=2, d=128)
            src = I.w_ukv.rearrange("(c p) (h two d) -> p c h two d", p=128, two=2, d=128)
            for c in range(2):
                P.dma("pool", wuk[:, c, :, :], src[:, c, :, 0, :], writes=["wuk"])
                P.dma("pool", wuv[:, c, :, :], src[:, c, :, 1, :], writes=["wuv"])
            ckv32 = A.alloc(2 * 512, F32).rearrange("p (c t) -> p c t", t=512)
            sqb = A.alloc(2 * 512).rearrange("p (c t) -> p c t", t=512)
            ckvn = A.alloc(2 * 512).rearrange("p (c t) -> p c t", t=512)
            rs = A.alloc(512, F32)
            kr32 = A.alloc(512, F32, parts=64); krsq = A.alloc(512, parts=64); krg32 = A.alloc(512, F32, parts=64)
            krgb = A.alloc(512, parts=64); kro = A.alloc(512, F32, parts=64); tmp64 = A.alloc(512, F32, parts=64)
            posi = A.alloc(512, I32, parts=64); ang = A.alloc(512, F32, parts=64); kf = A.alloc(512, F32, parts=64)
            cs = A.alloc(512, F32, parts=64); sn = A.alloc(512, F32, parts=64)
            kn32 = [A.alloc(512, F32) for _ in range(2)]
            knsq = [A.alloc(512) for _ in range(2)]
            rsh = [A.alloc(512, F32) for _ in range(2)]
            kon = [A.alloc(16 * 512).rearrange("p (h t) -> p h t", t=512) for _ in range(2)]
            kor = [A.alloc(16 * 512, parts=64).rearrange("p (h t) -> p h t", t=512) for _ in range(2)]
            vt = [A.alloc(D) for _ in range(2)]
            xT = UTA.rearrange("(k p) t -> p k t", p=128)
            nv = 0
            for tt in range(NTA):
                xb = xin[tt % 2]; xk = ("xin", tt % 2)
                P.dma("sp", xb, xT[:, :, tt * 512:(tt + 1) * 512], writes=[xk])
                rope_tables(pos_all, tt * 512, posi, ang, kf, cs, sn, "k")
                for fc in range(2):
                    for k in range(16):
                        P.op("pe", lambda e, fc=fc, k=k, xb=xb: e.matmul(ps[fc][:], wkv[:, k, fc * 128:(fc + 1) * 128], xb[:, k, :],
                                                                         start=(k == 0), stop=(k == 15)),
                             reads=[wkvk, xk], writes=[PS(fc)])
                    P.op("act", lambda e, fc=fc: e.copy(ckv32[:, fc, :], ps[fc][:]), reads=[PS(fc)], writes=[("ckv32", fc)])
                    P.op("act", lambda e, fc=fc: e.activation(sqb[:, fc, :], ps[fc][:], AF.Square), reads=[PS(fc)], writes=[("sqb", fc)])
                for k in range(16):
                    P.op("pe", lambda e, k=k, xb=xb: e.matmul(ps[2][0:64, :], wkv[:, k, 256:320], xb[:, k, :], start=(k == 0), stop=(k == 15)),
                         reads=[wkvk, xk], writes=[PS(2)])
                P.op("act", lambda e: e.copy(kr32, ps[2][0:64, :]), reads=[PS(2)], writes=["kr32"])
                P.op("act", lambda e: e.activation(krsq, ps[2][0:64, :], AF.Square), reads=[PS(2)], writes=["krsq"])
                for fc in range(2):
                    P.op("pe", lambda e, fc=fc: e.matmul(ps[3][:], onesb, sqb[:, fc, :], start=(fc == 0), stop=(fc == 1)),
                         reads=["const", ("sqb", fc)], writes=[PS(3)])
                rstd_bcast(rs, ps[3], PS(3), 256, "rs")
                for fc in range(2):
                    P.op("dve", lambda e, fc=fc: e.scalar_tensor_tensor(ckvn[:, fc, :], ckv32[:, fc, :], kvan[:, fc:fc + 1], rs, ALU.mult, ALU.mult),
                         reads=[("ckv32", fc), "rs", "const"], writes=[("ckvn", fc)])
                P.op("dve", lambda e: e.tensor_scalar(krg32, kr32, qkn[0:64, 3:4], None, ALU.mult), reads=["kr32", "const"], writes=["krg32"])
                P.op("act", lambda e: e.copy(krgb, krg32), reads=["krg32"], writes=["krgb"])
                P.op("pe", lambda e: e.matmul(ps[2][0:64, :], rot, krgb, start=True, stop=True), reads=["const", "krgb"], writes=[PS(2)])
                P.op("dve", lambda e: e.tensor_tensor(kro, krg32, cs, ALU.mult), reads=["krg32", "kcs"], writes=["kro"])
                P.op("dve", lambda e: e.tensor_tensor(tmp64, ps[2][0:64, :], sn, ALU.mult), reads=[PS(2), "kcs"], writes=["tmp64"])
                P.op("dve", lambda e: e.tensor_tensor(kro, kro, tmp64, ALU.add), reads=["kro", "tmp64"], writes=["kro"])
                KN = kon[tt % 2]; KR = kor[tt % 2]
                for h in range(16):
                    i = h % 2
                    pa = 4 + i; pb = 6 + i
                    for c in range(2):
                        P.op("pe", lambda e, h=h, c=c, pa=pa: e.matmul(ps[pa][:], wuk[:, c, h, :], ckvn[:, c, :], start=(c == 0), stop=(c == 1)),
                             reads=["wuk", ("ckvn", 0), ("ckvn", 1)], writes=[PS(pa)])
                    P.op("act", lambda e, i=i, pa=pa: e.copy(kn32[i], ps[pa][:]), reads=[PS(pa)], writes=[("kn32", i)])
                    P.op("act", lambda e, i=i, pa=pa: e.activation(knsq[i], ps[pa][:], AF.Square), reads=[PS(pa)], writes=[("knsq", i)])
                    P.op("pe", lambda e, i=i, pb=pb: e.matmul(ps[pb][:], onesb, knsq[i], start=True, stop=False),
                         reads=["const", ("knsq", i)], writes=[PS(pb)])
                    P.op("pe", lambda e, pb=pb: e.matmul(ps[pb][:], onesb[0:64, :], krsq, start=False, stop=True),
                         reads=["const", "krsq"], writes=[PS(pb)])
                    rstd_bcast(rsh[i], ps[pb], PS(pb), 192, ("rsh", i))
                    P.op("dve", lambda e, h=h, i=i: e.scalar_tensor_tensor(KN[:, h, :], kn32[i], qkn[:, 2:3], rsh[i], ALU.mult, ALU.mult),
                         reads=[("kn32", i), ("rsh", i), "const"], writes=[("kon", tt % 2)])
                    P.op("pool", lambda e, h=h, i=i: e.tensor_tensor(KR[:, h, :], kro, rsh[i][0:64, :], ALU.mult),
                         reads=["kro", ("rsh", i)], writes=[("kor", tt % 2)])
                P.dma("act", KT.rearrange("h p t -> p h t")[0:128, :, tt * 512:(tt + 1) * 512], KN, reads=[("kon", tt % 2)],
                      semkey=("kons", tt % 2))
                P.dma("act", KT.rearrange("h p t -> p h t")[128:192, :, tt * 512:(tt + 1) * 512], KR, reads=[("kor", tt % 2)],
                      semkey=("kors", tt % 2))
                for sub in range(4):
                    vb = vt[nv % 2]; vk = ("vt", nv % 2); nv += 1
                    for hc in range(4):
                        pi = hc
                        for c in range(2):
                            P.op("pe", lambda e, c=c, hc=hc, sub=sub, pi=pi: e.matmul(
                                ps[pi][:], ckvn[:, c, sub * 128:(sub + 1) * 128],
                                wuv[:, c, hc * 4:(hc + 1) * 4, :].rearrange("p h d -> p (h d)"), start=(c == 0), stop=(c == 1)),
                                reads=["wuv", ("ckvn", 0), ("ckvn", 1)], writes=[PS(pi)])
                        eng = "act" if hc % 2 == 0 else "dve"
                        if eng == "act":
                            P.op("act", lambda e, vb=vb, hc=hc, pi=pi: e.copy(vb[:, hc * 512:(hc + 1) * 512], ps[pi][:]),
                                 reads=[PS(pi)], writes=[vk])
                        else:
                            P.op("dve", lambda e, vb=vb, hc=hc, pi=pi: e.tensor_copy(vb[:, hc * 512:(hc + 1) * 512], ps[pi][:]),
                                 reads=[PS(pi)], writes=[vk])
                    r0 = tt * 512 + sub * 128
                    P.dma("act", VTK[r0:r0 + 128, :], vb, reads=[vk], semkey=("vts", (nv - 1) % 2))
            P.barrier()
            A.reset()

        def phase_q():
            xin = [A.alloc(16 * 512).rearrange("p (k t) -> p k t", t=512) for _ in range(2)]
            wq, wqk = load_w(I.w_in[:, 4096:4608], 512)
            wuq, wuqk = load_w(I.w_uq, 3072, nk=4)
            cq32 = A.alloc(4 * 512, F32).rearrange("p (c t) -> p c t", t=512)
            sqb = A.alloc(4 * 512).rearrange("p (c t) -> p c t", t=512)
            cqn = A.alloc(4 * 512).rearrange("p (c t) -> p c t", t=512)
            rs = A.alloc(512, F32)
            posi = A.alloc(512, I32, parts=64); ang = A.alloc(512, F32, parts=64); kf = A.alloc(512, F32, parts=64)
            cs = A.alloc(512, F32, parts=64); sn = A.alloc(512, F32, parts=64)
            qn32 = [A.alloc(512, F32) for _ in range(2)]
            qnsq = [A.alloc(512) for _ in range(2)]
            qr32 = [A.alloc(512, F32, parts=64) for _ in range(2)]
            qrsq = [A.alloc(512, parts=64) for _ in range(2)]
            qrgb = [A.alloc(512, parts=64) for _ in range(2)]
            qro = [A.alloc(512, F32, parts=64) for _ in range(2)]
            tmp64 = [A.alloc(512, F32, parts=64) for _ in range(2)]
            rsh = [A.alloc(512, F32) for _ in range(2)]
            qon = [A.alloc(16 * 512).rearrange("p (h t) -> p h t", t=512) for _ in range(2)]
            qor = [A.alloc(16 * 512, parts=64).rearrange("p (h t) -> p h t", t=512) for _ in range(2)]
            xT = UTO.rearrange("(k p) t -> p k t", p=128)
            for tt in range(NTO):
                xb = xin[tt % 2]; xk = ("xin", tt % 2)
                P.dma("sp", xb, xT[:, :, tt * 512:(tt + 1) * 512], writes=[xk])
                rope_tables(pos_own, tt * 512, posi, ang, kf, cs, sn, "q")
                for fc in range(4):
                    for k in range(16):
                        P.op("pe", lambda e, fc=fc, k=k, xb=xb: e.matmul(ps[fc][:], wq[:, k, fc * 128:(fc + 1) * 128], xb[:, k, :],
                                                                         start=(k == 0), stop=(k == 15)),
                             reads=[wqk, xk], writes=[PS(fc)])
                    P.op("act", lambda e, fc=fc: e.copy(cq32[:, fc, :], ps[fc][:]), reads=[PS(fc)], writes=[("cq32", fc)])
                    P.op("act", lambda e, fc=fc: e.activation(sqb[:, fc, :], ps[fc][:], AF.Square), reads=[PS(fc)], writes=[("sqb", fc)])
                for fc in range(4):
                    P.op("pe", lambda e, fc=fc: e.matmul(ps[4][:], onesb, sqb[:, fc, :], start=(fc == 0), stop=(fc == 3)),
                         reads=["const", ("sqb", fc)], writes=[PS(4)])
                rstd_bcast(rs, ps[4], PS(4), 512, "rs")
                for fc in range(4):
                    P.op("dve", lambda e, fc=fc: e.scalar_tensor_tensor(cqn[:, fc, :], cq32[:, fc, :], qan[:, fc:fc + 1], rs, ALU.mult, ALU.mult),
                         reads=[("cq32", fc), "rs", "const"], writes=["cqn"])
                QN = qon[tt % 2]; QR = qor[tt % 2]
                for h in range(16):
                    i = h % 2
                    pa = 0 + i; pb = 2 + i; pc = 4 + i; pd = 6 + i
                    for c in range(4):
                        P.op("pe", lambda e, h=h, c=c, pa=pa: e.matmul(ps[pa][:], wuq[:, c, h * 192:h * 192 + 128], cqn[:, c, :],
                                                                       start=(c == 0), stop=(c == 3)),
                             reads=[wuqk, "cqn"], writes=[PS(pa)])
                    for c in range(4):
                        P.op("pe", lambda e, h=h, c=c, pb=pb: e.matmul(ps[pb][0:64, :], wuq[:, c, h * 192 + 128:h * 192 + 192], cqn[:, c, :],
                                                                       start=(c == 0), stop=(c == 3)),
                             reads=[wuqk, "cqn"], writes=[PS(pb)])
                    P.op("act", lambda e, i=i, pa=pa: e.copy(qn32[i], ps[pa][:]), reads=[PS(pa)], writes=[("qn32", i)])
                    P.op("act", lambda e, i=i, pa=pa: e.activation(qnsq[i], ps[pa][:], AF.Square), reads=[PS(pa)], writes=[("qnsq", i)])
                    P.op("act", lambda e, i=i, pb=pb: e.activation(qr32[i], ps[pb][0:64, :], AF.Identity, scale=gqs[0:64, 1:2]),
                         reads=[PS(pb), "gqs"], writes=[("qr32", i)])
                    P.op("act", lambda e, i=i, pb=pb: e.activation(qrsq[i], ps[pb][0:64, :], AF.Square), reads=[PS(pb)], writes=[("qrsq", i)])
                    P.op("pe", lambda e, i=i, pc=pc: e.matmul(ps[pc][:], onesb, qnsq[i], start=True, stop=False),
                         reads=["const", ("qnsq", i)], writes=[PS(pc)])
                    P.op("pe", lambda e, i=i, pc=pc: e.matmul(ps[pc][:], onesb[0:64, :], qrsq[i], start=False, stop=True),
                         reads=["const", ("qrsq", i)], writes=[PS(pc)])
                    rstd_bcast(rsh[i], ps[pc], PS(pc), 192, ("rsh", i))
                    P.op("dve", lambda e, h=h, i=i: e.scalar_tensor_tensor(QN[:, h, :], qn32[i], gqs[:, 0:1], rsh[i], ALU.mult, ALU.mult),
                         reads=[("qn32", i), ("rsh", i), "gqs"], writes=[("qon", tt % 2)])
                    P.op("pool", lambda e, i=i: e.tensor_copy(qrgb[i], qr32[i]), reads=[("qr32", i)], writes=[("qrgb", i)])
                    P.op("pe", lambda e, i=i, pd=pd: e.matmul(ps[pd][0:64, :], rot, qrgb[i], start=True, stop=True),
                         reads=["const", ("qrgb", i)], writes=[PS(pd)])
                    P.op("pool", lambda e, i=i: e.tensor_tensor(qro[i], qr32[i], cs, ALU.mult), reads=[("qr32", i), "qcs"], writes=[("qro", i)])
                    P.op("dve", lambda e, i=i, pd=pd: e.tensor_tensor(tmp64[i], ps[pd][0:64, :], sn, ALU.mult), reads=[PS(pd), "qcs"], writes=[("tmp64", i)])
                    P.op("pool", lambda e, i=i: e.tensor_tensor(qro[i], qro[i], tmp64[i], ALU.add), reads=[("qro", i), ("tmp64", i)], writes=[("qro", i)])
                    P.op("pool", lambda e, h=h, i=i: e.tensor_tensor(QR[:, h, :], qro[i], rsh[i][0:64, :], ALU.mult),
                         reads=[("qro", i), ("rsh", i)], writes=[("qor", tt % 2)])
                P.dma("act", QT.rearrange("h p t -> p h t")[0:128, :, tt * 512:(tt + 1) * 512], QN, reads=[("qon", tt % 2)],
                      semkey=("qons", tt % 2))
                P.dma("act", QT.rearrange("h p t -> p h t")[128:192, :, tt * 512:(tt + 1) * 512], QR, reads=[("qor", tt % 2)],
                      semkey=("qors", tt % 2))
            P.barrier()
            A.reset()

        def phase_attn():
            NKB = S // 128
            ktn = [A.alloc(S) for _ in range(2)]
            ktr = [A.alloc(S, parts=64) for _ in range(2)]
            vh = [A.alloc(NKB * 128).rearrange("p (k d) -> p k d", d=128) for _ in range(2)]
            qtn = [A.alloc(T) for _ in range(2)]
            qtr = [A.alloc(T, parts=64) for _ in range(2)]
            pt = [A.alloc(512) for _ in range(4)]
            rl = A.alloc(512, F32)
            yo = [A.alloc(512) for _ in range(2)]
            lacc = [A.alloc(512, F32) for _ in range(2)]
            ones32 = A.alloc(128, F32)
            P.op("pool", lambda e: e.memset(ones32, 1.0), writes=["ones32"])
            def prefetch(h):
                hb = h % 2; hk = ("head", hb)
                P.dma("sp", ktn[hb], KT[h, 0:128, :], writes=[hk], semkey=hk)
                P.dma("sp", ktr[hb], KT[h, 128:192, :], writes=[hk], semkey=hk)
                P.dma("sp", vh[hb], VTK.rearrange("(k p) f -> p k f", p=128)[:, :, h * 128:(h + 1) * 128], writes=[hk], semkey=hk)
                P.dma("sp", qtn[hb], QT[h, 0:128, :], writes=[hk], semkey=hk)
                P.dma("sp", qtr[hb], QT[h, 128:192, :], writes=[hk], semkey=hk)

            steps = []
            nch = 0
            for h in range(16):
                for qc in range(NTO):
                    nkb = 8 * qc + 8
                    for kb in range(nkb):
                        steps.append((h, qc, kb, nkb, nch))
                    nch += 1

            def emit_S(i):
                h, qc, kb, nkb, nch_ = steps[i]
                hb = h % 2; hk = ("head", hb)
                jlo = max(0, kb // 2 - 4 * qc)
                Nv = (4 - jlo) * 128
                q0 = qc * 512 + jlo * 128
                si = i % 4
                P.op("pe", lambda e: e.matmul(ps[si][:, 0:Nv], ktn[hb][:, kb * 128:(kb + 1) * 128], qtn[hb][:, q0:q0 + Nv], start=True, stop=False),
                     reads=[hk], writes=[PS(si)])
                P.op("pe", lambda e: e.matmul(ps[si][:, 0:Nv], ktr[hb][:, kb * 128:(kb + 1) * 128], qtr[hb][:, q0:q0 + Nv], start=False, stop=True),
                     reads=[hk], writes=[PS(si)])

            prefetch(0)
            emit_S(0)
            emit_S(1)
            for i, (h, qc, kb, nkb, nch_) in enumerate(steps):
                hb = h % 2; hk = ("head", hb)
                if qc == 0 and kb == 0 and h + 1 < 16:
                    prefetch(h + 1)
                if i + 2 < len(steps):
                    emit_S(i + 2)
                po = 4 + (nch_ % 2) * 2; pl = po + 1
                jlo = max(0, kb // 2 - 4 * qc)
                Nv = (4 - jlo) * 128
                si = i % 4
                pb = pt[si]; pkey = ("pt", si)
                if jlo > 0:
                    P.op("pool", lambda e: e.memset(pb[:, 0:jlo * 128], 0.0), writes=[pkey])
                P.op("act", lambda e: e.activation(pb[:, jlo * 128:512], ps[si][:, 0:Nv], AF.Exp), reads=[PS(si)], writes=[pkey])
                if kb // 2 >= 4 * qc:
                    par = kb % 2
                    P.op("pool", lambda e: e.tensor_tensor(
                        pb[:, jlo * 128:(jlo + 1) * 128], pb[:, jlo * 128:(jlo + 1) * 128], masks[:, par * 128:(par + 1) * 128], ALU.mult),
                        reads=[pkey, "const"], writes=[pkey])
                P.op("pe", lambda e: e.matmul(ps[po][:], vh[hb][:, kb, :], pb, start=(kb == 0), stop=(kb == nkb - 1)),
                     reads=[hk, pkey], writes=[PS(po)])
                la = lacc[nch_ % 2]; lk = ("lacc", nch_ % 2)
                if kb == 0:
                    P.op("dve", lambda e: e.tensor_copy(la, pb), reads=[pkey], writes=[lk])
                else:
                    P.op("dve", lambda e: e.tensor_tensor(la, la, pb, ALU.add), reads=[pkey, lk], writes=[lk])
                if kb == nkb - 1:
                    P.op("pe", lambda e: e.matmul(ps[pl][:], ones32, la, start=True, stop=True),
                         reads=["ones32", lk], writes=[PS(pl)])
                    P.op("dve", lambda e: e.reciprocal(rl, ps[pl][:]), reads=[PS(pl)], writes=["rl"])
                    yb = yo[nch_ % 2]; yk = ("yo", nch_ % 2)
                    P.op("dve", lambda e: e.tensor_tensor(yb, ps[po][:], rl, ALU.mult), reads=[PS(po), "rl"], writes=[yk])
                    P.dma("act", YMT[h * 128:(h + 1) * 128, qc * 512:(qc + 1) * 512], yb, reads=[yk], semkey=("yos", nch_ % 2))
            P.barrier()
            A.reset()

        def simple_pass(wsrc, in_scr, mode, aux1, aux2, dst):
            xin = [A.alloc(16 * 512).rearrange("p (k t) -> p k t", t=512) for _ in range(2)]
            a1 = [A.alloc(8 * 512).rearrange("p (c t) -> p c t", t=512) for _ in range(2)] if aux1 is not None else None
            a2 = [A.alloc(8 * 512).rearrange("p (c t) -> p c t", t=512) for _ in range(2)] if aux2 is not None else None
            ob = [A.alloc(8 * 512).rearrange("p (c t) -> p c t", t=512) for _ in range(2)]
            t32 = [A.alloc(512, F32) for _ in range(2)]
            u32 = [A.alloc(512, F32) for _ in range(2)]
            wts = [load_w(wsrc[:, half * 1024:(half + 1) * 1024], 1024) for half in range(2)]
            for half in range(2):
                wt, wkey = wts[half]
                cnt = [0]
                last_tt = [-1]

                def epi(fc, tt, pst, pk, half=half, cnt=cnt, last_tt=last_tt):
                    i = cnt[0] % 2; cnt[0] += 1
                    o = ob[tt % 2]; ok = ("ob", tt % 2)
                    if tt != last_tt[0]:
                        last_tt[0] = tt
                        for aux, ab, nm in ((aux1, a1, "a1"), (aux2, a2, "a2")):
                            if aux is not None:
                                P.dma("sp", ab[tt % 2], aux.rearrange("(c p) t -> p c t", p=128)[:, half * 8:(half + 1) * 8, tt * 512:(tt + 1) * 512],
                                      writes=[(nm, tt % 2)])
                    if mode == "gelu_mul":
                        t = t32[i]; tk = ("t32", i); u = u32[i]; uk = ("u32", i)
                        P.op("act", lambda e: e.activation(t, pst[:], AF.Square), reads=[pk], writes=[tk])
                        P.op("dve", lambda e: e.tensor_scalar(t, t, 0.044715, 1.0, ALU.mult, ALU.add), reads=[tk], writes=[tk])
                        P.op("dve", lambda e: e.tensor_tensor(t, t, pst[:], ALU.mult), reads=[tk, pk], writes=[tk])
                        P.op("act", lambda e: e.activation(t, t, AF.Sigmoid, scale=2.0 * math.sqrt(2.0 / PI)), reads=[tk], writes=[tk])
                        P.op("dve", lambda e: e.tensor_tensor(u, t, pst[:], ALU.mult), reads=[tk, pk], writes=[uk])
                        P.op("pool", lambda e: e.tensor_tensor(o[:, fc, :], u, a1[tt % 2][:, fc, :], ALU.mult),
                             reads=[uk, ("a1", tt % 2)], writes=[ok])
                    elif mode == "sigmoid":
                        P.op("act", lambda e: e.activation(o[:, fc, :], pst[:], AF.Sigmoid), reads=[pk], writes=[ok])
                    elif mode == "mul":
                        P.op("dve", lambda e: e.tensor_tensor(o[:, fc, :], pst[:], a1[tt % 2][:, fc, :], ALU.mult),
                             reads=[pk, ("a1", tt % 2)], writes=[ok])
                    elif mode == "mul_add":
                        u = u32[i]; uk = ("u32", i)
                        P.op("dve", lambda e: e.tensor_tensor(u, pst[:], a1[tt % 2][:, fc, :], ALU.mult),
                             reads=[pk, ("a1", tt % 2)], writes=[uk])
                        P.op("pool", lambda e: e.tensor_tensor(o[:, fc, :], u, a2[tt % 2][:, fc, :], ALU.add),
                             reads=[uk, ("a2", tt % 2)], writes=[ok])
                    if fc == 7:
                        P.dma("act", dst.rearrange("(c p) t -> p c t", p=128)[:, half * 8:(half + 1) * 8, tt * 512:(tt + 1) * 512], o,
                              reads=[ok], semkey=("obs", tt % 2))
                    return None
                fm_pass(wt, wkey, 1024, in_scr, T, epi, [0, 1, 2, 3], xin)
            P.barrier()
            A.reset()

        def make_row(dst, col, tagr):
            bt = A.alloc(128, F32)
            for j in range(16):
                P.op("dve", lambda e, j=j: e.tensor_copy(bt, col[:, j:j + 1].to_broadcast([128, 128])),
                     reads=["modc", "A2"], writes=["bt"])
                P.op("pe", lambda e: e.transpose(ps[7][:, 0:128], bt, ident32), reads=["bt", "const"], writes=[PS(7)])
                P.op("act", lambda e, j=j: e.copy(dst[:, j * 128:(j + 1) * 128], ps[7][:, 0:128]), reads=[PS(7)], writes=[tagr])

        NT128 = T // 128

        regc = {}

        def phase_out_route(rwk, gidx):
            g1row = A.alloc(D, F32); a2row = A.alloc(D, F32); sh2row = A.alloc(D, F32)
            make_row(g1row, modc[:, 32:48], "g1row")
            make_row(a2row, A2, "a2row")
            make_row(sh2row, modc[:, 48:64], "sh2row")
            wo = [load_w(I.w_out[:, half * 1024:(half + 1) * 1024], 1024) for half in range(2)]
            wr32 = A.alloc(16 * 64, F32).rearrange("p (k e) -> p k e", e=64)
            P.dma("sp", wr32, I.w_router.rearrange("(k p) e -> p k e", p=128), writes=["wr32"])
            xin = [A.alloc(16 * 512).rearrange("p (k t) -> p k t", t=512) for _ in range(2)]
            xo = [A.alloc(D, F32) for _ in range(2)]
            vtk = A.alloc(D, F32)
            vbf = [A.alloc(D) for _ in range(2)]
            junk = A.alloc(D)
            vT = A.alloc(16 * 128, F32).rearrange("p (k t) -> p k t", t=128)
            sm = A.alloc(64, F32)
            sc_ = A.alloc(64, F32); sel = A.alloc(64, F32); msk = A.alloc(64, F32); rw = A.alloc(64, F32)
            mskb = A.alloc(64); pos = A.alloc(64, F32); carry = A.alloc(64, F32); oh = A.alloc(64, F32)
            mx8 = A.alloc(8, F32); ix8 = A.alloc(8, U32); ixf = A.alloc(8, F32); posk = A.alloc(8, F32)
            dst_f = A.alloc(8, F32); ovf = A.alloc(8, F32); dsti = [A.alloc(8, I32) for _ in range(2)]
            P.op("pool", lambda e: e.memset(carry, 0.0), writes=["carry"])
            mT = MT.rearrange("(k p) t -> p k t", p=128)
            for tt in range(NTO):
                xb = xin[tt % 2]; xk = ("xin", tt % 2)
                P.dma("sp", xb, mT[:, :, tt * 512:(tt + 1) * 512], writes=[xk])
                for sub in range(4):
                    ti = tt * 4 + sub
                    r0 = ti * 128
                    X = xo[ti % 2]; Xk = ("xo", ti % 2)
                    P.dma("sp", X, I.x_own[r0:r0 + 128, :], writes=[Xk])
                    for dc in range(4):
                        wt, wkey = wo[dc // 2]
                        for k in range(16):
                            P.op("pe", lambda e, dc=dc, k=k, wt=wt, xb=xb, sub=sub: e.matmul(
                                ps[dc][:], xb[:, k, sub * 128:(sub + 1) * 128], wt[:, k, (dc % 2) * 512:(dc % 2 + 1) * 512],
                                start=(k == 0), stop=(k == 15)), reads=[wkey, xk], writes=[PS(dc)])
                        P.op("dve", lambda e, dc=dc: e.tensor_tensor(vtk[:, dc * 512:(dc + 1) * 512], ps[dc][:], g1row[:, dc * 512:(dc + 1) * 512], ALU.mult),
                             reads=[PS(dc), "g1row"], writes=[("vtk", dc)])
                        P.op("pool", lambda e, dc=dc, X=X: e.tensor_tensor(X[:, dc * 512:(dc + 1) * 512], X[:, dc * 512:(dc + 1) * 512], vtk[:, dc * 512:(dc + 1) * 512], ALU.add),
                             reads=[Xk, ("vtk", dc)], writes=[Xk])
                    P.dma("act", X1[r0:r0 + 128, :], X, reads=[Xk], semkey=("x1s", ti % 2))
                    ss = sm[:, 0:1]; rs_ = sm[:, 1:2]
                    P.op("act", lambda e, X=X: e.activation(junk, X, AF.Square, accum_out=ss), reads=[Xk], writes=["junk", "ss"])
                    P.op("act", lambda e: e.activation(rs_, ss, AF.Sqrt, scale=1.0 / D, bias=EPS), reads=["ss"], writes=["rs_"])
                    P.op("dve", lambda e: e.reciprocal(rs_, rs_), reads=["rs_"], writes=["rs_"])
                    P.op("dve", lambda e, X=X: e.scalar_tensor_tensor(vtk, X, rs_, a2row, ALU.mult, ALU.mult),
                         reads=[Xk, "rs_", "a2row"] + [("vtk", d) for d in range(4)], writes=[("vtk", d) for d in range(4)] + ["vtkall"])
                    P.op("pool", lambda e: e.tensor_tensor(vtk, vtk, sh2row, ALU.add), reads=["vtkall", "sh2row"], writes=["vtkall"] + [("vtk", d) for d in range(4)])
                    VB = vbf[ti % 2]; VBk = ("vbf", ti % 2)
                    P.op("act", lambda e, VB=VB: e.copy(VB, vtk), reads=["vtkall"], writes=[VBk])
                    for q4 in range(4):
                        pi = 4 + q4 % 2
                        for j in range(4):
                            fc = q4 * 4 + j
                            P.op("pe", lambda e, fc=fc, j=j, pi=pi: e.transpose(ps[pi][:, j * 128:(j + 1) * 128], vtk[:, fc * 128:(fc + 1) * 128], ident32),
                                 reads=["vtkall", "const"], writes=[PS(pi)])
                        P.op("act", lambda e, q4=q4, pi=pi: e.copy(vT[:, q4 * 4:(q4 + 1) * 4, :].rearrange("p k t -> p (k t)"), ps[pi][:]),
                             reads=[PS(pi)], writes=["vT"])
                    for k in range(16):
                        P.op("pe", lambda e, k=k: e.matmul(ps[6][:, 0:64], vT[:, k, :], wr32[:, k, :], start=(k == 0), stop=(k == 15)),
                             reads=["vT", "wr32"], writes=[PS(6)])
                    P.op("act", lambda e: e.activation(sc_, ps[6][:, 0:64], AF.Sigmoid), reads=[PS(6)], writes=["sc_"])
                    P.op("dve", lambda e: e.tensor_tensor(sel, sc_, rbias, ALU.add), reads=["sc_", "const"], writes=["sel"])
                    P.op("dve", lambda e: e.max(mx8, sel), reads=["sel"], writes=["mx8"])
                    P.op("dve", lambda e: e.max_index(ix8, mx8, sel), reads=["sel", "mx8"], writes=["ix8"])
                    P.op("dve", lambda e: e.tensor_scalar(msk, sel, mx8[:, 5:6], None, ALU.is_ge), reads=["sel", "mx8"], writes=["msk"])
                    P.op("dve", lambda e: e.tensor_tensor(rw, sc_, msk, ALU.mult), reads=["sc_", "msk"], writes=["rw"])
                    den = sm[:, 2:3]
                    P.op("dve", lambda e: e.reduce_sum(den, rw, axis=AX.X), reads=["rw"], writes=["den"])
                    P.op("dve", lambda e: e.reciprocal(den, den), reads=["den"], writes=["den"])
                    P.op("dve", lambda e: e.tensor_scalar(rw, rw, den, 2.5, ALU.mult, ALU.mult), reads=["rw", "den"], writes=["rw"])
                    P.op("act", lambda e: e.copy(mskb, msk), reads=["msk"], writes=["mskb"])
                    P.op("pe", lambda e: e.matmul(ps[7][:, 0:64], ustr, mskb, start=True, stop=True), reads=["const", "mskb"], writes=[PS(7)])
                    P.op("pe", lambda e: e.matmul(ps[7][:, 64:128], onesb, mskb, start=True, stop=True), reads=["const", "mskb"], writes=[PS(7)])
                    P.op("dve", lambda e: e.tensor_tensor(pos, ps[7][:, 0:64], carry, ALU.add), reads=[PS(7), "carry"], writes=["pos"])
                    P.op("dve", lambda e: e.tensor_tensor(carry, ps[7][:, 64:128], carry, ALU.add), reads=[PS(7), "carry"], writes=["carry"])
                    P.op("dve", lambda e: e.tensor_copy(ixf, ix8), reads=["ix8"], writes=["ixf"])
                    for k in range(TOPK):
                        P.op("dve", lambda e, k=k: e.tensor_scalar(oh, iota, ixf[:, k:k + 1], None, ALU.is_equal), reads=["const", "ixf"], writes=["oh"])
                        P.op("dve", lambda e: e.tensor_tensor(msk, oh, pos, ALU.mult), reads=["oh", "pos", "msk"], writes=["msk"])
                        P.op("dve", lambda e, k=k: e.reduce_sum(posk[:, k:k + 1], msk, axis=AX.X), reads=["msk"], writes=["posk"])
                        P.op("dve", lambda e: e.tensor_tensor(msk, oh, rw, ALU.mult), reads=["oh", "rw", "msk"], writes=["msk"])
                        P.op("dve", lambda e, k=k, ti=ti: e.reduce_sum(rwk[:, ti * 8 + k:ti * 8 + k + 1], msk, axis=AX.X), reads=["msk"], writes=["rwk"])
                    P.op("dve", lambda e: e.scalar_tensor_tensor(dst_f[:, 0:6], ixf[:, 0:6], float(C), posk[:, 0:6], ALU.mult, ALU.add),
                         reads=["ixf", "posk"], writes=["dst_f"])
                    P.op("dve", lambda e: e.tensor_scalar(ovf[:, 0:6], posk[:, 0:6], float(C), None, ALU.is_ge), reads=["posk"], writes=["ovf"])
                    P.op("dve", lambda e: e.tensor_scalar(posk[:, 0:6], ovf[:, 0:6], -1.0, 1.0, ALU.mult, ALU.add), reads=["ovf", "posk"], writes=["posk"])
                    P.op("dve", lambda e, ti=ti: e.tensor_tensor(rwk[:, ti * 8:ti * 8 + 6], rwk[:, ti * 8:ti * 8 + 6], posk[:, 0:6], ALU.mult),
                         reads=["posk", "rwk"], writes=["rwk"])
                    P.op("dve", lambda e: e.tensor_tensor(dst_f[:, 0:6], dst_f[:, 0:6], posk[:, 0:6], ALU.mult), reads=["dst_f", "posk"], writes=["dst_f"])
                    DI = dsti[ti % 2]; DIk = ("dsti", ti % 2)
                    P.op("dve", lambda e: e.scalar_tensor_tensor(posk[:, 0:6], ovf[:, 0:6], 4194304.0, dst_f[:, 0:6], ALU.mult, ALU.add),
                         reads=["ovf", "dst_f", "posk"], writes=["posk"])
                    P.op("dve", lambda e, DI=DI: e.tensor_copy(DI[:, 0:6], posk[:, 0:6]), reads=["posk"], writes=[DIk])
                    P.op("dve", lambda e, DI=DI, ti=ti: e.tensor_copy(gidx[:, ti * 8:ti * 8 + 6], dst_f[:, 0:6]), reads=["dst_f"], writes=["gidx"])
                    for k in range(TOPK):
                        def scat(e, DI=DI, VB=VB, k=k):
                            if "r" not in regc:
                                regc["r"] = e.to_reg(NROW - 1)
                            return e.indirect_dma_start(
                                out=XG, out_offset=bass.IndirectOffsetOnAxis(ap=DI[:, k:k + 1], axis=0), in_=VB, in_offset=None,
                                bounds_check=regc["r"], oob_is_err=False)
                        P.raw("pool", scat,
                            reads=[DIk, VBk], writes=[], semkey=("sc", ti % 2))
                    P.dma("act", XS[r0:r0 + 128, :], VB, reads=[VBk], semkey=("xgs", ti % 2))
            P.barrier()
            A.reset()

        def phase_experts():
            NH = C // 512
            xg = [A.alloc(4 * D).rearrange("p (b f) -> p b f", f=D) for _ in range(2)]
            xT = A.alloc(16 * C).rearrange("p (k s) -> p k s", s=C)
            wg = [A.alloc(16 * 128).rearrange("p (k f) -> p k f", f=128) for _ in range(3)]
            wu = [A.alloc(16 * 128).rearrange("p (k f) -> p k f", f=128) for _ in range(3)]
            hT = A.alloc(NFC * C).rearrange("p (f s) -> p f s", s=C)
            wd = [A.alloc(NFC * 512).rearrange("p (f d) -> p f d", d=512) for _ in range(3)]
            sg = [A.alloc(512, F32) for _ in range(2)]
            yb = [A.alloc(512) for _ in range(4)]
            nxg = 0; nw = 0; nwd = 0; nsg = 0; ny = 0; npsA = 0; ntr = 0
            for ex in range(NX):
                if ex > 0 and ex % 8 == 0:
                    P.barrier()
                if ex < NE:
                    Wg = getattr(I, "wg%02d" % (ex // 4))[ex % 4]; Wu = getattr(I, "wu%02d" % (ex // 4))[ex % 4]
                    Wd = getattr(I, "wd%02d" % (ex // 4))[ex % 4]
                    wq_, wdeps = "pool", []
                else:
                    Wg = I.ws_gate; Wu = I.ws_up; Wd = I.ws_down
                    wq_, wdeps = "pool", []
                for hh_ in range(NH):
                    g = xg[nxg % 2]; gk = ("xg", nxg % 2); nxg += 1
                    r0 = (ex if ex < NE else ex - NE) * C + hh_ * 512
                    XSRC = XG if ex < NE else XS
                    P.dma("sp", g, XSRC[r0:r0 + 512, :].rearrange("(b p) f -> p b f", p=128), writes=[gk])
                    for k in range(16):
                        pi = 6 + ntr % 2; ntr += 1
                        pbf = ps[pi][:].bitcast(BF16)
                        for b4 in range(4):
                            P.op("pe", lambda e, g=g, b4=b4, k=k, pbf=pbf: e.transpose(pbf[:, b4 * 128:(b4 + 1) * 128], g[:, b4, k * 128:(k + 1) * 128], identb),
                                 reads=[gk, "const"], writes=[PS(pi)])
                        eng = "act" if k % 2 == 0 else "dve"
                        if eng == "act":
                            P.op("act", lambda e, k=k, hh_=hh_, pbf=pbf: e.copy(xT[:, k, hh_ * 512:(hh_ + 1) * 512], pbf[:, 0:512]),
                                 reads=[PS(pi)], writes=["xT"])
                        else:
                            P.op("dve", lambda e, k=k, hh_=hh_, pbf=pbf: e.tensor_copy(xT[:, k, hh_ * 512:(hh_ + 1) * 512], pbf[:, 0:512]),
                                 reads=[PS(pi)], writes=["xT"])
                Wgv = Wg.rearrange("(k p) f -> p k f", p=128); Wuv = Wu.rearrange("(k p) f -> p k f", p=128)
                for fc in range(NFC):
                    wi = nw % 3; nw += 1
                    P.dma(wq_, wg[wi], Wgv[:, :, fc * 128:(fc + 1) * 128], reads=wdeps[0:1], writes=[("wg", wi)])
                    P.dma(wq_, wu[wi], Wuv[:, :, fc * 128:(fc + 1) * 128], reads=wdeps[1:2], writes=[("wu", wi)])
                    for hh_ in range(NH):
                        pg = (npsA % 3) * 2; pu = pg + 1; npsA += 1
                        for k in range(16):
                            P.op("pe", lambda e, wi=wi, k=k, hh_=hh_, pg=pg: e.matmul(ps[pg][:], wg[wi][:, k, :], xT[:, k, hh_ * 512:(hh_ + 1) * 512],
                                                                                    start=(k == 0), stop=(k == 15)),
                                 reads=[("wg", wi), "xT"], writes=[PS(pg)])
                        for k in range(16):
                            P.op("pe", lambda e, wi=wi, k=k, hh_=hh_, pu=pu: e.matmul(ps[pu][:], wu[wi][:, k, :], xT[:, k, hh_ * 512:(hh_ + 1) * 512],
                                                                                    start=(k == 0), stop=(k == 15)),
                                 reads=[("wu", wi), "xT"], writes=[PS(pu)])
                        s = sg[nsg % 2]; sk_ = ("sg", nsg % 2); nsg += 1
                        P.op("act", lambda e, s=s, pg=pg: e.activation(s, ps[pg][:], AF.Silu), reads=[PS(pg)], writes=[sk_])
                        P.op("dve", lambda e, s=s, pu=pu, fc=fc, hh_=hh_: e.tensor_tensor(hT[:, fc, hh_ * 512:(hh_ + 1) * 512], s, ps[pu][:], ALU.mult),
                             reads=[sk_, PS(pu)], writes=["hT"])
                Wdv = Wd.rearrange("(f p) d -> p f d", p=128)
                for dc in range(4):
                    wi = nwd % 3; nwd += 1
                    P.dma(wq_, wd[wi], Wdv[:, :, dc * 512:(dc + 1) * 512], reads=wdeps[2:3], writes=[("wd", wi)])
                    for sb in range(C // 128):
                        pi = (npsA % 3) * 2 + (sb % 2);
                        if sb % 2 == 1:
                            npsA += 1
                        for fc in range(NFC):
                            P.op("pe", lambda e, wi=wi, fc=fc, sb=sb, pi=pi: e.matmul(ps[pi][:], hT[:, fc, sb * 128:(sb + 1) * 128], wd[wi][:, fc, :],
                                                                                    start=(fc == 0), stop=(fc == NFC - 1)),
                                 reads=[("wd", wi), "hT"], writes=[PS(pi)])
                        y = yb[ny % 4]; yk = ("yb", ny % 4)
                        if ny % 2 == 0:
                            P.op("act", lambda e, y=y, pi=pi: e.copy(y, ps[pi][:]), reads=[PS(pi)], writes=[yk])
                        else:
                            P.op("dve", lambda e, y=y, pi=pi: e.tensor_copy(y, ps[pi][:]), reads=[PS(pi)], writes=[yk])
                        r0 = (ex if ex < NE else ex - NE) * C + sb * 128
                        YDST = YG if ex < NE else YS
                        P.dma("act", YDST[r0:r0 + 128, dc * 512:(dc + 1) * 512], y, reads=[yk], semkey=("ybs", ny % 4))
                        ny += 1
                    if C // 128 % 2 == 1:
                        npsA += 1
            P.barrier()
            A.reset()

        def phase_combine(rwk, gidx):
            g2row = A.alloc(D, F32)
            make_row(g2row, modc[:, 80:96], "g2row")
            yg = [A.alloc(7 * D).rearrange("p (k f) -> p k f", f=D) for _ in range(2)]
            acc = [A.alloc(D, F32) for _ in range(2)]
            x1 = [A.alloc(D, F32) for _ in range(2)]
            for ti in range(NT128):
                r0 = ti * 128
                Y = yg[ti % 2]; Yk = ("yg", ti % 2)
                for k in range(TOPK):
                    P.raw("pool", lambda e, Y=Y, k=k, ti=ti: e.indirect_dma_start(
                        out=Y[:, k, :], out_offset=None, in_=YG, in_offset=bass.IndirectOffsetOnAxis(ap=gidx[:, ti * 8 + k:ti * 8 + k + 1], axis=0)),
                        reads=["gidx"], writes=[Yk], semkey=Yk)
                P.dma("sp", Y[:, 6, :], YS[r0:r0 + 128, :], writes=[Yk], semkey=Yk)
                X = x1[ti % 2]; Xk = ("x1", ti % 2)
                P.dma("sp", X, X1[r0:r0 + 128, :], writes=[Xk])
                a = acc[ti % 2]; ak = ("acc", ti % 2)
                P.op("dve", lambda e, a=a, Y=Y, ti=ti: e.scalar_tensor_tensor(a, Y[:, 0, :], rwk[:, ti * 8:ti * 8 + 1], Y[:, 6, :], ALU.mult, ALU.add),
                     reads=[Yk, "rwk"], writes=[ak])
                for k in range(1, TOPK):
                    P.op("dve", lambda e, a=a, Y=Y, ti=ti, k=k: e.scalar_tensor_tensor(a, Y[:, k, :], rwk[:, ti * 8 + k:ti * 8 + k + 1], a, ALU.mult, ALU.add),
                         reads=[Yk, "rwk", ak], writes=[ak])
                P.op("dve", lambda e, a=a: e.tensor_tensor(a, a, g2row, ALU.mult), reads=[ak, "g2row"], writes=[ak])
                P.op("pool", lambda e, a=a, X=X: e.tensor_tensor(X, X, a, ALU.add), reads=[ak, Xk], writes=[Xk])
                P.dma("act", out_d[r0:r0 + 128, :], X, reads=[Xk], semkey=("outs", ti % 2))
            P.barrier()

        def zero_scratch():
            z = A.alloc(4 * D).rearrange("p (b f) -> p b f", f=D)
            P.op("pool", lambda e: e.memset(z, 0.0), writes=["z"])
            for r in range(0, 512, 512):
                P.dma("sp", XG[r:r + 512, :].rearrange("(b p) f -> p b f", p=128), z, reads=["z"], semkey="zx")
            P.barrier()
            A.reset()

        rwk = A.alloc(NT128 * 8, F32)
        gidx = A.alloc(NT128 * 8, I32)
        A.mark()

        phases = dbg_phases if (dbg_phases := getattr(build, "phases", None)) else None
        def want(nm):
            return phases is None or nm in phases
        if want("zero"):
            zero_scratch()
        if want("p0"):
            phase0()
        else:
            P.dma("sp", modc, MODC, writes=["modc"])
        if want("p1"):
            norm_T(I.x_all, S, UTA, A1, modc[:, 0:16])
            norm_T(I.x_own, T, UTO, A1, modc[:, 0:16])
        if want("rnn"):
            phase_rnn()
        if want("kv"):
            phase_kv()
        if want("q"):
            phase_q()
        if want("attn"):
            phase_attn()
        if want("merge"):
            simple_pass(I.w_in[:, 2048:4096], UTO, "gelu_mul", HT, None, YRT)
            simple_pass(I.w_in[:, 4928:6976], UTO, "sigmoid", None, None, GAT)
            simple_pass(I.w_in[:, 6976:9024], UTO, "sigmoid", None, None, GBT)
            simple_pass(I.w_rnn_out, YRT, "mul", GAT, None, MA)
            simple_pass(I.w_mla_out, YMT, "mul_add", GBT, MA, MT)
        if want("route"):
            phase_out_route(rwk, gidx)
        if want("experts"):
            phase_experts()
        if want("combine"):
            phase_combine(rwk, gidx)
        P.barrier()
        P.emit()
    nc._declared = declared
    return nc


def _col(v, n):
    return np.ascontiguousarray(np.asarray(v, np.float32).reshape(n, 128).T)


def make_in_maps(S, inp):
    bf = ml_dtypes.bfloat16
    x = np.asarray(inp["x"]); B = x.shape[0]
    pos = np.asarray(inp["positions"]).astype(np.int32)
    sq = lambda k: np.ascontiguousarray(np.asarray(inp[k])[0])
    ident = np.eye(128, dtype=np.float32)
    ustr = np.triu(np.ones((128, 128), np.float32), 1)
    rot = np.zeros((64, 64), np.float32)
    for m in range(32):
        rot[m + 32, m] = -1.0
        rot[m, m + 32] = 1.0
    invf = (np.float32(10000.0) ** (-np.arange(0, 64, 2, dtype=np.float32) / np.float32(64))).astype(np.float32)
    invf2 = np.concatenate([invf, invf]).reshape(64, 1).astype(np.float32)
    iota = np.broadcast_to(np.arange(64, dtype=np.float32)[None, :], (128, 64)).copy()
    vec16 = np.concatenate([_col(sq("norm1"), 16), _col(sq("norm2"), 16), _col(sq("conv_b"), 16), _col(sq("b_a"), 16),
                            _col(sq("b_i"), 16), _col(sq("lru_lambda"), 16), np.zeros((128, 16), np.float32)], axis=1)
    cw = sq("conv_w")
    convw = np.concatenate([_col(cw[j], 16) for j in range(4)], axis=1)
    qn = sq("q_norm"); kn = sq("k_norm")
    qkn = np.zeros((128, 4), np.float32)
    qkn[:, 0] = qn[:128]; qkn[:64, 1] = qn[128:]; qkn[:, 2] = kn[:128]; qkn[:64, 3] = kn[128:]
    shared = dict(
        ident32=ident, identb=ident.astype(bf), onesb=np.ones((128, 128), bf), ustrict=ustr.astype(bf),
        rot=rot.astype(bf), iota_row=iota, invf=invf2, bmod_col=_col(sq("b_mod"), 96), vec16=vec16, convw_col=convw,
        qan_col=_col(sq("q_a_norm"), 4), kvan_col=_col(sq("kv_a_norm"), 2), qkn_col=qkn,
        rbias_row=np.broadcast_to(sq("router_bias")[None, :], (128, 64)).copy(),
        w_mod=sq("w_mod"), w_in=sq("w_in"), w_a=sq("w_a"), w_i=sq("w_i"),
        w_uq=sq("w_uq").reshape(512, 3072), w_ukv=sq("w_ukv").reshape(256, 4096),
        w_rnn_out=sq("w_rnn_out"), w_mla_out=sq("w_mla_out"), w_out=sq("w_out"), w_router=sq("w_router"),
        ws_gate=sq("ws_gate"), ws_up=sq("ws_up"), ws_down=sq("ws_down"),
    )
    wg_ = np.asarray(inp["w_gate"])[0]; wu_ = np.asarray(inp["w_up"])[0]; wd_ = np.asarray(inp["w_down"])[0]
    for e_ in range(NE // 4):
        shared["wg%02d" % e_] = wg_[4 * e_:4 * e_ + 4]; shared["wu%02d" % e_] = wu_[4 * e_:4 * e_ + 4]
        shared["wd%02d" % e_] = wd_[4 * e_:4 * e_ + 4]
    maps = []
    for core in range(2 * B):
        b = core // 2; c = core % 2
        xb = np.ascontiguousarray(x[b])
        xo = np.ascontiguousarray(xb.reshape(S // 128, 128, D)[c::2].reshape(S // 2, D))
        pa = pos[b]
        po = np.ascontiguousarray(pa.reshape(S // 128, 128)[c::2].reshape(S // 2))
        q = np.arange(128)[None, :] // 64; k = np.arange(128)[:, None] // 64
        diag = (k <= q).astype(np.float32)
        if c == 0:
            m_even = diag; m_odd = np.zeros((128, 128), np.float32)
        else:
            m_even = np.ones((128, 128), np.float32); m_odd = diag
        m = dict(shared)
        m.update(

            x_all=xb, x_own=xo,
            pos_all=np.broadcast_to(pa[None, :], (64, S)).copy(), pos_own=np.broadcast_to(po[None, :], (64, S // 2)).copy(),
            c_col=_col(np.asarray(inp["c"])[b], 16),
            cpar=np.broadcast_to(np.array([[c, 1 - c]], np.float32), (128, 2)).copy(),
            masks=np.concatenate([m_even, m_odd], axis=1).astype(bf),
        )
        maps.append(m)
    return maps


_CACHE = {}


def run(inp, S, C, dbg=(), ncores=None):
    key = (S, C, tuple(dbg))
    if key not in _CACHE:
        _CACHE[key] = build(S, C, dbg)
    nc = _CACHE[key]
    maps = make_in_maps(S, inp)
    if ncores is not None:
        maps = maps[:ncores]
    maps = [{k: m[k] for k in nc._declared} for m in maps]
    res = run_bass_kernel_spmd(nc, maps, core_ids=list(range(len(maps))))
    return res


def kernel(**inputs):
    x = np.asarray(inputs["x"])
    B, S, _ = x.shape
    res = run(inputs, S, 1024)
    out = np.empty((B, S, D), np.float32)
    for core, r in enumerate(res.results):
        b = core // 2; c = core % 2
        out[b].reshape(S // 128, 128, D)[c::2] = r["out"].reshape(S // 256, 128, D)
    return out
```

```python
import math
import types
import numpy as np
import ml_dtypes
import concourse.bass as bass
import concourse.mybir as mybir
from concourse.bass_utils import run_bass_kernel_spmd
from contextlib import ExitStack

F32 = mybir.dt.float32
BF16 = mybir.dt.bfloat16
I32 = mybir.dt.int32
U32 = mybir.dt.uint32
AF = mybir.ActivationFunctionType
ALU = mybir.AluOpType
AX = mybir.AxisListType

ENGS = ("pe", "act", "dve", "pool", "sp")
D = 2048
NKC = 16
EPS = 1e-6
NE = 64
DEXP = 1408
NFC = 11
TOPK = 6
PI = math.pi


def _freeze(fn):
    if fn is None or fn.__closure__ is None:
        return fn
    cells = []
    for c in fn.__closure__:
        try:
            cells.append(types.CellType(c.cell_contents))
        except ValueError:
            cells.append(c)
    return types.FunctionType(fn.__code__, fn.__globals__, fn.__name__, fn.__defaults__, tuple(cells))


class Prog:
    def __init__(self, nc, stack):
        self.nc = nc
        self.stack = stack
        self.q = {e: [] for e in ENGS}
        self.cnt = {e: 0 for e in ENGS}
        self.waited = {e: {} for e in ENGS}
        self.res = {}
        self.dsems = {}
        self.semobj = {}
        self.gen = {e: 0 for e in ENGS}
        self.dfree = []
        self.nds = 0
        self.epoch = 0
        for e in ENGS:
            self.semobj[("e", e, 0)] = stack.enter_context(nc.semaphore("s_" + e))

    def _need(self, eng, tok, waits):
        if tok is None:
            return
        sk, val, src = tok
        if src == "pe" and eng == "pe":
            return
        if self.waited[eng].get(sk, 0) >= val:
            return
        self.waited[eng][sk] = val
        waits.append((sk, val))

    def _deps(self, eng, reads, writes):
        waits = []
        for r in reads:
            st = self.res.get(r)
            if st:
                self._need(eng, st[0], waits)
        for w in writes:
            st = self.res.get(w)
            if st:
                self._need(eng, st[0], waits)
                for t in st[1]:
                    self._need(eng, t, waits)
        return waits

    def _commit(self, tok, reads, writes):
        for r in reads:
            st = self.res.setdefault(r, [None, []])
            st[1].append(tok)
        for w in writes:
            self.res[w] = [tok, []]

    def op(self, eng, fn, reads=(), writes=()):
        waits = self._deps(eng, reads, writes)
        self.cnt[eng] += 1
        sk = ("e", eng, self.gen[eng])
        tok = (sk, self.cnt[eng], eng)
        self.q[eng].append((waits, _freeze(fn), sk, 1))
        self._commit(tok, reads, writes)
        return tok

    def _dsem(self, semkey):
        if semkey not in self.dsems:
            if self.dfree:
                ent = self.dfree.pop()
            else:
                self.nds += 1
                ent = [self.stack.enter_context(self.nc.semaphore("d%d" % self.nds)), 0]
            self.dsems[semkey] = ent
            self.semobj[semkey] = ent[0]
        self.dsems[semkey][1] += 16
        return self.dsems[semkey][1]

    def raw(self, queue, fn, reads=(), writes=(), semkey=None):
        waits = self._deps(queue, reads, writes)
        semkey = ("d", semkey, self.epoch)
        val = self._dsem(semkey)
        tok = (semkey, val, None)
        self.q[queue].append((waits, _freeze(fn), semkey, 16))
        self._commit(tok, reads, writes)
        return tok

    def dma(self, queue, out, in_, reads=(), writes=(), semkey=None, **kw):
        if semkey is None:
            semkey = writes[0] if writes else reads[0]

        def fn(e, out=out, in_=in_, kw=kw):
            return e.dma_start(out=out, in_=in_, **kw)
        return self.raw(queue, fn, reads, writes, semkey)

    def barrier(self):
        toks = [(("e", e, self.gen[e]), self.cnt[e], e) for e in ENGS if self.cnt[e] > 0]
        toks += [(k, v[1], None) for k, v in self.dsems.items() if v[1] > 0]
        for eng in ENGS:
            waits = []
            for sk, val, src in toks:
                if src == eng:
                    continue
                if self.waited[eng].get(sk, 0) >= val:
                    continue
                self.waited[eng][sk] = val
                waits.append((sk, val))
            if waits:
                self.q[eng].append((waits, None, None, 0))
        self.res = {}
        self.dfree.extend(self.dsems.values())
        self.dsems = {}
        self.epoch += 1
        for e in ENGS:
            if self.cnt[e] > 30000:
                self.gen[e] += 1
                self.cnt[e] = 0
                self.semobj[("e", e, self.gen[e])] = self.stack.enter_context(
                    self.nc.semaphore("s_%s_%d" % (e, self.gen[e])))

    def emit(self):
        nc = self.nc
        with nc.Block() as block:
            def run(engname, e):
                for waits, fn, sk, inc in self.q[engname]:
                    for wk, wv in waits:
                        e.wait_ge(self.semobj[wk], wv)
                    if fn is not None:
                        fn(e).then_inc(self.semobj[sk], inc)

            @block.tensor
            def _(e):
                run("pe", e)

            @block.scalar
            def _(e):
                run("act", e)

            @block.vector
            def _(e):
                run("dve", e)

            @block.gpsimd
            def _(e):
                run("pool", e)

            @block.sync
            def _(e):
                run("sp", e)


class Arena:
    def __init__(self, ap, nelem):
        self.ap = ap
        self.n = nelem
        self.off = 0
        self.base = 0

    def alloc(self, n, dt=BF16, parts=128):
        mul = 2 if dt in (F32, I32, U32) else 1
        sz = n * mul
        sz = (sz + 15) // 16 * 16
        assert self.off + sz <= self.n, "arena overflow %d + %d > %d" % (self.off, sz, self.n)
        a = self.ap[:, self.off:self.off + n * mul]
        self.off += sz
        if dt != BF16:
            a = a.bitcast(dt)
        if parts != 128:
            a = a[0:parts, :]
        return a

    def mark(self):
        self.base = self.off

    def reset(self):
        self.off = self.base


def build(S, C, dbg=()):
    T = S // 2
    NTA = S // 512
    NTO = T // 512
    assert T % C == 0 and C % 512 == 0
    NPS = T // C
    NX = NE + NPS
    NROW = NE * C
    nc = bass.Bass("TRN2", target_bir_lowering=False)

    declared = []

    def din(name, shape, dt=F32):
        declared.append(name)
        return nc.dram_tensor(name, list(shape), dt, kind="ExternalInput").ap()

    def dscr(name, shape, dt=BF16):
        kind = "ExternalOutput" if name in dbg else "Internal"
        return nc.dram_tensor(name, list(shape), dt, kind=kind).ap()

    pos_all = din("pos_all", [64, S], I32); pos_own = din("pos_own", [64, T], I32)
    c_col = din("c_col", [128, 16])
    cpar = din("cpar", [128, 2])
    masks_d = din("masks", [128, 256], BF16)
    ident32_d = din("ident32", [128, 128]); identb_d = din("identb", [128, 128], BF16)
    onesb_d = din("onesb", [128, 128], BF16); ustr_d = din("ustrict", [128, 128], BF16)
    rot_d = din("rot", [64, 64], BF16); iota_d = din("iota_row", [128, 64])
    invf_d = din("invf", [64, 1])
    bmod_d = din("bmod_col", [128, 96])
    vec16_d = din("vec16", [128, 7 * 16])
    convw_d = din("convw_col", [128, 64])
    qan_d = din("qan_col", [128, 4]); kvan_d = din("kvan_col", [128, 2])
    qkn_d = din("qkn_col", [128, 4])
    rbias_d = din("rbias_row", [128, 64])
    BIG = dict(x_all=[S, D], x_own=[T, D], w_mod=[D, 6 * D], w_in=[D, 9024], w_a=[16, 128, 128], w_i=[16, 128, 128],
               w_uq=[512, 3072], w_ukv=[256, 4096], w_rnn_out=[D, D], w_mla_out=[D, D], w_out=[D, D], w_router=[D, NE],
               ws_gate=[D, DEXP], ws_up=[D, DEXP],
               ws_down=[DEXP, D])

    for e_ in range(NE // 4):
        BIG["wg%02d" % e_] = [4, D, DEXP]; BIG["wu%02d" % e_] = [4, D, DEXP]; BIG["wd%02d" % e_] = [4, DEXP, D]

    class _Lazy:
        def __init__(self):
            self.c = {}

        def __getattr__(self, name):
            c = self.__dict__["c"]
            if name not in c:
                c[name] = din(name, BIG[name])
            return c[name]
    I = _Lazy()
    out_d = nc.dram_tensor("out", [T, D], F32, kind="ExternalOutput").ap()

    UTA = dscr("UTA", [D, S]); UTO = dscr("UTO", [D, T])
    HT = dscr("HT", [D, T]); KT = dscr("KT", [16, 192, S]); VTK = dscr("VTK", [S, D]); QT = dscr("QT", [16, 192, T])
    YMT = dscr("YMT", [D, T]); YRT = dscr("YRT", [D, T]); GAT = dscr("GAT", [D, T]); GBT = dscr("GBT", [D, T])
    MA = dscr("MA", [D, T]); MT = dscr("MT", [D, T])
    X1 = dscr("X1", [T, D], F32)
    XG = dscr("XG", [NROW, D]); YG = dscr("YG", [NROW, D])
    XS = dscr("XS", [T, D]); YS = dscr("YS", [T, D])
    MODC = dscr("MODC", [128, 96], F32)

    with ExitStack() as st:
        ARN = 103424
        arena_t = st.enter_context(nc.sbuf_tensor("arena", [128, ARN], BF16))
        A = Arena(arena_t, ARN)
        ps = [st.enter_context(nc.psum_tensor("ps%d" % i, [128, 512], F32)) for i in range(8)]
        P = Prog(nc, st)
        ldq = ["sp"]

        def PS(i):
            return ("ps", i)

        ident32 = A.alloc(128, F32); identb = A.alloc(128); onesb = A.alloc(128); ustr = A.alloc(128)
        rot = A.alloc(64, parts=64); masks = A.alloc(256); iota = A.alloc(64, F32); rbias = A.alloc(64, F32)
        invf = A.alloc(1, F32, parts=64); cp = A.alloc(2, F32)
        bmod = A.alloc(96, F32); vec16 = A.alloc(112, F32); convw = A.alloc(64, F32)
        qan = A.alloc(4, F32); kvan = A.alloc(2, F32); qkn = A.alloc(4, F32)
        modc = A.alloc(96, F32); A1 = A.alloc(16, F32); A2 = A.alloc(16, F32); nsp8 = A.alloc(16, F32)
        gqs = A.alloc(2, F32); ccol = A.alloc(16, F32); scs = A.alloc(16, F32)
        negpi = A.alloc(1, F32); tmpc = A.alloc(64, F32)
        for dst, src in ((ident32, ident32_d), (identb, identb_d), (onesb, onesb_d), (ustr, ustr_d), (rot, rot_d),
                         (masks, masks_d), (iota, iota_d), (rbias, rbias_d), (invf, invf_d), (cp, cpar),
                         (bmod, bmod_d), (vec16, vec16_d), (convw, convw_d), (qan, qan_d), (kvan, kvan_d),
                         (qkn, qkn_d), (ccol, c_col)):
            P.dma("sp", dst, src, writes=["const"], semkey="const")
        norm1 = vec16[:, 0:16]; norm2 = vec16[:, 16:32]; convb = vec16[:, 32:48]
        b_a = vec16[:, 48:64]; b_i = vec16[:, 64:80]; lam = vec16[:, 80:96]
        cvec = cp[:, 0:1]; omc = cp[:, 1:2]
        A.mark()

        def phase0():
            P.op("act", lambda e: e.activation(scs, ccol, AF.Silu), reads=["const"], writes=["scs"])
            P.op("pool", lambda e: e.memset(negpi, -PI), writes=["negpi"])
            wm = [A.alloc(16 * 768, F32).rearrange("p (k c) -> p k c", c=768) for _ in range(2)]
            wsrc = I.w_mod.rearrange("(k p) c -> p k c", p=128)
            for blk in range(16):
                b = wm[blk % 2]
                for k0 in range(0, 16, 4):
                    P.dma("sp", b[:, k0:k0 + 4, :], wsrc[:, k0:k0 + 4, blk * 768:(blk + 1) * 768],
                          writes=[("wm", blk % 2)], semkey=("wm", blk % 2))
                for f in range(6):
                    fa = blk * 6 + f
                    for kc in range(16):
                        P.op("pe", lambda e, b=b, f=f, fa=fa, kc=kc: e.matmul(
                            ps[0][:, fa:fa + 1], b[:, kc, f * 128:(f + 1) * 128], scs[:, kc:kc + 1],
                            start=(kc == 0), stop=(kc == 15)),
                            reads=[("wm", blk % 2), "scs"], writes=[PS(0)])
            P.op("dve", lambda e: e.tensor_tensor(modc, ps[0][:, 0:96], bmod, ALU.add),
                 reads=[PS(0), "const"], writes=["modc"])
            P.op("dve", lambda e: e.scalar_tensor_tensor(A1, modc[:, 16:32], 1.0, norm1, ALU.add, ALU.mult),
                 reads=["modc"], writes=["A1"])
            P.op("dve", lambda e: e.scalar_tensor_tensor(A2, modc[:, 64:80], 1.0, norm2, ALU.add, ALU.mult),
                 reads=["modc"], writes=["A2"])
            t0 = tmpc[:, 0:16]; t1 = tmpc[:, 16:32]; t2 = tmpc[:, 32:48]; t3 = tmpc[:, 48:64]
            P.op("dve", lambda e: e.tensor_scalar(t0, lam, -1.0, None, ALU.mult), reads=["const"], writes=["t0"])
            P.op("dve", lambda e: e.tensor_tensor(t0, t0, lam, ALU.max), reads=["const", "t0"], writes=["t0"])
            P.op("act", lambda e: e.activation(t1, t0, AF.Exp, scale=-1.0), reads=["t0"], writes=["t1"])
            P.op("dve", lambda e: e.tensor_scalar(t2, t1, 2.0, None, ALU.add), reads=["t1"], writes=["t2"])
            P.op("dve", lambda e: e.reciprocal(t2, t2), reads=["t2"], writes=["t2"])
            P.op("dve", lambda e: e.tensor_tensor(t1, t1, t2, ALU.mult), reads=["t1", "t2"], writes=["t1"])
            P.op("dve", lambda e: e.tensor_tensor(t2, t1, t1, ALU.mult), reads=["t1"], writes=["t2"])
            P.op("dve", lambda e: e.tensor_scalar(t3, t2, 1.0 / 9, 1.0 / 7, ALU.mult, ALU.add), reads=["t2"], writes=["t3"])
            for cst in (1.0 / 5, 1.0 / 3, 1.0):
                P.op("dve", lambda e: e.tensor_tensor(t3, t3, t2, ALU.mult), reads=["t3", "t2"], writes=["t3"])
                P.op("dve", lambda e, cst=cst: e.tensor_scalar(t3, t3, cst, None, ALU.add), reads=["t3"], writes=["t3"])
            P.op("dve", lambda e: e.tensor_tensor(t3, t3, t1, ALU.mult), reads=["t3", "t1"], writes=["t3"])
            P.op("dve", lambda e: e.tensor_scalar(t0, lam, -1.0, 0.0, ALU.mult, ALU.max), reads=["const"], writes=["t0"])
            P.op("dve", lambda e: e.scalar_tensor_tensor(t3, t3, 2.0, t0, ALU.mult, ALU.add), reads=["t3", "t0"], writes=["t3"])
            P.op("dve", lambda e: e.tensor_scalar(nsp8, t3, -8.0, None, ALU.mult), reads=["t3"], writes=["nsp8"])
            P.op("dve", lambda e: e.tensor_scalar(gqs, qkn[:, 0:2], 192.0 ** -0.5, None, ALU.mult),
                 reads=["const"], writes=["gqs"])
            P.dma("sp", MODC, modc, reads=["modc"], semkey="modc_out")
            P.barrier()
            A.reset()

        def norm_T(x_src, ntok, dst, Acol, shcol):
            xt = [A.alloc(D, F32) for _ in range(2)]
            junk = A.alloc(D)
            ss = A.alloc(4, F32)
            ut = [A.alloc(16 * 512).rearrange("p (c t) -> p c t", t=512) for _ in range(2)]
            n = 0
            for g in range(ntok // 512):
                u = ut[g % 2]
                for sub in range(4):
                    b = xt[n % 2]; xk = ("xt", n % 2)
                    r0 = g * 512 + sub * 128
                    P.dma("sp", b, x_src[r0:r0 + 128, :], writes=[xk])
                    sc = ss[:, 0:1]; sc2 = ss[:, 1:2]
                    P.op("act", lambda e, b=b, sc=sc: e.activation(junk, b, AF.Square, accum_out=sc),
                         reads=[xk], writes=["junk", "ss"])
                    P.op("act", lambda e, sc=sc, sc2=sc2: e.activation(sc2, sc, AF.Sqrt, scale=1.0 / D, bias=EPS),
                         reads=["ss"], writes=["ss2"])
                    P.op("dve", lambda e, sc2=sc2: e.reciprocal(sc2, sc2), reads=["ss2"], writes=["ss2"])
                    P.op("pool", lambda e, b=b, sc2=sc2: e.tensor_scalar(b, b, sc2, 1.0, ALU.mult, ALU.mult),
                         reads=[xk, "ss2"], writes=[xk])
                    for q4 in range(4):
                        pi = (n * 4 + q4) % 4
                        for j in range(4):
                            fc = q4 * 4 + j
                            P.op("pe", lambda e, b=b, fc=fc, j=j, pi=pi: e.transpose(
                                ps[pi][:, j * 128:(j + 1) * 128], b[:, fc * 128:(fc + 1) * 128], ident32),
                                reads=[xk, "const"], writes=[PS(pi)])
                        for j in range(4):
                            fc = q4 * 4 + j
                            P.op("act", lambda e, u=u, fc=fc, j=j, pi=pi, sub=sub: e.activation(
                                u[:, fc, sub * 128:(sub + 1) * 128], ps[pi][:, j * 128:(j + 1) * 128], AF.Identity,
                                bias=shcol[:, fc:fc + 1], scale=Acol[:, fc:fc + 1]),
                                reads=[PS(pi), "modc", "A1", "A2"], writes=[("ut", g % 2)])
                    n += 1
                P.dma("act", dst.rearrange("(c p) t -> p c t", p=128)[:, :, g * 512:(g + 1) * 512], u,
                      reads=[("ut", g % 2)], semkey=("uts", g % 2))
            P.barrier()
            A.reset()

        wpar = [0]

        def load_w(wsrc, ncols, nk=NKC):
            wt = A.alloc(nk * ncols).rearrange("p (k c) -> p k c", c=ncols)
            key = ("w", wpar[0]); wpar[0] += 1
            src = wsrc.rearrange("(k p) c -> p k c", p=128)
            cw_ = min(ncols, 1024)
            step = max(1, 4096 // cw_)
            for c0 in range(0, ncols, cw_):
                c1 = min(ncols, c0 + cw_)
                for k0 in range(0, nk, step):
                    k1 = min(nk, k0 + step)
                    P.dma("pool", wt[:, k0:k1, c0:c1], src[:, k0:k1, c0:c1], writes=[key], semkey=key)
            return wt, key

        def fm_pass(wt, wkey, ncols, in_scr, ntok, epi, psbanks, xin_bufs, nk=NKC, fc0=0):
            pend = None
            n = 0
            for tt in range(ntok // 512):
                xb = xin_bufs[tt % 2]; xk = ("xin", tt % 2)
                P.dma("sp", xb[:, 0:nk, :], in_scr.rearrange("(k p) t -> p k t", p=128)[:, :, tt * 512:(tt + 1) * 512],
                      writes=[xk])
                for fc in range(ncols // 128):
                    pi = psbanks[n % len(psbanks)]; n += 1
                    for k in range(nk):
                        P.op("pe", lambda e, pi=pi, k=k, fc=fc, xb=xb: e.matmul(
                            ps[pi][:], wt[:, k, fc * 128:(fc + 1) * 128], xb[:, k, :], start=(k == 0), stop=(k == nk - 1)),
                            reads=[wkey, xk], writes=[PS(pi)])
                    if pend is not None:
                        pend()
                    pend = epi(fc0 + fc, tt, ps[pi], PS(pi))
            if pend is not None:
                pend()

        def phase_rnn():
            xin = [A.alloc(16 * 512).rearrange("p (k t) -> p k t", t=512) for _ in range(2)]
            wab = A.alloc(16 * 128).rearrange("p (h j) -> p h j", j=128)
            wib = A.alloc(16 * 128).rearrange("p (h j) -> p h j", j=128)
            P.dma("pool", wab, I.w_a.rearrange("h i j -> i h j"), writes=["wab"])
            P.dma("pool", wib, I.w_i.rearrange("h i j -> i h j"), writes=["wib"])
            xr = A.alloc(8 * 516, F32).rearrange("p (c t) -> p c t", t=516)
            carry = A.alloc(8, F32)
            xc = [A.alloc(512, F32) for _ in range(2)]
            xcb = [A.alloc(512) for _ in range(2)]
            rr = [A.alloc(512, F32) for _ in range(2)]
            ii = [A.alloc(512, F32) for _ in range(2)]
            aa = [A.alloc(512, F32) for _ in range(2)]
            bb = [A.alloc(512, F32) for _ in range(2)]
            hh = [A.alloc(512, F32) for _ in range(2)]
            tq = [A.alloc(256, F32) for _ in range(2)]
            hout = [A.alloc(8 * 256).rearrange("p (c t) -> p c t", t=256) for _ in range(2)]
            for half in range(2):
                wt, wkey = load_w(I.w_in[:, half * 1024:(half + 1) * 1024], 1024)
                P.op("pool", lambda e: e.memset(xr, 0.0), writes=["xr"] + [("xr", c) for c in range(8)])
                P.op("pool", lambda e: e.memset(carry, 0.0), writes=[("carry", c) for c in range(8)])
                cnt = [0]

                def epi(fc, tt, pst, pk, half=half, cnt=cnt):
                    gfc = half * 8 + fc
                    i = cnt[0] % 2; cnt[0] += 1
                    X = xr[:, fc, :]
                    P.op("act", lambda e: e.copy(X[:, 4:516], pst[:]), reads=[pk], writes=[("xr", fc)])
                    c0 = xc[i]; k0 = ("xc", i)
                    P.op("dve", lambda e: e.tensor_scalar(c0, X[:, 1:513], convw[:, gfc:gfc + 1], convb[:, gfc:gfc + 1],
                                                          ALU.mult, ALU.add), reads=[("xr", fc), "const"], writes=[k0])
                    for j in range(1, 4):
                        P.op("dve", lambda e, j=j: e.scalar_tensor_tensor(
                            c0, X[:, 1 + j:513 + j], convw[:, j * 16 + gfc:j * 16 + gfc + 1], c0, ALU.mult, ALU.add),
                            reads=[("xr", fc), k0], writes=[k0])
                    P.op("pool", lambda e: e.tensor_copy(X[:, 0:4], X[:, 512:516]), reads=[("xr", fc)], writes=[("xr", fc)])
                    P.op("act", lambda e: e.copy(xcb[i], c0), reads=[k0], writes=[("xcb", i)])

                    def stage2():
                        pr = 4 + i * 2; pq = 5 + i * 2
                        P.op("pe", lambda e: e.matmul(ps[pr][:], wab[:, gfc, :], xcb[i], start=True, stop=True),
                             reads=["wab", ("xcb", i)], writes=[PS(pr)])
                        P.op("pe", lambda e: e.matmul(ps[pq][:], wib[:, gfc, :], xcb[i], start=True, stop=True),
                             reads=["wib", ("xcb", i)], writes=[PS(pq)])
                        P.op("act", lambda e: e.activation(rr[i], ps[pr][:], AF.Sigmoid, bias=b_a[:, gfc:gfc + 1]),
                             reads=[PS(pr), "const"], writes=[("rr", i)])
                        P.op("act", lambda e: e.activation(ii[i], ps[pq][:], AF.Sigmoid, bias=b_i[:, gfc:gfc + 1]),
                             reads=[PS(pq), "const"], writes=[("ii", i)])
                        P.op("act", lambda e: e.activation(aa[i], rr[i], AF.Exp, scale=nsp8[:, gfc:gfc + 1]),
                             reads=[("rr", i), "nsp8"], writes=[("aa", i)])
                        P.op("pool", lambda e: e.tensor_tensor(rr[i], aa[i], aa[i], ALU.mult),
                             reads=[("aa", i), ("rr", i)], writes=[("rr", i)])
                        P.op("act", lambda e: e.activation(rr[i], rr[i], AF.Sqrt, scale=-1.0, bias=1.0),
                             reads=[("rr", i)], writes=[("rr", i)])
                        P.op("pool", lambda e: e.tensor_tensor(ii[i], ii[i], c0, ALU.mult),
                             reads=[("ii", i), k0], writes=[("ii", i)])
                        P.op("dve", lambda e: e.tensor_tensor(bb[i], rr[i], ii[i], ALU.mult),
                             reads=[("rr", i), ("ii", i)], writes=[("bb", i)])
                        P.op("dve", lambda e: e.tensor_tensor_scan(hh[i], aa[i], bb[i], carry[:, fc:fc + 1], ALU.mult, ALU.add),
                             reads=[("aa", i), ("bb", i), ("carry", fc)], writes=[("hh", i)])
                        P.op("pool", lambda e: e.tensor_copy(carry[:, fc:fc + 1], hh[i][:, 511:512]),
                             reads=[("hh", i)], writes=[("carry", fc)])
                        h4 = hh[i].rearrange("p (j two q) -> p j two q", two=2, q=128)
                        t4 = tq[i].rearrange("p (j q) -> p j q", q=128)
                        ho = hout[tt % 2]
                        P.op("dve", lambda e: e.tensor_scalar(t4, h4[:, :, 0, :], omc, None, ALU.mult),
                             reads=[("hh", i), "const"], writes=[("tq", i)])
                        P.op("dve", lambda e: e.scalar_tensor_tensor(
                            ho[:, fc, :].rearrange("p (j q) -> p j q", q=128), h4[:, :, 1, :], cvec, t4, ALU.mult, ALU.add),
                            reads=[("hh", i), ("tq", i), "const"], writes=[("hout", tt % 2)])
                        if fc == 7:
                            P.dma("act", HT.rearrange("(c p) t -> p c t", p=128)[:, half * 8:(half + 1) * 8, tt * 256:(tt + 1) * 256],
                                  ho, reads=[("hout", tt % 2)], semkey=("houts", tt % 2))
                    return stage2
                fm_pass(wt, wkey, 1024, UTA, S, epi, [0, 1, 2, 3], xin)
            P.barrier()
            A.reset()

        def rope_tables(pos_src, t0, posi, ang, kf, cs, sn, tag):
            P.dma("sp", posi, pos_src[:, t0:t0 + 512], writes=[tag + "posi"])
            P.op("dve", lambda e: e.tensor_copy(ang, posi), reads=[tag + "posi"], writes=[tag + "ang"])
            P.op("dve", lambda e: e.tensor_scalar(ang, ang, invf, None, ALU.mult), reads=[tag + "ang", "const"], writes=[tag + "ang"])
            ki = posi
            P.op("dve", lambda e: e.tensor_scalar(kf, ang, 1.0 / (2 * PI), None, ALU.mult), reads=[tag + "ang"], writes=[tag + "kf"])
            P.op("dve", lambda e: e.tensor_copy(ki, kf), reads=[tag + "kf"], writes=[tag + "posi"])
            P.op("dve", lambda e: e.tensor_copy(kf, ki), reads=[tag + "posi"], writes=[tag + "kf"])
            P.op("dve", lambda e: e.scalar_tensor_tensor(ang, kf, -2 * PI, ang, ALU.mult, ALU.add),
                 reads=[tag + "kf", tag + "ang"], writes=[tag + "ang"])
            for dst, shift in ((sn, 0.0), (cs, PI / 2)):
                P.op("dve", lambda e, shift=shift: e.tensor_scalar(kf, ang, shift, None, ALU.add),
                     reads=[tag + "ang", tag + "kf"], writes=[tag + "kf"])
                for _ in range(2):
                    P.op("dve", lambda e, dst=dst: e.tensor_scalar(dst, kf, PI, -2 * PI, ALU.is_gt, ALU.mult),
                         reads=[tag + "kf"], writes=[tag + "cs"])
                    P.op("dve", lambda e, dst=dst: e.tensor_tensor(kf, kf, dst, ALU.add),
                         reads=[tag + "kf", tag + "cs"], writes=[tag + "kf"])
                P.op("dve", lambda e, dst=dst: e.tensor_scalar(dst, kf, -PI, 2 * PI, ALU.is_lt, ALU.mult),
                     reads=[tag + "kf"], writes=[tag + "cs"])
                P.op("dve", lambda e, dst=dst: e.tensor_tensor(kf, kf, dst, ALU.add),
                     reads=[tag + "kf", tag + "cs"], writes=[tag + "kf"])
                P.op("act", lambda e, dst=dst: e.activation(dst, kf, AF.Sin), reads=[tag + "kf"], writes=[tag + "cs"])

        def rstd_bcast(dst, pst, pk, n, tagw):
            P.op("act", lambda e: e.activation(dst, pst[:], AF.Sqrt, scale=1.0 / n, bias=EPS), reads=[pk], writes=[tagw])
            P.op("dve", lambda e: e.reciprocal(dst, dst), reads=[tagw], writes=[tagw])

        def phase_kv():
            xin = [A.alloc(16 * 512).rearrange("p (k t) -> p k t", t=512) for _ in range(2)]
            wkv, wkvk = load_w(I.w_in[:, 4608:4928], 320)
            wuk = A.alloc(2 * 2048).rearrange("p (c h d) -> p c h d", c=2, d=128)
            wuv = A.alloc(2 * 2048).rearrange("p (c h d) -> p c h d", c=2, d=128)
            src = I.w_ukv.rearrange("(c p) (h two d) -> p c h two d", p=128, two=2, d=128)
            for c in range(2):
                P.dma("pool", wuk[:, c, :, :], src[:, c, :, 0, :], writes=["wuk"])
                P.dma("pool", wuv[:, c, :, :], src[:, c, :, 1, :], writes=["wuv"])
            ckv32 = A.alloc(2 * 512, F32).rearrange("p (c t) -> p c t", t=512)
            sqb = A.alloc(2 * 512).rearrange("p (c t) -> p c t", t=512)
            ckvn = A.alloc(2 * 512).rearrange("p (c t) -> p c t", t=512)
            rs = A.alloc(512, F32)
            kr32 = A.alloc(512, F32, parts=64); krsq = A.alloc(512, parts=64); krg32 = A.alloc(512, F32, parts=64)
            krgb = A.alloc(512, parts=64); kro = A.alloc(512, F32, parts=64); tmp64 = A.alloc(512, F32, parts=64)
            posi = A.alloc(512, I32, parts=64); ang = A.alloc(512, F32, parts=64); kf = A.alloc(512, F32, parts=64)
            cs = A.alloc(512, F32, parts=64); sn = A.alloc(512, F32, parts=64)
            kn32 = [A.alloc(512, F32) for _ in range(2)]
            knsq = [A.alloc(512) for _ in range(2)]
            rsh = [A.alloc(512, F32) for _ in range(2)]
            kon = [A.alloc(16 * 512).rearrange("p (h t) -> p h t", t=512) for _ in range(2)]
            kor = [A.alloc(16 * 512, parts=64).rearrange("p (h t) -> p h t", t=512) for _ in range(2)]
            vt = [A.alloc(D) for _ in range(2)]
            xT = UTA.rearrange("(k p) t -> p k t", p=128)
            nv = 0
            for tt in range(NTA):
                xb = xin[tt % 2]; xk = ("xin", tt % 2)
                P.dma("sp", xb, xT[:, :, tt * 512:(tt + 1) * 512], writes=[xk])
                rope_tables(pos_all, tt * 512, posi, ang, kf, cs, sn, "k")
                for fc in range(2):
                    for k in range(16):
                        P.op("pe", lambda e, fc=fc, k=k, xb=xb: e.matmul(ps[fc][:], wkv[:, k, fc * 128:(fc + 1) * 128], xb[:, k, :],
                                                                         start=(k == 0), stop=(k == 15)),
                             reads=[wkvk, xk], writes=[PS(fc)])
                    P.op("act", lambda e, fc=fc: e.copy(ckv32[:, fc, :], ps[fc][:]), reads=[PS(fc)], writes=[("ckv32", fc)])
                    P.op("act", lambda e, fc=fc: e.activation(sqb[:, fc, :], ps[fc][:], AF.Square), reads=[PS(fc)], writes=[("sqb", fc)])
                for k in range(16):
                    P.op("pe", lambda e, k=k, xb=xb: e.matmul(ps[2][0:64, :], wkv[:, k, 256:320], xb[:, k, :], start=(k == 0), stop=(k == 15)),
                         reads=[wkvk, xk], writes=[PS(2)])
                P.op("act", lambda e: e.copy(kr32, ps[2][0:64, :]), reads=[PS(2)], writes=["kr32"])
                P.op("act", lambda e: e.activation(krsq, ps[2][0:64, :], AF.Square), reads=[PS(2)], writes=["krsq"])
                for fc in range(2):
                    P.op("pe", lambda e, fc=fc: e.matmul(ps[3][:], onesb, sqb[:, fc, :], start=(fc == 0), stop=(fc == 1)),
                         reads=["const", ("sqb", fc)], writes=[PS(3)])
                rstd_bcast(rs, ps[3], PS(3), 256, "rs")
                for fc in range(2):
                    P.op("dve", lambda e, fc=fc: e.scalar_tensor_tensor(ckvn[:, fc, :], ckv32[:, fc, :], kvan[:, fc:fc + 1], rs, ALU.mult, ALU.mult),
                         reads=[("ckv32", fc), "rs", "const"], writes=[("ckvn", fc)])
                P.op("dve", lambda e: e.tensor_scalar(krg32, kr32, qkn[0:64, 3:4], None, ALU.mult), reads=["kr32", "const"], writes=["krg32"])
                P.op("act", lambda e: e.copy(krgb, krg32), reads=["krg32"], writes=["krgb"])
                P.op("pe", lambda e: e.matmul(ps[2][0:64, :], rot, krgb, start=True, stop=True), reads=["const", "krgb"], writes=[PS(2)])
                P.op("dve", lambda e: e.tensor_tensor(kro, krg32, cs, ALU.mult), reads=["krg32", "kcs"], writes=["kro"])
                P.op("dve", lambda e: e.tensor_tensor(tmp64, ps[2][0:64, :], sn, ALU.mult), reads=[PS(2), "kcs"], writes=["tmp64"])
                P.op("dve", lambda e: e.tensor_tensor(kro, kro, tmp64, ALU.add), reads=["kro", "tmp64"], writes=["kro"])
                KN = kon[tt % 2]; KR = kor[tt % 2]
                for h in range(16):
                    i = h % 2
                    pa = 4 + i; pb = 6 + i
                    for c in range(2):
                        P.op("pe", lambda e, h=h, c=c, pa=pa: e.matmul(ps[pa][:], wuk[:, c, h, :], ckvn[:, c, :], start=(c == 0), stop=(c == 1)),
                             reads=["wuk", ("ckvn", 0), ("ckvn", 1)], writes=[PS(pa)])
                    P.op("act", lambda e, i=i, pa=pa: e.copy(kn32[i], ps[pa][:]), reads=[PS(pa)], writes=[("kn32", i)])
                    P.op("act", lambda e, i=i, pa=pa: e.activation(knsq[i], ps[pa][:], AF.Square), reads=[PS(pa)], writes=[("knsq", i)])
                    P.op("pe", lambda e, i=i, pb=pb: e.matmul(ps[pb][:], onesb, knsq[i], start=True, stop=False),
                         reads=["const", ("knsq", i)], writes=[PS(pb)])
                    P.op("pe", lambda e, pb=pb: e.matmul(ps[pb][:], onesb[0:64, :], krsq, start=False, stop=True),
                         reads=["const", "krsq"], writes=[PS(pb)])
                    rstd_bcast(rsh[i], ps[pb], PS(pb), 192, ("rsh", i))
                    P.op("dve", lambda e, h=h, i=i: e.scalar_tensor_tensor(KN[:, h, :], kn32[i], qkn[:, 2:3], rsh[i], ALU.mult, ALU.mult),
                         reads=[("kn32", i), ("rsh", i), "const"], writes=[("kon", tt % 2)])
                    P.op("pool", lambda e, h=h, i=i: e.tensor_tensor(KR[:, h, :], kro, rsh[i][0:64, :], ALU.mult),
                         reads=["kro", ("rsh", i)], writes=[("kor", tt % 2)])
                P.dma("act", KT.rearrange("h p t -> p h t")[0:128, :, tt * 512:(tt + 1) * 512], KN, reads=[("kon", tt % 2)],
                      semkey=("kons", tt % 2))
                P.dma("act", KT.rearrange("h p t -> p h t")[128:192, :, tt * 512:(tt + 1) * 512], KR, reads=[("kor", tt % 2)],
                      semkey=("kors", tt % 2))
                for sub in range(4):
                    vb = vt[nv % 2]; vk = ("vt", nv % 2); nv += 1
                    for hc in range(4):
                        pi = hc
                        for c in range(2):
                            P.op("pe", lambda e, c=c, hc=hc, sub=sub, pi=pi: e.matmul(
                                ps[pi][:], ckvn[:, c, sub * 128:(sub + 1) * 128],
                                wuv[:, c, hc * 4:(hc + 1) * 4, :].rearrange("p h d -> p (h d)"), start=(c == 0), stop=(c == 1)),
                                reads=["wuv", ("ckvn", 0), ("ckvn", 1)], writes=[PS(pi)])
                        eng = "act" if hc % 2 == 0 else "dve"
                        if eng == "act":
                            P.op("act", lambda e, vb=vb, hc=hc, pi=pi: e.copy(vb[:, hc * 512:(hc + 1) * 512], ps[pi][:]),
                                 reads=[PS(pi)], writes=[vk])
                        else:
                            P.op("dve", lambda e, vb=vb, hc=hc, pi=pi: e.tensor_copy(vb[:, hc * 512:(hc + 1) * 512], ps[pi][:]),
                                 reads=[PS(pi)], writes=[vk])
                    r0 = tt * 512 + sub * 128
                    P.dma("act", VTK[r0:r0 + 128, :], vb, reads=[vk], semkey=("vts", (nv - 1) % 2))
            P.barrier()
            A.reset()

        def phase_q():
            xin = [A.alloc(16 * 512).rearrange("p (k t) -> p k t", t=512) for _ in range(2)]
            wq, wqk = load_w(I.w_in[:, 4096:4608], 512)
            wuq, wuqk = load_w(I.w_uq, 3072, nk=4)
            cq32 = A.alloc(4 * 512, F32).rearrange("p (c t) -> p c t", t=512)
            sqb = A.alloc(4 * 512).rearrange("p (c t) -> p c t", t=512)
            cqn = A.alloc(4 * 512).rearrange("p (c t) -> p c t", t=512)
            rs = A.alloc(512, F32)
            posi = A.alloc(512, I32, parts=64); ang = A.alloc(512, F32, parts=64); kf = A.alloc(512, F32, parts=64)
            cs = A.alloc(512, F32, parts=64); sn = A.alloc(512, F32, parts=64)
            qn32 = [A.alloc(512, F32) for _ in range(2)]
            qnsq = [A.alloc(512) for _ in range(2)]
            qr32 = [A.alloc(512, F32, parts=64) for _ in range(2)]
            qrsq = [A.alloc(512, parts=64) for _ in range(2)]
            qrgb = [A.alloc(512, parts=64) for _ in range(2)]
            qro = [A.alloc(512, F32, parts=64) for _ in range(2)]
            tmp64 = [A.alloc(512, F32, parts=64) for _ in range(2)]
            rsh = [A.alloc(512, F32) for _ in range(2)]
            qon = [A.alloc(16 * 512).rearrange("p (h t) -> p h t", t=512) for _ in range(2)]
            qor = [A.alloc(16 * 512, parts=64).rearrange("p (h t) -> p h t", t=512) for _ in range(2)]
            xT = UTO.rearrange("(k p) t -> p k t", p=128)
            for tt in range(NTO):
                xb = xin[tt % 2]; xk = ("xin", tt % 2)
                P.dma("sp", xb, xT[:, :, tt * 512:(tt + 1) * 512], writes=[xk])
                rope_tables(pos_own, tt * 512, posi, ang, kf, cs, sn, "q")
                for fc in range(4):
                    for k in range(16):
                        P.op("pe", lambda e, fc=fc, k=k, xb=xb: e.matmul(ps[fc][:], wq[:, k, fc * 128:(fc + 1) * 128], xb[:, k, :],
                                                                         start=(k == 0), stop=(k == 15)),
                             reads=[wqk, xk], writes=[PS(fc)])
                    P.op("act", lambda e, fc=fc: e.copy(cq32[:, fc, :], ps[fc][:]), reads=[PS(fc)], writes=[("cq32", fc)])
                    P.op("act", lambda e, fc=fc: e.activation(sqb[:, fc, :], ps[fc][:], AF.Square), reads=[PS(fc)], writes=[("sqb", fc)])
                for fc in range(4):
                    P.op("pe", lambda e, fc=fc: e.matmul(ps[4][:], onesb, sqb[:, fc, :], start=(fc == 0), stop=(fc == 3)),
                         reads=["const", ("sqb", fc)], writes=[PS(4)])
                rstd_bcast(rs, ps[4], PS(4), 512, "rs")
                for fc in range(4):
                    P.op("dve", lambda e, fc=fc: e.scalar_tensor_tensor(cqn[:, fc, :], cq32[:, fc, :], qan[:, fc:fc + 1], rs, ALU.mult, ALU.mult),
                         reads=[("cq32", fc), "rs", "const"], writes=["cqn"])
                QN = qon[tt % 2]; QR = qor[tt % 2]
                for h in range(16):
                    i = h % 2
                    pa = 0 + i; pb = 2 + i; pc = 4 + i; pd = 6 + i
                    for c in range(4):
                        P.op("pe", lambda e, h=h, c=c, pa=pa: e.matmul(ps[pa][:], wuq[:, c, h * 192:h * 192 + 128], cqn[:, c, :],
                                                                       start=(c == 0), stop=(c == 3)),
                             reads=[wuqk, "cqn"], writes=[PS(pa)])
                    for c in range(4):
                        P.op("pe", lambda e, h=h, c=c, pb=pb: e.matmul(ps[pb][0:64, :], wuq[:, c, h * 192 + 128:h * 192 + 192], cqn[:, c, :],
                                                                       start=(c == 0), stop=(c == 3)),
                             reads=[wuqk, "cqn"], writes=[PS(pb)])
                    P.op("act", lambda e, i=i, pa=pa: e.copy(qn32[i], ps[pa][:]), reads=[PS(pa)], writes=[("qn32", i)])
                    P.op("act", lambda e, i=i, pa=pa: e.activation(qnsq[i], ps[pa][:], AF.Square), reads=[PS(pa)], writes=[("qnsq", i)])
                    P.op("act", lambda e, i=i, pb=pb: e.activation(qr32[i], ps[pb][0:64, :], AF.Identity, scale=gqs[0:64, 1:2]),
                         reads=[PS(pb), "gqs"], writes=[("qr32", i)])
                    P.op("act", lambda e, i=i, pb=pb: e.activation(qrsq[i], ps[pb][0:64, :], AF.Square), reads=[PS(pb)], writes=[("qrsq", i)])
                    P.op("pe", lambda e, i=i, pc=pc: e.matmul(ps[pc][:], onesb, qnsq[i], start=True, stop=False),
                         reads=["const", ("qnsq", i)], writes=[PS(pc)])
                    P.op("pe", lambda e, i=i, pc=pc: e.matmul(ps[pc][:], onesb[0:64, :], qrsq[i], start=False, stop=True),
                         reads=["const", ("qrsq", i)], writes=[PS(pc)])
                    rstd_bcast(rsh[i], ps[pc], PS(pc), 192, ("rsh", i))
                    P.op("dve", lambda e, h=h, i=i: e.scalar_tensor_tensor(QN[:, h, :], qn32[i], gqs[:, 0:1], rsh[i], ALU.mult, ALU.mult),
                         reads=[("qn32", i), ("rsh", i), "gqs"], writes=[("qon", tt % 2)])
                    P.op("pool", lambda e, i=i: e.tensor_copy(qrgb[i], qr32[i]), reads=[("qr32", i)], writes=[("qrgb", i)])
                    P.op("pe", lambda e, i=i, pd=pd: e.matmul(ps[pd][0:64, :], rot, qrgb[i], start=True, stop=True),
                         reads=["const", ("qrgb", i)], writes=[PS(pd)])
                    P.op("pool", lambda e, i=i: e.tensor_tensor(qro[i], qr32[i], cs, ALU.mult), reads=[("qr32", i), "qcs"], writes=[("qro", i)])
                    P.op("dve", lambda e, i=i, pd=pd: e.tensor_tensor(tmp64[i], ps[pd][0:64, :], sn, ALU.mult), reads=[PS(pd), "qcs"], writes=[("tmp64", i)])
                    P.op("pool", lambda e, i=i: e.tensor_tensor(qro[i], qro[i], tmp64[i], ALU.add), reads=[("qro", i), ("tmp64", i)], writes=[("qro", i)])
                    P.op("pool", lambda e, h=h, i=i: e.tensor_tensor(QR[:, h, :], qro[i], rsh[i][0:64, :], ALU.mult),
                         reads=[("qro", i), ("rsh", i)], writes=[("qor", tt % 2)])
                P.dma("act", QT.rearrange("h p t -> p h t")[0:128, :, tt * 512:(tt + 1) * 512], QN, reads=[("qon", tt % 2)],
                      semkey=("qons", tt % 2))
                P.dma("act", QT.rearrange("h p t -> p h t")[128:192, :, tt * 512:(tt + 1) * 512], QR, reads=[("qor", tt % 2)],
                      semkey=("qors", tt % 2))
            P.barrier()
            A.reset()

        def phase_attn():
            NKB = S // 128
            ktn = [A.alloc(S) for _ in range(2)]
            ktr = [A.alloc(S, parts=64) for _ in range(2)]
            vh = [A.alloc(NKB * 128).rearrange("p (k d) -> p k d", d=128) for _ in range(2)]
            qtn = [A.alloc(T) for _ in range(2)]
            qtr = [A.alloc(T, parts=64) for _ in range(2)]
            pt = [A.alloc(512) for _ in range(3)]
            rl = A.alloc(512, F32)
            yo = [A.alloc(512) for _ in range(2)]
            def prefetch(h):
                hb = h % 2; hk = ("head", hb)
                P.dma("sp", ktn[hb], KT[h, 0:128, :], writes=[hk], semkey=hk)
                P.dma("sp", ktr[hb], KT[h, 128:192, :], writes=[hk], semkey=hk)
                P.dma("sp", vh[hb], VTK.rearrange("(k p) f -> p k f", p=128)[:, :, h * 128:(h + 1) * 128], writes=[hk], semkey=hk)
                P.dma("sp", qtn[hb], QT[h, 0:128, :], writes=[hk], semkey=hk)
                P.dma("sp", qtr[hb], QT[h, 128:192, :], writes=[hk], semkey=hk)

            steps = []
            nch = 0
            for h in range(16):
                for qc in range(NTO):
                    nkb = 8 * qc + 8
                    for kb in range(nkb):
                        steps.append((h, qc, kb, nkb, nch))
                    nch += 1

            def emit_S(i):
                h, qc, kb, nkb, nch_ = steps[i]
                hb = h % 2; hk = ("head", hb)
                jlo = max(0, kb // 2 - 4 * qc)
                Nv = (4 - jlo) * 128
                q0 = qc * 512 + jlo * 128
                si = i % 3
                P.op("pe", lambda e: e.matmul(ps[si][:, 0:Nv], ktn[hb][:, kb * 128:(kb + 1) * 128], qtn[hb][:, q0:q0 + Nv], start=True, stop=False),
                     reads=[hk], writes=[PS(si)])
                P.op("pe", lambda e: e.matmul(ps[si][:, 0:Nv], ktr[hb][:, kb * 128:(kb + 1) * 128], qtr[hb][:, q0:q0 + Nv], start=False, stop=True),
                     reads=[hk], writes=[PS(si)])

            prefetch(0)
            emit_S(0)
            for i, (h, qc, kb, nkb, nch_) in enumerate(steps):
                hb = h % 2; hk = ("head", hb)
                if qc == 0 and kb == 0 and h + 1 < 16:
                    prefetch(h + 1)
                if i + 1 < len(steps):
                    emit_S(i + 1)
                po = 3 + (nch_ % 2) * 2; pl = po + 1
                jlo = max(0, kb // 2 - 4 * qc)
                Nv = (4 - jlo) * 128
                si = i % 3
                pb = pt[si]; pkey = ("pt", si)
                if jlo > 0:
                    P.op("pool", lambda e: e.memset(pb[:, 0:jlo * 128], 0.0), writes=[pkey])
                P.op("act", lambda e: e.activation(pb[:, jlo * 128:512], ps[si][:, 0:Nv], AF.Exp), reads=[PS(si)], writes=[pkey])
                if kb // 2 >= 4 * qc:
                    par = kb % 2
                    P.op("pool", lambda e: e.tensor_tensor(
                        pb[:, jlo * 128:(jlo + 1) * 128], pb[:, jlo * 128:(jlo + 1) * 128], masks[:, par * 128:(par + 1) * 128], ALU.mult),
                        reads=[pkey, "const"], writes=[pkey])
                P.op("pe", lambda e: e.matmul(ps[po][:], vh[hb][:, kb, :], pb, start=(kb == 0), stop=(kb == nkb - 1)),
                     reads=[hk, pkey], writes=[PS(po)])
                P.op("pe", lambda e: e.matmul(ps[pl][:], onesb, pb, start=(kb == 0), stop=(kb == nkb - 1)),
                     reads=["const", pkey], writes=[PS(pl)])
                if kb == nkb - 1:
                    P.op("dve", lambda e: e.reciprocal(rl, ps[pl][:]), reads=[PS(pl)], writes=["rl"])
                    yb = yo[nch_ % 2]; yk = ("yo", nch_ % 2)
                    P.op("dve", lambda e: e.tensor_tensor(yb, ps[po][:], rl, ALU.mult), reads=[PS(po), "rl"], writes=[yk])
                    P.dma("act", YMT[h * 128:(h + 1) * 128, qc * 512:(qc + 1) * 512], yb, reads=[yk], semkey=("yos", nch_ % 2))
            P.barrier()
            A.reset()

        def simple_pass(wsrc, in_scr, mode, aux1, aux2, dst):
            xin = [A.alloc(16 * 512).rearrange("p (k t) -> p k t", t=512) for _ in range(2)]
            a1 = [A.alloc(8 * 512).rearrange("p (c t) -> p c t", t=512) for _ in range(2)] if aux1 is not None else None
            a2 = [A.alloc(8 * 512).rearrange("p (c t) -> p c t", t=512) for _ in range(2)] if aux2 is not None else None
            ob = [A.alloc(8 * 512).rearrange("p (c t) -> p c t", t=512) for _ in range(2)]
            t32 = [A.alloc(512, F32) for _ in range(2)]
            u32 = [A.alloc(512, F32) for _ in range(2)]
            wts = [load_w(wsrc[:, half * 1024:(half + 1) * 1024], 1024) for half in range(2)]
            for half in range(2):
                wt, wkey = wts[half]
                cnt = [0]
                last_tt = [-1]

                def epi(fc, tt, pst, pk, half=half, cnt=cnt, last_tt=last_tt):
                    i = cnt[0] % 2; cnt[0] += 1
                    o = ob[tt % 2]; ok = ("ob", tt % 2)
                    if tt != last_tt[0]:
                        last_tt[0] = tt
                        for aux, ab, nm in ((aux1, a1, "a1"), (aux2, a2, "a2")):
                            if aux is not None:
                                P.dma("sp", ab[tt % 2], aux.rearrange("(c p) t -> p c t", p=128)[:, half * 8:(half + 1) * 8, tt * 512:(tt + 1) * 512],
                                      writes=[(nm, tt % 2)])
                    if mode == "gelu_mul":
                        t = t32[i]; tk = ("t32", i); u = u32[i]; uk = ("u32", i)
                        P.op("act", lambda e: e.activation(t, pst[:], AF.Square), reads=[pk], writes=[tk])
                        P.op("dve", lambda e: e.tensor_scalar(t, t, 0.044715, 1.0, ALU.mult, ALU.add), reads=[tk], writes=[tk])
                        P.op("dve", lambda e: e.tensor_tensor(t, t, pst[:], ALU.mult), reads=[tk, pk], writes=[tk])
                        P.op("act", lambda e: e.activation(t, t, AF.Sigmoid, scale=2.0 * math.sqrt(2.0 / PI)), reads=[tk], writes=[tk])
                        P.op("dve", lambda e: e.tensor_tensor(u, t, pst[:], ALU.mult), reads=[tk, pk], writes=[uk])
                        P.op("pool", lambda e: e.tensor_tensor(o[:, fc, :], u, a1[tt % 2][:, fc, :], ALU.mult),
                             reads=[uk, ("a1", tt % 2)], writes=[ok])
                    elif mode == "sigmoid":
                        P.op("act", lambda e: e.activation(o[:, fc, :], pst[:], AF.Sigmoid), reads=[pk], writes=[ok])
                    elif mode == "mul":
                        P.op("dve", lambda e: e.tensor_tensor(o[:, fc, :], pst[:], a1[tt % 2][:, fc, :], ALU.mult),
                             reads=[pk, ("a1", tt % 2)], writes=[ok])
                    elif mode == "mul_add":
                        u = u32[i]; uk = ("u32", i)
                        P.op("dve", lambda e: e.tensor_tensor(u, pst[:], a1[tt % 2][:, fc, :], ALU.mult),
                             reads=[pk, ("a1", tt % 2)], writes=[uk])
                        P.op("pool", lambda e: e.tensor_tensor(o[:, fc, :], u, a2[tt % 2][:, fc, :], ALU.add),
                             reads=[uk, ("a2", tt % 2)], writes=[ok])
                    if fc == 7:
                        P.dma("act", dst.rearrange("(c p) t -> p c t", p=128)[:, half * 8:(half + 1) * 8, tt * 512:(tt + 1) * 512], o,
                              reads=[ok], semkey=("obs", tt % 2))
                    return None
                fm_pass(wt, wkey, 1024, in_scr, T, epi, [0, 1, 2, 3], xin)
            P.barrier()
            A.reset()

        def make_row(dst, col, tagr):
            bt = A.alloc(128, F32)
            for j in range(16):
                P.op("dve", lambda e, j=j: e.tensor_copy(bt, col[:, j:j + 1].to_broadcast([128, 128])),
                     reads=["modc", "A2"], writes=["bt"])
                P.op("pe", lambda e: e.transpose(ps[7][:, 0:128], bt, ident32), reads=["bt", "const"], writes=[PS(7)])
                P.op("act", lambda e, j=j: e.copy(dst[:, j * 128:(j + 1) * 128], ps[7][:, 0:128]), reads=[PS(7)], writes=[tagr])

        NT128 = T // 128

        regc = {}

        def phase_out_route(rwk, gidx):
            g1row = A.alloc(D, F32); a2row = A.alloc(D, F32); sh2row = A.alloc(D, F32)
            make_row(g1row, modc[:, 32:48], "g1row")
            make_row(a2row, A2, "a2row")
            make_row(sh2row, modc[:, 48:64], "sh2row")
            wo = [load_w(I.w_out[:, half * 1024:(half + 1) * 1024], 1024) for half in range(2)]
            wr32 = A.alloc(16 * 64, F32).rearrange("p (k e) -> p k e", e=64)
            P.dma("sp", wr32, I.w_router.rearrange("(k p) e -> p k e", p=128), writes=["wr32"])
            xin = [A.alloc(16 * 512).rearrange("p (k t) -> p k t", t=512) for _ in range(2)]
            xo = [A.alloc(D, F32) for _ in range(2)]
            vtk = A.alloc(D, F32)
            vbf = [A.alloc(D) for _ in range(2)]
            junk = A.alloc(D)
            vT = A.alloc(16 * 128, F32).rearrange("p (k t) -> p k t", t=128)
            sm = A.alloc(64, F32)
            sc_ = A.alloc(64, F32); sel = A.alloc(64, F32); msk = A.alloc(64, F32); rw = A.alloc(64, F32)
            mskb = A.alloc(64); pos = A.alloc(64, F32); carry = A.alloc(64, F32); oh = A.alloc(64, F32)
            mx8 = A.alloc(8, F32); ix8 = A.alloc(8, U32); ixf = A.alloc(8, F32); posk = A.alloc(8, F32)
            dst_f = A.alloc(8, F32); ovf = A.alloc(8, F32); dsti = [A.alloc(8, I32) for _ in range(2)]
            P.op("pool", lambda e: e.memset(carry, 0.0), writes=["carry"])
            mT = MT.rearrange("(k p) t -> p k t", p=128)
            for tt in range(NTO):
                xb = xin[tt % 2]; xk = ("xin", tt % 2)
                P.dma("sp", xb, mT[:, :, tt * 512:(tt + 1) * 512], writes=[xk])
                for sub in range(4):
                    ti = tt * 4 + sub
                    r0 = ti * 128
                    X = xo[ti % 2]; Xk = ("xo", ti % 2)
                    P.dma("sp", X, I.x_own[r0:r0 + 128, :], writes=[Xk])
                    for dc in range(4):
                        wt, wkey = wo[dc // 2]
                        for k in range(16):
                            P.op("pe", lambda e, dc=dc, k=k, wt=wt, xb=xb, sub=sub: e.matmul(
                                ps[dc][:], xb[:, k, sub * 128:(sub + 1) * 128], wt[:, k, (dc % 2) * 512:(dc % 2 + 1) * 512],
                                start=(k == 0), stop=(k == 15)), reads=[wkey, xk], writes=[PS(dc)])
                        P.op("dve", lambda e, dc=dc: e.tensor_tensor(vtk[:, dc * 512:(dc + 1) * 512], ps[dc][:], g1row[:, dc * 512:(dc + 1) * 512], ALU.mult),
                             reads=[PS(dc), "g1row"], writes=[("vtk", dc)])
                        P.op("pool", lambda e, dc=dc, X=X: e.tensor_tensor(X[:, dc * 512:(dc + 1) * 512], X[:, dc * 512:(dc + 1) * 512], vtk[:, dc * 512:(dc + 1) * 512], ALU.add),
                             reads=[Xk, ("vtk", dc)], writes=[Xk])
                    P.dma("act", X1[r0:r0 + 128, :], X, reads=[Xk], semkey=("x1s", ti % 2))
                    ss = sm[:, 0:1]; rs_ = sm[:, 1:2]
                    P.op("act", lambda e, X=X: e.activation(junk, X, AF.Square, accum_out=ss), reads=[Xk], writes=["junk", "ss"])
                    P.op("act", lambda e: e.activation(rs_, ss, AF.Sqrt, scale=1.0 / D, bias=EPS), reads=["ss"], writes=["rs_"])
                    P.op("dve", lambda e: e.reciprocal(rs_, rs_), reads=["rs_"], writes=["rs_"])
                    P.op("dve", lambda e, X=X: e.scalar_tensor_tensor(vtk, X, rs_, a2row, ALU.mult, ALU.mult),
                         reads=[Xk, "rs_", "a2row"] + [("vtk", d) for d in range(4)], writes=[("vtk", d) for d in range(4)] + ["vtkall"])
                    P.op("pool", lambda e: e.tensor_tensor(vtk, vtk, sh2row, ALU.add), reads=["vtkall", "sh2row"], writes=["vtkall"] + [("vtk", d) for d in range(4)])
                    VB = vbf[ti % 2]; VBk = ("vbf", ti % 2)
                    P.op("act", lambda e, VB=VB: e.copy(VB, vtk), reads=["vtkall"], writes=[VBk])
                    for q4 in range(4):
                        pi = 4 + q4 % 2
                        for j in range(4):
                            fc = q4 * 4 + j
                            P.op("pe", lambda e, fc=fc, j=j, pi=pi: e.transpose(ps[pi][:, j * 128:(j + 1) * 128], vtk[:, fc * 128:(fc + 1) * 128], ident32),
                                 reads=["vtkall", "const"], writes=[PS(pi)])
                        P.op("act", lambda e, q4=q4, pi=pi: e.copy(vT[:, q4 * 4:(q4 + 1) * 4, :].rearrange("p k t -> p (k t)"), ps[pi][:]),
                             reads=[PS(pi)], writes=["vT"])
                    for k in range(16):
                        P.op("pe", lambda e, k=k: e.matmul(ps[6][:, 0:64], vT[:, k, :], wr32[:, k, :], start=(k == 0), stop=(k == 15)),
                             reads=["vT", "wr32"], writes=[PS(6)])
                    P.op("act", lambda e: e.activation(sc_, ps[6][:, 0:64], AF.Sigmoid), reads=[PS(6)], writes=["sc_"])
                    P.op("dve", lambda e: e.tensor_tensor(sel, sc_, rbias, ALU.add), reads=["sc_", "const"], writes=["sel"])
                    P.op("dve", lambda e: e.max(mx8, sel), reads=["sel"], writes=["mx8"])
                    P.op("dve", lambda e: e.max_index(ix8, mx8, sel), reads=["sel", "mx8"], writes=["ix8"])
                    P.op("dve", lambda e: e.tensor_scalar(msk, sel, mx8[:, 5:6], None, ALU.is_ge), reads=["sel", "mx8"], writes=["msk"])
                    P.op("dve", lambda e: e.tensor_tensor(rw, sc_, msk, ALU.mult), reads=["sc_", "msk"], writes=["rw"])
                    den = sm[:, 2:3]
                    P.op("dve", lambda e: e.reduce_sum(den, rw, axis=AX.X), reads=["rw"], writes=["den"])
                    P.op("dve", lambda e: e.reciprocal(den, den), reads=["den"], writes=["den"])
                    P.op("dve", lambda e: e.tensor_scalar(rw, rw, den, 2.5, ALU.mult, ALU.mult), reads=["rw", "den"], writes=["rw"])
                    P.op("act", lambda e: e.copy(mskb, msk), reads=["msk"], writes=["mskb"])
                    P.op("pe", lambda e: e.matmul(ps[7][:, 0:64], ustr, mskb, start=True, stop=True), reads=["const", "mskb"], writes=[PS(7)])
                    P.op("pe", lambda e: e.matmul(ps[7][:, 64:128], onesb, mskb, start=True, stop=True), reads=["const", "mskb"], writes=[PS(7)])
                    P.op("dve", lambda e: e.tensor_tensor(pos, ps[7][:, 0:64], carry, ALU.add), reads=[PS(7), "carry"], writes=["pos"])
                    P.op("dve", lambda e: e.tensor_tensor(carry, ps[7][:, 64:128], carry, ALU.add), reads=[PS(7), "carry"], writes=["carry"])
                    P.op("dve", lambda e: e.tensor_copy(ixf, ix8), reads=["ix8"], writes=["ixf"])
                    for k in range(TOPK):
                        P.op("dve", lambda e, k=k: e.tensor_scalar(oh, iota, ixf[:, k:k + 1], None, ALU.is_equal), reads=["const", "ixf"], writes=["oh"])
                        P.op("dve", lambda e: e.tensor_tensor(msk, oh, pos, ALU.mult), reads=["oh", "pos", "msk"], writes=["msk"])
                        P.op("dve", lambda e, k=k: e.reduce_sum(posk[:, k:k + 1], msk, axis=AX.X), reads=["msk"], writes=["posk"])
                        P.op("dve", lambda e: e.tensor_tensor(msk, oh, rw, ALU.mult), reads=["oh", "rw", "msk"], writes=["msk"])
                        P.op("dve", lambda e, k=k, ti=ti: e.reduce_sum(rwk[:, ti * 8 + k:ti * 8 + k + 1], msk, axis=AX.X), reads=["msk"], writes=["rwk"])
                    P.op("dve", lambda e: e.scalar_tensor_tensor(dst_f[:, 0:6], ixf[:, 0:6], float(C), posk[:, 0:6], ALU.mult, ALU.add),
                         reads=["ixf", "posk"], writes=["dst_f"])
                    P.op("dve", lambda e: e.tensor_scalar(ovf[:, 0:6], posk[:, 0:6], float(C), None, ALU.is_ge), reads=["posk"], writes=["ovf"])
                    P.op("dve", lambda e: e.tensor_scalar(posk[:, 0:6], ovf[:, 0:6], -1.0, 1.0, ALU.mult, ALU.add), reads=["ovf", "posk"], writes=["posk"])
                    P.op("dve", lambda e, ti=ti: e.tensor_tensor(rwk[:, ti * 8:ti * 8 + 6], rwk[:, ti * 8:ti * 8 + 6], posk[:, 0:6], ALU.mult),
                         reads=["posk", "rwk"], writes=["rwk"])
                    P.op("dve", lambda e: e.tensor_tensor(dst_f[:, 0:6], dst_f[:, 0:6], posk[:, 0:6], ALU.mult), reads=["dst_f", "posk"], writes=["dst_f"])
                    DI = dsti[ti % 2]; DIk = ("dsti", ti % 2)
                    P.op("dve", lambda e: e.scalar_tensor_tensor(posk[:, 0:6], ovf[:, 0:6], 4194304.0, dst_f[:, 0:6], ALU.mult, ALU.add),
                         reads=["ovf", "dst_f", "posk"], writes=["posk"])
                    P.op("dve", lambda e, DI=DI: e.tensor_copy(DI[:, 0:6], posk[:, 0:6]), reads=["posk"], writes=[DIk])
                    P.op("dve", lambda e, DI=DI, ti=ti: e.tensor_copy(gidx[:, ti * 8:ti * 8 + 6], dst_f[:, 0:6]), reads=["dst_f"], writes=["gidx"])
                    for k in range(TOPK):
                        def scat(e, DI=DI, VB=VB, k=k):
                            if "r" not in regc:
                                regc["r"] = e.to_reg(NROW - 1)
                            return e.indirect_dma_start(
                                out=XG, out_offset=bass.IndirectOffsetOnAxis(ap=DI[:, k:k + 1], axis=0), in_=VB, in_offset=None,
                                bounds_check=regc["r"], oob_is_err=False)
                        P.raw("pool", scat,
                            reads=[DIk, VBk], writes=[], semkey=("sc", ti % 2))
                    P.dma("act", XS[r0:r0 + 128, :], VB, reads=[VBk], semkey=("xgs", ti % 2))
            P.barrier()
            A.reset()

        def phase_experts():
            NH = C // 512
            xg = [A.alloc(4 * D).rearrange("p (b f) -> p b f", f=D) for _ in range(2)]
            xT = A.alloc(16 * C).rearrange("p (k s) -> p k s", s=C)
            wg = [A.alloc(16 * 128).rearrange("p (k f) -> p k f", f=128) for _ in range(3)]
            wu = [A.alloc(16 * 128).rearrange("p (k f) -> p k f", f=128) for _ in range(3)]
            hT = A.alloc(NFC * C).rearrange("p (f s) -> p f s", s=C)
            wd = [A.alloc(NFC * 512).rearrange("p (f d) -> p f d", d=512) for _ in range(3)]
            sg = [A.alloc(512, F32) for _ in range(2)]
            yb = [A.alloc(512) for _ in range(4)]
            nxg = 0; nw = 0; nwd = 0; nsg = 0; ny = 0; npsA = 0; ntr = 0
            for ex in range(NX):
                if ex > 0 and ex % 8 == 0:
                    P.barrier()
                if ex < NE:
                    Wg = getattr(I, "wg%02d" % (ex // 4))[ex % 4]; Wu = getattr(I, "wu%02d" % (ex // 4))[ex % 4]
                    Wd = getattr(I, "wd%02d" % (ex // 4))[ex % 4]
                    wq_, wdeps = "pool", []
                else:
                    Wg = I.ws_gate; Wu = I.ws_up; Wd = I.ws_down
                    wq_, wdeps = "pool", []
                for hh_ in range(NH):
                    g = xg[nxg % 2]; gk = ("xg", nxg % 2); nxg += 1
                    r0 = (ex if ex < NE else ex - NE) * C + hh_ * 512
                    XSRC = XG if ex < NE else XS
                    P.dma("sp", g, XSRC[r0:r0 + 512, :].rearrange("(b p) f -> p b f", p=128), writes=[gk])
                    for k in range(16):
                        pi = 6 + ntr % 2; ntr += 1
                        pbf = ps[pi][:].bitcast(BF16)
                        for b4 in range(4):
                            P.op("pe", lambda e, g=g, b4=b4, k=k, pbf=pbf: e.transpose(pbf[:, b4 * 128:(b4 + 1) * 128], g[:, b4, k * 128:(k + 1) * 128], identb),
                                 reads=[gk, "const"], writes=[PS(pi)])
                        eng = "act" if k % 2 == 0 else "dve"
                        if eng == "act":
                            P.op("act", lambda e, k=k, hh_=hh_, pbf=pbf: e.copy(xT[:, k, hh_ * 512:(hh_ + 1) * 512], pbf[:, 0:512]),
                                 reads=[PS(pi)], writes=["xT"])
                        else:
                            P.op("dve", lambda e, k=k, hh_=hh_, pbf=pbf: e.tensor_copy(xT[:, k, hh_ * 512:(hh_ + 1) * 512], pbf[:, 0:512]),
                                 reads=[PS(pi)], writes=["xT"])
                Wgv = Wg.rearrange("(k p) f -> p k f", p=128); Wuv = Wu.rearrange("(k p) f -> p k f", p=128)
                for fc in range(NFC):
                    wi = nw % 3; nw += 1
                    P.dma(wq_, wg[wi], Wgv[:, :, fc * 128:(fc + 1) * 128], reads=wdeps[0:1], writes=[("wg", wi)])
                    P.dma(wq_, wu[wi], Wuv[:, :, fc * 128:(fc + 1) * 128], reads=wdeps[1:2], writes=[("wu", wi)])
                    for hh_ in range(NH):
                        pg = (npsA % 3) * 2; pu = pg + 1; npsA += 1
                        for k in range(16):
                            P.op("pe", lambda e, wi=wi, k=k, hh_=hh_, pg=pg: e.matmul(ps[pg][:], wg[wi][:, k, :], xT[:, k, hh_ * 512:(hh_ + 1) * 512],
                                                                                    start=(k == 0), stop=(k == 15)),
                                 reads=[("wg", wi), "xT"], writes=[PS(pg)])
                        for k in range(16):
                            P.op("pe", lambda e, wi=wi, k=k, hh_=hh_, pu=pu: e.matmul(ps[pu][:], wu[wi][:, k, :], xT[:, k, hh_ * 512:(hh_ + 1) * 512],
                                                                                    start=(k == 0), stop=(k == 15)),
                                 reads=[("wu", wi), "xT"], writes=[PS(pu)])
                        s = sg[nsg % 2]; sk_ = ("sg", nsg % 2); nsg += 1
                        P.op("act", lambda e, s=s, pg=pg: e.activation(s, ps[pg][:], AF.Silu), reads=[PS(pg)], writes=[sk_])
                        P.op("dve", lambda e, s=s, pu=pu, fc=fc, hh_=hh_: e.tensor_tensor(hT[:, fc, hh_ * 512:(hh_ + 1) * 512], s, ps[pu][:], ALU.mult),
                             reads=[sk_, PS(pu)], writes=["hT"])
                Wdv = Wd.rearrange("(f p) d -> p f d", p=128)
                for dc in range(4):
                    wi = nwd % 3; nwd += 1
                    P.dma(wq_, wd[wi], Wdv[:, :, dc * 512:(dc + 1) * 512], reads=wdeps[2:3], writes=[("wd", wi)])
                    for sb in range(C // 128):
                        pi = (npsA % 3) * 2 + (sb % 2);
                        if sb % 2 == 1:
                            npsA += 1
                        for fc in range(NFC):
                            P.op("pe", lambda e, wi=wi, fc=fc, sb=sb, pi=pi: e.matmul(ps[pi][:], hT[:, fc, sb * 128:(sb + 1) * 128], wd[wi][:, fc, :],
                                                                                    start=(fc == 0), stop=(fc == NFC - 1)),
                                 reads=[("wd", wi), "hT"], writes=[PS(pi)])
                        y = yb[ny % 4]; yk = ("yb", ny % 4)
                        if ny % 2 == 0:
                            P.op("act", lambda e, y=y, pi=pi: e.copy(y, ps[pi][:]), reads=[PS(pi)], writes=[yk])
                        else:
                            P.op("dve", lambda e, y=y, pi=pi: e.tensor_copy(y, ps[pi][:]), reads=[PS(pi)], writes=[yk])
                        r0 = (ex if ex < NE else ex - NE) * C + sb * 128
                        YDST = YG if ex < NE else YS
                        P.dma("act", YDST[r0:r0 + 128, dc * 512:(dc + 1) * 512], y, reads=[yk], semkey=("ybs", ny % 4))
                        ny += 1
                    if C // 128 % 2 == 1:
                        npsA += 1
            P.barrier()
            A.reset()

        def phase_combine(rwk, gidx):
            g2row = A.alloc(D, F32)
            make_row(g2row, modc[:, 80:96], "g2row")
            yg = [A.alloc(7 * D).rearrange("p (k f) -> p k f", f=D) for _ in range(2)]
            acc = [A.alloc(D, F32) for _ in range(2)]
            x1 = [A.alloc(D, F32) for _ in range(2)]
            for ti in range(NT128):
                r0 = ti * 128
                Y = yg[ti % 2]; Yk = ("yg", ti % 2)
                for k in range(TOPK):
                    P.raw("pool", lambda e, Y=Y, k=k, ti=ti: e.indirect_dma_start(
                        out=Y[:, k, :], out_offset=None, in_=YG, in_offset=bass.IndirectOffsetOnAxis(ap=gidx[:, ti * 8 + k:ti * 8 + k + 1], axis=0)),
                        reads=["gidx"], writes=[Yk], semkey=Yk)
                P.dma("sp", Y[:, 6, :], YS[r0:r0 + 128, :], writes=[Yk], semkey=Yk)
                X = x1[ti % 2]; Xk = ("x1", ti % 2)
                P.dma("sp", X, X1[r0:r0 + 128, :], writes=[Xk])
                a = acc[ti % 2]; ak = ("acc", ti % 2)
                P.op("dve", lambda e, a=a, Y=Y, ti=ti: e.scalar_tensor_tensor(a, Y[:, 0, :], rwk[:, ti * 8:ti * 8 + 1], Y[:, 6, :], ALU.mult, ALU.add),
                     reads=[Yk, "rwk"], writes=[ak])
                for k in range(1, TOPK):
                    P.op("dve", lambda e, a=a, Y=Y, ti=ti, k=k: e.scalar_tensor_tensor(a, Y[:, k, :], rwk[:, ti * 8 + k:ti * 8 + k + 1], a, ALU.mult, ALU.add),
                         reads=[Yk, "rwk", ak], writes=[ak])
                P.op("dve", lambda e, a=a: e.tensor_tensor(a, a, g2row, ALU.mult), reads=[ak, "g2row"], writes=[ak])
                P.op("pool", lambda e, a=a, X=X: e.tensor_tensor(X, X, a, ALU.add), reads=[ak, Xk], writes=[Xk])
                P.dma("act", out_d[r0:r0 + 128, :], X, reads=[Xk], semkey=("outs", ti % 2))
            P.barrier()

        def zero_scratch():
            z = A.alloc(4 * D).rearrange("p (b f) -> p b f", f=D)
            P.op("pool", lambda e: e.memset(z, 0.0), writes=["z"])
            for r in range(0, 512, 512):
                P.dma("sp", XG[r:r + 512, :].rearrange("(b p) f -> p b f", p=128), z, reads=["z"], semkey="zx")
            P.barrier()
            A.reset()

        rwk = A.alloc(NT128 * 8, F32)
        gidx = A.alloc(NT128 * 8, I32)
        A.mark()

        phases = dbg_phases if (dbg_phases := getattr(build, "phases", None)) else None
        def want(nm):
            return phases is None or nm in phases
        if want("zero"):
            zero_scratch()
        if want("p0"):
            phase0()
        else:
            P.dma("sp", modc, MODC, writes=["modc"])
        if want("p1"):
            norm_T(I.x_all, S, UTA, A1, modc[:, 0:16])
            norm_T(I.x_own, T, UTO, A1, modc[:, 0:16])
        if want("rnn"):
            phase_rnn()
        if want("kv"):
            phase_kv()
        if want("q"):
            phase_q()
        if want("attn"):
            phase_attn()
        if want("merge"):
            simple_pass(I.w_in[:, 2048:4096], UTO, "gelu_mul", HT, None, YRT)
            simple_pass(I.w_in[:, 4928:6976], UTO, "sigmoid", None, None, GAT)
            simple_pass(I.w_in[:, 6976:9024], UTO, "sigmoid", None, None, GBT)
            simple_pass(I.w_rnn_out, YRT, "mul", GAT, None, MA)
            simple_pass(I.w_mla_out, YMT, "mul_add", GBT, MA, MT)
        if want("route"):
            phase_out_route(rwk, gidx)
        if want("experts"):
            phase_experts()
        if want("combine"):
            phase_combine(rwk, gidx)
        P.barrier()
        P.emit()
    nc._declared = declared
    return nc


def _col(v, n):
    return np.ascontiguousarray(np.asarray(v, np.float32).reshape(n, 128).T)


def make_in_maps(S, inp):
    bf = ml_dtypes.bfloat16
    x = np.asarray(inp["x"]); B = x.shape[0]
    pos = np.asarray(inp["positions"]).astype(np.int32)
    sq = lambda k: np.ascontiguousarray(np.asarray(inp[k])[0])
    ident = np.eye(128, dtype=np.float32)
    ustr = np.triu(np.ones((128, 128), np.float32), 1)
    rot = np.zeros((64, 64), np.float32)
    for m in range(32):
        rot[m + 32, m] = -1.0
        rot[m, m + 32] = 1.0
    invf = (np.float32(10000.0) ** (-np.arange(0, 64, 2, dtype=np.float32) / np.float32(64))).astype(np.float32)
    invf2 = np.concatenate([invf, invf]).reshape(64, 1).astype(np.float32)
    iota = np.broadcast_to(np.arange(64, dtype=np.float32)[None, :], (128, 64)).copy()
    vec16 = np.concatenate([_col(sq("norm1"), 16), _col(sq("norm2"), 16), _col(sq("conv_b"), 16), _col(sq("b_a"), 16),
                            _col(sq("b_i"), 16), _col(sq("lru_lambda"), 16), np.zeros((128, 16), np.float32)], axis=1)
    cw = sq("conv_w")
    convw = np.concatenate([_col(cw[j], 16) for j in range(4)], axis=1)
    qn = sq("q_norm"); kn = sq("k_norm")
    qkn = np.zeros((128, 4), np.float32)
    qkn[:, 0] = qn[:128]; qkn[:64, 1] = qn[128:]; qkn[:, 2] = kn[:128]; qkn[:64, 3] = kn[128:]
    shared = dict(
        ident32=ident, identb=ident.astype(bf), onesb=np.ones((128, 128), bf), ustrict=ustr.astype(bf),
        rot=rot.astype(bf), iota_row=iota, invf=invf2, bmod_col=_col(sq("b_mod"), 96), vec16=vec16, convw_col=convw,
        qan_col=_col(sq("q_a_norm"), 4), kvan_col=_col(sq("kv_a_norm"), 2), qkn_col=qkn,
        rbias_row=np.broadcast_to(sq("router_bias")[None, :], (128, 64)).copy(),
        w_mod=sq("w_mod"), w_in=sq("w_in"), w_a=sq("w_a"), w_i=sq("w_i"),
        w_uq=sq("w_uq").reshape(512, 3072), w_ukv=sq("w_ukv").reshape(256, 4096),
        w_rnn_out=sq("w_rnn_out"), w_mla_out=sq("w_mla_out"), w_out=sq("w_out"), w_router=sq("w_router"),
        ws_gate=sq("ws_gate"), ws_up=sq("ws_up"), ws_down=sq("ws_down"),
    )
    wg_ = np.asarray(inp["w_gate"])[0]; wu_ = np.asarray(inp["w_up"])[0]; wd_ = np.asarray(inp["w_down"])[0]
    for e_ in range(NE // 4):
        shared["wg%02d" % e_] = wg_[4 * e_:4 * e_ + 4]; shared["wu%02d" % e_] = wu_[4 * e_:4 * e_ + 4]
        shared["wd%02d" % e_] = wd_[4 * e_:4 * e_ + 4]
    maps = []
    for core in range(2 * B):
        b = core // 2; c = core % 2
        xb = np.ascontiguousarray(x[b])
        xo = np.ascontiguousarray(xb.reshape(S // 128, 128, D)[c::2].reshape(S // 2, D))
        pa = pos[b]
        po = np.ascontiguousarray(pa.reshape(S // 128, 128)[c::2].reshape(S // 2))
        q = np.arange(128)[None, :] // 64; k = np.arange(128)[:, None] // 64
        diag = (k <= q).astype(np.float32)
        if c == 0:
            m_even = diag; m_odd = np.zeros((128, 128), np.float32)
        else:
            m_even = np.ones((128, 128), np.float32); m_odd = diag
        m = dict(shared)
        m.update(

            x_all=xb, x_own=xo,
            pos_all=np.broadcast_to(pa[None, :], (64, S)).copy(), pos_own=np.broadcast_to(po[None, :], (64, S // 2)).copy(),
            c_col=_col(np.asarray(inp["c"])[b], 16),
            cpar=np.broadcast_to(np.array([[c, 1 - c]], np.float32), (128, 2)).copy(),
            masks=np.concatenate([m_even, m_odd], axis=1).astype(bf),
        )
        maps.append(m)
    return maps


_CACHE = {}


def run(inp, S, C, dbg=(), ncores=None):
    key = (S, C, tuple(dbg))
    if key not in _CACHE:
        _CACHE[key] = build(S, C, dbg)
    nc = _CACHE[key]
    maps = make_in_maps(S, inp)
    if ncores is not None:
        maps = maps[:ncores]
    maps = [{k: m[k] for k in nc._declared} for m in maps]
    res = run_bass_kernel_spmd(nc, maps, core_ids=list(range(len(maps))))
    return res


def kernel(**inputs):
    x = np.asarray(inputs["x"])
    B, S, _ = x.shape
    res = run(inputs, S, 1024)
    out = np.empty((B, S, D), np.float32)
    for core, r in enumerate(res.results):
        b = core // 2; c = core % 2
        out[b].reshape(S // 128, 128, D)[c::2] = r["out"].reshape(S // 256, 128, D)
    return out
```
